# Optimizing a Trainium2 kernel written in Bass

```python
import math
import jax, jax.numpy as jnp
from jax import lax
import numpy as np

D_MODEL = 2048
BATCH = 2
SEQ = 16384
DEPTH = 2

CHUNK = 64
Q_BLOCK = 128
NORM_EPS = 1e-6

A_HEADS = 8
A_HEAD_DIM = 128
IDX_HEADS = 16
IDX_DIM = 64
TOPK_MAX = 256
POOL_WINDOWS = (2, 4, 8, 16)
POOL_GROUP = 256
POOL_WIDTH = POOL_GROUP * len(POOL_WINDOWS)
C_HEADS = 8
Q_LORA = 512
KV_LORA = 256
QK_NOPE = 128
QK_ROPE = 64
V_HEAD = 128
ROPE_THETA = 10000.0
LRU_WIDTH = 1024
LRU_BLOCKS = 8
LRU_CONV = 4
LRU_C = 8.0
D_FF = 4096
FFN_CONV = 3

A_WIDTH = A_HEADS * A_HEAD_DIM
AB_IN = 3 * A_WIDTH + IDX_HEADS * IDX_DIM + IDX_DIM + IDX_HEADS + POOL_WIDTH
AB_OUT = A_WIDTH + POOL_WIDTH
CD_IN = Q_LORA + KV_LORA + QK_ROPE + 2 * LRU_WIDTH
CD_OUT = C_HEADS * V_HEAD + LRU_WIDTH
N_EVEN = (DEPTH + 1) // 2
N_ODD = DEPTH // 2

kernel_name = 'hybrid_dsa_pool_mla_rglru_convffn'


def rms_norm(x, g):
    xf = x.astype(jnp.float32)
    y = xf * lax.rsqrt(jnp.mean(xf * xf, axis=-1, keepdims=True) + NORM_EPS)
    return (y * g.astype(jnp.float32)).astype(x.dtype)


def causal_dwconv(x, w, b):
    K = w.shape[0]
    T = x.shape[1]
    xp = jnp.pad(x, ((0, 0), (K - 1, 0), (0, 0)))
    y = xp[:, 0:T] * w[0]
    for k in range(1, K):
        y = y + xp[:, k:k + T] * w[k]
    return y + b


def chunk_limit(q_pos):
    return (q_pos // CHUNK + 1) * CHUNK


def rope(x, pos):
    half = x.shape[-1] // 2
    freq = ROPE_THETA ** (-jnp.arange(half, dtype=jnp.float32) / half)
    ang = pos.astype(jnp.float32)[:, :, None, None] * freq
    cos, sin = jnp.cos(ang), jnp.sin(ang)
    xf = x.astype(jnp.float32)
    x1, x2 = xf[..., :half], xf[..., half:]
    return jnp.concatenate([x1 * cos - x2 * sin, x2 * cos + x1 * sin], axis=-1).astype(x.dtype)


def dsa_attention(q, k, v, q_idx, k_idx, w_idx):
    Bsz, T = q.shape[:2]
    k_top = min(TOPK_MAX, T // 4)
    n_blocks = T // Q_BLOCK
    key_pos = jnp.arange(T)
    idx_scale = (IDX_DIM ** -0.5) * (IDX_HEADS ** -0.5)
    att_scale = A_HEAD_DIM ** -0.5

    def block(i):
        start = i * Q_BLOCK
        qb = lax.dynamic_slice_in_dim(q, start, Q_BLOCK, 1)
        qib = lax.dynamic_slice_in_dim(q_idx, start, Q_BLOCK, 1)
        wb = lax.dynamic_slice_in_dim(w_idx, start, Q_BLOCK, 1)
        limit = chunk_limit(start + jnp.arange(Q_BLOCK))
        admissible = key_pos[None, :] < limit[:, None]
        logits = jnp.einsum('bqhd,bsd->bqhs', qib, k_idx, preferred_element_type=jnp.float32)
        score = jnp.einsum('bqhs,bqh->bqs', jax.nn.relu(logits), wb.astype(jnp.float32)) * idx_scale
        score = jnp.where(admissible[None], score, -jnp.inf)
        _, sel = lax.top_k(score, k_top)
        sel_ok = sel < limit[None, :, None]
        k_sel = jax.vmap(lambda kb, ib: kb[ib])(k, sel)
        v_sel = jax.vmap(lambda vb, ib: vb[ib])(v, sel)
        s = jnp.einsum('bqhd,bqkhd->bhqk', qb, k_sel, preferred_element_type=jnp.float32) * att_scale
        s = jnp.where(sel_ok[:, None], s, -jnp.inf)
        p = jax.nn.softmax(s, axis=-1)
        return jnp.einsum('bhqk,bqkhd->bqhd', p.astype(v.dtype), v_sel)

    out = lax.map(block, jnp.arange(n_blocks))
    return jnp.moveaxis(out, 0, 1).reshape(Bsz, T, A_WIDTH)


def multiscale_pool(u, w, b, scale):
    Bsz, T, _ = u.shape
    G = len(POOL_WINDOWS)
    uf = u.reshape(Bsz, T, G, POOL_GROUP).astype(jnp.float32)
    csum = jnp.cumsum(uf, axis=1)
    t = jnp.arange(T)
    outs = []
    for g, win in enumerate(POOL_WINDOWS):
        c = csum[:, :, g]
        c_prev = jnp.pad(c, ((0, 0), (win, 0), (0, 0)))[:, :T]
        count = jnp.minimum(t + 1, win).astype(jnp.float32)[None, :, None]
        outs.append((c - c_prev) / count - uf[:, :, g])
    pooled = jnp.stack(outs, axis=2).astype(u.dtype)
    mixed = jnp.einsum('btgc,gcd->btgd', pooled, w) + b
    return mixed.reshape(Bsz, T, POOL_WIDTH) * scale


def mla_attention(c_q, c_kv, k_rope_in, positions, q_norm_g, w_q_up, kv_norm_g, w_kv_up):
    Bsz, T = c_q.shape[:2]
    q = (rms_norm(c_q, q_norm_g) @ w_q_up).reshape(Bsz, T, C_HEADS, QK_NOPE + QK_ROPE)
    kv = (rms_norm(c_kv, kv_norm_g) @ w_kv_up).reshape(Bsz, T, C_HEADS, QK_NOPE + V_HEAD)
    q_nope = q[..., :QK_NOPE]
    q_rope = rope(q[..., QK_NOPE:], positions)
    k_nope = kv[..., :QK_NOPE]
    v = kv[..., QK_NOPE:]
    k_rope = rope(k_rope_in[:, :, None, :], positions)[:, :, 0]
    scale = (QK_NOPE + QK_ROPE) ** -0.5
    key_pos = jnp.arange(T)
    n_blocks = T // Q_BLOCK

    def block(i):
        start = i * Q_BLOCK
        qn = lax.dynamic_slice_in_dim(q_nope, start, Q_BLOCK, 1)
        qr = lax.dynamic_slice_in_dim(q_rope, start, Q_BLOCK, 1)
        limit = chunk_limit(start + jnp.arange(Q_BLOCK))
        admissible = key_pos[None, :] < limit[:, None]
        s = (jnp.einsum('bqhd,bshd->bhqs', qn, k_nope, preferred_element_type=jnp.float32)
             + jnp.einsum('bqhr,bsr->bhqs', qr, k_rope, preferred_element_type=jnp.float32)) * scale
        s = jnp.where(admissible[None, None], s, -jnp.inf)
        p = jax.nn.softmax(s, axis=-1)
        return jnp.einsum('bhqs,bshd->bqhd', p.astype(v.dtype), v)

    out = lax.map(block, jnp.arange(n_blocks))
    return jnp.moveaxis(out, 0, 1).reshape(Bsz, T, C_HEADS * V_HEAD)


def rg_lru_branch(x_in, y_in, conv_w, conv_b, wa, ba, wx, bx, lam):
    Bsz, T, _ = x_in.shape
    xc = causal_dwconv(x_in, conv_w, conv_b)
    xb = xc.reshape(Bsz, T, LRU_BLOCKS, LRU_WIDTH // LRU_BLOCKS)
    gate_r = jax.nn.sigmoid(jnp.einsum('btnc,ncd->btnd', xb, wa).reshape(Bsz, T, LRU_WIDTH) + ba)
    gate_i = jax.nn.sigmoid(jnp.einsum('btnc,ncd->btnd', xb, wx).reshape(Bsz, T, LRU_WIDTH) + bx)
    log_a = (-LRU_C * gate_r.astype(jnp.float32)) * jax.nn.softplus(-lam.astype(jnp.float32))
    a = jnp.exp(log_a)
    mult = jnp.sqrt(-jnp.expm1(2.0 * log_a))
    bterm = mult * (gate_i * xc).astype(jnp.float32)

    def combine(lhs, rhs):
        a_l, b_l = lhs
        a_r, b_r = rhs
        return a_l * a_r, a_r * b_l + b_r

    _, h = lax.associative_scan(combine, (a, bterm), axis=1)
    return h.astype(x_in.dtype) * jax.nn.gelu(y_in, approximate=True)


def mixer_ab(h, w_in, pool_w, pool_b, pool_scale, w_out):
    Bsz, T, _ = h.shape
    z = h @ w_in
    sizes = [A_WIDTH, A_WIDTH, A_WIDTH, IDX_HEADS * IDX_DIM, IDX_DIM, IDX_HEADS]
    cuts = [int(c) for c in np.cumsum(sizes)]
    q, k, v, qi, ki, wi, u = jnp.split(z, cuts, axis=-1)
    heads = (Bsz, T, A_HEADS, A_HEAD_DIM)
    a_out = dsa_attention(q.reshape(heads), k.reshape(heads), v.reshape(heads),
                          qi.reshape(Bsz, T, IDX_HEADS, IDX_DIM), ki, wi)
    b_out = multiscale_pool(u, pool_w, pool_b, pool_scale)
    return jnp.concatenate([a_out, b_out], axis=-1) @ w_out


def mixer_cd(h, positions, w_in, q_norm_g, w_q_up, kv_norm_g, w_kv_up,
             conv_w, conv_b, wa, ba, wx, bx, lam, w_out):
    z = h @ w_in
    sizes = [Q_LORA, KV_LORA, QK_ROPE, LRU_WIDTH]
    cuts = [int(c) for c in np.cumsum(sizes)]
    c_q, c_kv, k_rope, x_lru, y_gate = jnp.split(z, cuts, axis=-1)
    c_out = mla_attention(c_q, c_kv, k_rope, positions, q_norm_g, w_q_up, kv_norm_g, w_kv_up)
    d_out = rg_lru_branch(x_lru, y_gate, conv_w, conv_b, wa, ba, wx, bx, lam)
    return jnp.concatenate([c_out, d_out], axis=-1) @ w_out


def channel_mixer(h, up, conv_w, conv_b, down):
    hid = causal_dwconv(h @ up, conv_w, conv_b)
    gate, val = jnp.split(hid, 2, axis=-1)
    return (jax.nn.gelu(gate, approximate=True) * val) @ down


def setup_inputs(seed: int = 0) -> dict:
    key = jax.random.key(seed)
    keys = iter(jax.random.split(key, 40))

    def dense(shape, fan_in):
        return jax.random.normal(next(keys), shape, jnp.float32) * (fan_in ** -0.5)

    def gain(shape):
        return 1.0 + 0.05 * jax.random.normal(next(keys), shape, jnp.float32)

    def small(shape, s=0.02):
        return s * jax.random.normal(next(keys), shape, jnp.float32)

    x = jax.random.normal(next(keys), (BATCH, SEQ, D_MODEL), jnp.float32)
    offset = jax.random.randint(next(keys), (BATCH, 1), 0, 64) * CHUNK
    positions = (offset + jnp.arange(SEQ)[None, :]).astype(jnp.int32)
    u = jax.random.uniform(next(keys), (N_ODD, LRU_WIDTH), jnp.float32, 0.9, 0.999)
    s = u ** (1.0 / LRU_C)
    lru_lambda = jnp.log(s) - jnp.log1p(-s)
    bw = LRU_WIDTH // LRU_BLOCKS
    return {
        'x': x,
        'positions': positions,
        'mix_pre_g': gain((DEPTH, D_MODEL)),
        'mix_post_g': gain((DEPTH, D_MODEL)),
        'ffn_pre_g': gain((DEPTH, D_MODEL)),
        'ffn_post_g': gain((DEPTH, D_MODEL)),
        'ffn_up': dense((DEPTH, D_MODEL, 2 * D_FF), D_MODEL),
        'ffn_conv_w': dense((DEPTH, FFN_CONV, 2 * D_FF), FFN_CONV),
        'ffn_conv_b': small((DEPTH, 2 * D_FF)),
        'ffn_down': dense((DEPTH, D_FF, D_MODEL), D_FF),
        'ab_w_in': dense((N_EVEN, D_MODEL, AB_IN), D_MODEL),
        'pool_w': dense((N_EVEN, len(POOL_WINDOWS), POOL_GROUP, POOL_GROUP), POOL_GROUP),
        'pool_b': small((N_EVEN, len(POOL_WINDOWS), POOL_GROUP)),
        'pool_scale': 1.0 + 0.1 * jax.random.normal(next(keys), (N_EVEN, POOL_WIDTH), jnp.float32),
        'ab_w_out': dense((N_EVEN, AB_OUT, D_MODEL), AB_OUT),
        'cd_w_in': dense((N_ODD, D_MODEL, CD_IN), D_MODEL),
        'q_norm_g': gain((N_ODD, Q_LORA)),
        'w_q_up': dense((N_ODD, Q_LORA, C_HEADS * (QK_NOPE + QK_ROPE)), Q_LORA),
        'kv_norm_g': gain((N_ODD, KV_LORA)),
        'w_kv_up': dense((N_ODD, KV_LORA, C_HEADS * (QK_NOPE + V_HEAD)), KV_LORA),
        'lru_conv_w': dense((N_ODD, LRU_CONV, LRU_WIDTH), LRU_CONV),
        'lru_conv_b': small((N_ODD, LRU_WIDTH)),
        'lru_wa': dense((N_ODD, LRU_BLOCKS, bw, bw), bw),
        'lru_ba': small((N_ODD, LRU_WIDTH), 0.1),
        'lru_wx': dense((N_ODD, LRU_BLOCKS, bw, bw), bw),
        'lru_bx': small((N_ODD, LRU_WIDTH), 0.1),
        'lru_lambda': lru_lambda,
        'cd_w_out': dense((N_ODD, CD_OUT, D_MODEL), CD_OUT),
    }


def reference(x, positions, mix_pre_g, mix_post_g, ffn_pre_g, ffn_post_g,
              ffn_up, ffn_conv_w, ffn_conv_b, ffn_down,
              ab_w_in, pool_w, pool_b, pool_scale, ab_w_out,
              cd_w_in, q_norm_g, w_q_up, kv_norm_g, w_kv_up,
              lru_conv_w, lru_conv_b, lru_wa, lru_ba, lru_wx, lru_bx, lru_lambda, cd_w_out):
    for layer in range(DEPTH):
        j = layer // 2
        h = rms_norm(x, mix_pre_g[layer])
        if layer % 2 == 0:
            m = mixer_ab(h, ab_w_in[j], pool_w[j], pool_b[j], pool_scale[j], ab_w_out[j])
        else:
            m = mixer_cd(h, positions, cd_w_in[j], q_norm_g[j], w_q_up[j], kv_norm_g[j], w_kv_up[j],
                         lru_conv_w[j], lru_conv_b[j], lru_wa[j], lru_ba[j], lru_wx[j], lru_bx[j],
                         lru_lambda[j], cd_w_out[j])
        x = x + rms_norm(m, mix_post_g[layer])
        h = rms_norm(x, ffn_pre_g[layer])
        f = channel_mixer(h, ffn_up[layer], ffn_conv_w[layer], ffn_conv_b[layer], ffn_down[layer])
        x = x + rms_norm(f, ffn_post_g[layer])
    return x
```

```python
import numpy as np
import concourse.bass as bass
import concourse.mybir as mybir

F32 = mybir.dt.float32
BF16 = mybir.dt.bfloat16
I32 = mybir.dt.int32
ALU = mybir.AluOpType
AF = mybir.ActivationFunctionType
AX = mybir.AxisListType


class Buf:
    __slots__ = ("name", "t", "last_w", "readers")

    def __init__(self, name, t=None):
        self.name = name
        self.t = t
        self.last_w = None
        self.readers = []

    def __getitem__(self, idx):
        return self.t[idx]


class Prog:
    COMPUTE = ("tensor", "vector", "scalar", "gpsimd")
    DMAQ = ("sync", "gpsimd")

    def __init__(self, nc, n_chan=12):
        self.nc = nc
        self.stack = []
        self.ops = {e: [] for e in ("sync", "tensor", "vector", "scalar", "gpsimd")}
        self.cnt = {}
        self.sems = {}
        for e in self.COMPUTE:
            self.sems[e] = self._enter(nc.semaphore("s_" + e))
            self.cnt[e] = 0
        self.chans = []
        for i in range(n_chan):
            k = "ch%d" % i
            self.sems[k] = self._enter(nc.semaphore("s_" + k))
            self.cnt[k] = 0
            self.chans.append(k)
        self.chan_rr = 0
        self.waited = {e: {} for e in self.ops}
        self.nbuf = 0

    def _enter(self, cm):
        v = cm.__enter__()
        self.stack.append(cm)
        return v

    def sb(self, name, shape, dt):
        t = self._enter(self.nc.sbuf_tensor("sb_" + name, list(shape), dt))
        return Buf(name, t)

    def ps(self, name, shape, dt=F32):
        t = self._enter(self.nc.psum_tensor("ps_" + name, list(shape), dt))
        return Buf(name, t)

    def view(self, name):
        return Buf(name, None)

    def _deps(self, reads, writes):
        deps = {}
        def add(tok):
            if tok is None:
                return
            k, v = tok
            if deps.get(k, 0) < v:
                deps[k] = v
        for b in reads:
            add(b.last_w)
        for b in writes:
            add(b.last_w)
            for r in b.readers:
                add(r)
        return deps

    def _commit(self, tok, reads, writes):
        for b in reads:
            b.readers.append(tok)
            if len(b.readers) > 64:
                m = {}
                for k, v in b.readers:
                    if m.get(k, 0) < v:
                        m[k] = v
                b.readers = list(m.items())
        for b in writes:
            b.last_w = tok
            b.readers = []

    def _waits(self, eng, deps, same_engine_sync=True):
        w = []
        wd = self.waited[eng]
        for k, v in deps.items():
            if k == eng and (eng == "tensor" or not same_engine_sync):
                continue
            if wd.get(k, 0) >= v:
                continue
            wd[k] = v
            w.append((k, v))
        return w

    def op(self, eng, fn, reads=(), writes=(), sync_self=True):
        deps = self._deps(reads, writes)
        waits = self._waits(eng, deps, sync_self)
        self.cnt[eng] += 1
        tok = (eng, self.cnt[eng])
        self.ops[eng].append((waits, fn, (eng, 1)))
        self._commit(tok, reads, writes)
        return tok

    def dma(self, out_ap, in_ap, reads=(), writes=(), q="sync", **kw):
        deps = self._deps(reads, writes)
        ch = self.chans[self.chan_rr]
        self.chan_rr = (self.chan_rr + 1) % len(self.chans)
        if self.cnt[ch] > 0:
            v = 16 * self.cnt[ch]
            if deps.get(ch, 0) < v:
                deps[ch] = v
        waits = self._waits(q, deps)
        self.cnt[ch] += 1
        tok = (ch, 16 * self.cnt[ch])

        def fn(e, out_ap=out_ap, in_ap=in_ap, kw=kw):
            return e.dma_start(out=out_ap, in_=in_ap, **kw)
        self.ops[q].append((waits, fn, (ch, 16)))
        self._commit(tok, reads, writes)
        return tok

    def finish_wait(self, eng, bufs):
        deps = self._deps(bufs, ())
        waits = self._waits(eng, deps)
        self.ops[eng].append((waits, None, None))

    def build(self):
        nc = self.nc
        blk = self._enter(nc.Block())
        P = self

        def emit(ename):
            def body(e):
                for waits, fn, inc in P.ops[ename]:
                    for k, v in waits:
                        e.wait_ge(P.sems[k], v)
                    if fn is not None:
                        ins = fn(e)
                        ins.then_inc(P.sems[inc[0]], inc[1])
            return body

        blk.sync(emit("sync"))
        blk.tensor(emit("tensor"))
        blk.vector(emit("vector"))
        blk.scalar(emit("scalar"))
        blk.gpsimd(emit("gpsimd"))

    def close(self):
        while self.stack:
            cm = self.stack.pop()
            cm.__exit__(None, None, None)


EPS = 1e-6


def dram_in(nc, name, shape, dt):
    return nc.dram_tensor(name, list(shape), dt, kind="ExternalInput").ap()


def dram_out(nc, name, shape, dt):
    return nc.dram_tensor(name, list(shape), dt, kind="ExternalOutput").ap()


def wait_all_dma(P, eng="sync"):
    for ch in P.chans:
        if P.cnt[ch]:
            P.ops[eng].append(([(ch, 16 * P.cnt[ch])], None, None))


def emit_rmsnorm_T(P, xs, g_sb, ones_bf, sq, ss_ps, rstd, hT, hoff, N, D, nch, x_reads, tag=""):
    P.op("scalar", lambda e: e.activation(out=sq[:, 0:nch, 0:N], in_=xs[:, 0:nch, 0:N], func=AF.Square), [xs], [sq])

    def mm(e):
        ins = None
        for c in range(nch):
            ins = e.matmul(ss_ps[:, 0:N], lhsT=ones_bf[:], rhs=sq[:, c, 0:N], start=(c == 0), stop=(c == nch - 1))
        return ins
    P.op("tensor", mm, [sq, ones_bf], [ss_ps])
    P.op("vector", lambda e: e.tensor_scalar(rstd[:, 0:N], ss_ps[:, 0:N], 1.0 / D, EPS, op0=ALU.mult, op1=ALU.add), [ss_ps], [rstd])
    P.op("scalar", lambda e: e.activation(out=rstd[:, 0:N], in_=rstd[:, 0:N], func=AF.Sqrt), [rstd], [rstd])
    P.op("vector", lambda e: e.reciprocal(rstd[:, 0:N], rstd[:, 0:N]), [rstd], [rstd])

    def sc(e):
        ins = None
        for c in range(nch):
            ins = e.scalar_tensor_tensor(out=hT[:, c, hoff:hoff + N], in0=xs[:, c, 0:N], scalar=g_sb[:, c:c + 1],
                                         in1=rstd[:, 0:N], op0=ALU.mult, op1=ALU.mult)
        return ins
    P.op("vector", sc, [xs, g_sb, rstd], [hT])


def build_KA(tiles, NOUT, outs, TOK=4096, TOKB=2048, D=2048, WG=256):
    nc = bass.Bass("TRN2", target_bir_lowering=False)
    P = Prog(nc)
    nch = D // 128
    x_d = dram_in(nc, "xT", [D, TOK], F32).rearrange("(c p) n -> p c n", p=128)
    g_d = dram_in(nc, "g", [128, nch], F32)
    w_d = dram_in(nc, "W", [D, NOUT], BF16).rearrange("(c p) n -> p c n", p=128)
    ones_d = dram_in(nc, "ones", [128, 128], F32)
    o_d = {k: dram_out(nc, k, [r, TOK], dt) for k, (r, dt) in outs.items()}
    odt = {k: dt for k, (r, dt) in outs.items()}

    NG = 512
    xs = P.sb("xs", [128, nch, NG], F32)
    sq = P.sb("sq", [128, nch, NG], BF16)
    hT = P.sb("hT", [128, nch, TOKB], BF16)
    g_sb = P.sb("g_sb", [128, nch], F32)
    ones_f = P.sb("ones_f", [128, 128], F32)
    ones_bf = P.sb("ones_bf", [128, 128], BF16)
    rstd = P.sb("rstd", [128, NG], F32)
    ss_ps = P.ps("ss_ps", [128, NG], F32)
    wbf = [P.sb("wbf%d" % i, [128, nch, WG], BF16) for i in range(3)]
    mm_ps = [P.ps("mm_ps%d" % i, [128, NG], F32) for i in range(3)]
    ost = {}
    for k, (r, dt) in outs.items():
        if dt not in ost:
            ost[dt] = [P.sb("ost_%s_%d" % (str(dt)[-4:], i), [128, TOKB], dt) for i in range(2)]
    ocnt = {dt: 0 for dt in ost}

    P.dma(g_sb[:], g_d, writes=[g_sb])
    P.dma(ones_f[:], ones_d, writes=[ones_f])
    P.op("vector", lambda e: e.tensor_copy(ones_bf[:], ones_f[:]), [ones_f], [ones_bf])

    groups = []
    cur = []
    for t in tiles:
        if cur and (t[0] + t[1] - cur[0][0] > WG or t[0] != cur[-1][0] + cur[-1][1]):
            groups.append(cur)
            cur = []
        cur.append(t)
    if cur:
        groups.append(cur)

    outv = []
    wi = 0
    pi = 0
    for tb in range(TOK // TOKB):
        t0 = tb * TOKB
        for gi in range(TOKB // NG):
            P.dma(xs[:], x_d[:, :, t0 + gi * NG: t0 + (gi + 1) * NG], writes=[xs])
            emit_rmsnorm_T(P, xs, g_sb, ones_bf, sq, ss_ps, rstd, hT, gi * NG, NG, D, nch, None)
        for grp in groups:
            c0 = grp[0][0]
            c1 = grp[-1][0] + grp[-1][1]
            wb_ = wbf[wi % 3]
            wi += 1
            P.dma(wb_[:, :, 0:c1 - c0], w_d[:, :, c0:c1], writes=[wb_])
            for (col0, ncols, oname, row0) in grp:
                dt = odt[oname]
                ob = ost[dt][ocnt[dt] % 2]
                ocnt[dt] += 1
                for gi in range(TOKB // NG):
                    ps = mm_ps[pi % 3]
                    pi += 1

                    def mm(e, ps=ps, wb_=wb_, off=col0 - c0, ncols=ncols, gi=gi):
                        ins = None
                        for c in range(nch):
                            ins = e.matmul(ps[0:ncols, :], lhsT=wb_[:, c, off:off + ncols], rhs=hT[:, c, gi * NG:(gi + 1) * NG],
                                           start=(c == 0), stop=(c == nch - 1))
                        return ins
                    P.op("tensor", mm, [wb_, hT], [ps])
                    if gi % 2 == 0:
                        P.op("scalar", lambda e, ps=ps, ob=ob, ncols=ncols, gi=gi: e.activation(out=ob[0:ncols, gi * NG:(gi + 1) * NG], in_=ps[0:ncols, :], func=AF.Copy), [ps], [ob])
                    else:
                        P.op("vector", lambda e, ps=ps, ob=ob, ncols=ncols, gi=gi: e.tensor_copy(ob[0:ncols, gi * NG:(gi + 1) * NG], ps[0:ncols, :]), [ps], [ob])
                dst = P.view("o")
                outv.append(dst)
                P.dma(o_d[oname][row0:row0 + ncols, t0:t0 + TOKB], ob[0:ncols, :], reads=[ob], writes=[dst])
    wait_all_dma(P)
    P.build()
    P.close()
    return nc


AB_TILES = ([(i * 128, 128, "qT", i * 128) for i in range(8)]
            + [(1024 + i * 128, 128, "kT", i * 128) for i in range(8)]
            + [(2048 + i * 128, 128, "vT", i * 128) for i in range(8)]
            + [(3072 + i * 128, 128, "qiT", i * 128) for i in range(8)]
            + [(4096, 64, "kiT", 0), (4160, 16, "wiT", 0)]
            + [(4176 + i * 128, 128, "uT", i * 128) for i in range(8)])
AB_OUTS = {"qT": (1024, BF16), "kT": (1024, BF16), "vT": (1024, BF16), "qiT": (1024, BF16),
           "kiT": (64, BF16), "wiT": (16, F32), "uT": (1024, F32)}

CD_TILES = ([(i * 128, 128, "cqT", i * 128) for i in range(4)]
            + [(512 + i * 128, 128, "ckvT", i * 128) for i in range(2)]
            + [(768, 64, "krT", 0)]
            + [(832 + i * 128, 128, "xlT", i * 128) for i in range(8)]
            + [(1856 + i * 128, 128, "ygT", i * 128) for i in range(8)])
CD_OUTS = {"cqT": (512, F32), "ckvT": (256, F32), "krT": (64, F32), "xlT": (1024, F32), "ygT": (1024, F32)}


D = 2048
NCH = 16
DFF = 4096


def emit_rstd(P, src, nch, N, ones_bf, sq, ss_ps, rstd, Dn):
    P.op("scalar", lambda e: e.activation(out=sq[:, 0:nch, 0:N], in_=src[:, 0:nch, 0:N], func=AF.Square), [src], [sq])

    def mm(e):
        ins = None
        for c in range(nch):
            ins = e.matmul(ss_ps[:, 0:N], lhsT=ones_bf[:], rhs=sq[:, c, 0:N], start=(c == 0), stop=(c == nch - 1))
        return ins
    P.op("tensor", mm, [sq, ones_bf], [ss_ps])
    P.op("vector", lambda e: e.tensor_scalar(rstd[:, 0:N], ss_ps[:, 0:N], 1.0 / Dn, EPS, op0=ALU.mult, op1=ALU.add), [ss_ps], [rstd])
    P.op("scalar", lambda e: e.activation(out=rstd[:, 0:N], in_=rstd[:, 0:N], func=AF.Sqrt), [rstd], [rstd])
    P.op("vector", lambda e: e.reciprocal(rstd[:, 0:N], rstd[:, 0:N]), [rstd], [rstd])


def emit_scale(P, eng, dst, src, g_sb, gcol0, rstd, nch, N):
    def sc(e):
        ins = None
        for c in range(nch):
            ins = e.scalar_tensor_tensor(out=dst[:, c, 0:N], in0=src[:, c, 0:N], scalar=g_sb[:, gcol0 + c:gcol0 + c + 1],
                                         in1=rstd[:, 0:N], op0=ALU.mult, op1=ALU.mult)
        return ins
    rd = [src, g_sb, rstd]
    P.op(eng, sc, rd, [dst])


def split_groups(TOK, maxn=510):
    ng = -(-TOK // maxn)
    base = -(-TOK // ng)
    base = -(-base // 8) * 8
    sizes = []
    rem = TOK
    while rem > 0:
        n = min(base, rem)
        sizes.append(n)
        rem -= n
    return sizes


def build_KE(TOK=4096, stop_after=None):
    nc = bass.Bass("TRN2", target_bir_lowering=False)
    P = Prog(nc)
    r3 = lambda ap: ap.rearrange("(c p) n -> p c n", p=128)
    x_d = r3(dram_in(nc, "xT", [D, TOK], F32))
    cat_d = r3(dram_in(nc, "catT", [D, TOK], BF16))
    xh_d = r3(dram_in(nc, "xhT", [D, 2], F32))
    cath_d = r3(dram_in(nc, "cathT", [D, 2], BF16))
    gs_d = dram_in(nc, "gs", [128, 3 * NCH], F32)
    wout_d = r3(dram_in(nc, "w_out", [D, D], BF16))
    up_d = r3(dram_in(nc, "up", [D, 2 * DFF], BF16))
    down_d = r3(dram_in(nc, "down", [DFF, D], BF16))
    cw_d = dram_in(nc, "cw", [128, 3 * 64], F32)
    cb_d = dram_in(nc, "cb", [128, 64], F32)
    ones_d = dram_in(nc, "ones", [128, 128], F32)
    y_d = r3(dram_out(nc, "yT", [D, TOK], F32))

    NG = 512
    A = P.sb("A", [128, NCH, NG], F32)
    B = P.sb("B", [128, NCH, NG], F32)
    cb16 = P.sb("cb16", [128, NCH, NG], BF16)
    hb = P.sb("hb", [128, NCH, NG], BF16)
    act = P.sb("act", [128, 32, NG], BF16)
    NWB = 4
    wb = [P.sb("wb%d" % i, [128, NCH, 256], BF16) for i in range(NWB)]
    acc = [[P.sb("acc%d%d" % (i, j), [128, NG], F32) for j in range(2)] for i in range(2)]
    rstd = P.sb("rstd", [128, NG], F32)
    gs = P.sb("gs", [128, 3 * NCH], F32)
    cw = P.sb("cw", [128, 3 * 64], F32)
    cb = P.sb("cb", [128, 64], F32)
    ones_f = P.sb("ones_f", [128, 128], F32)
    ones_bf = P.sb("ones_bf", [128, 128], BF16)
    ss_ps = P.ps("ss_ps", [128, NG], F32)
    NPS = 5
    mm_ps = [P.ps("mm_ps%d" % i, [128, NG], F32) for i in range(NPS)]
    st = {"wi": 0, "pi": 0, "ti": 0}

    P.dma(gs[:], gs_d, writes=[gs])
    P.dma(cw[:], cw_d, writes=[cw])
    P.dma(cb[:], cb_d, writes=[cb])
    P.dma(ones_f[:], ones_d, writes=[ones_f])
    P.op("vector", lambda e: e.tensor_copy(ones_bf[:], ones_f[:]), [ones_f], [ones_bf])

    def next_wb():
        b = wb[st["wi"] % NWB]
        st["wi"] += 1
        return b

    def next_ps():
        b = mm_ps[st["pi"] % NPS]
        st["pi"] += 1
        return b

    def load_w(src_ap, ncols=256):
        b = next_wb()
        P.dma(b[:, :, 0:ncols], src_ap, writes=[b])
        return b

    def matmul_group(ps, M, N, parts, roff=0):
        def mm(e):
            ins = None
            tot = sum(len(p[3]) for p in parts)
            i = 0
            for (w, off, rb, chunks) in parts:
                for (wc, rc) in chunks:
                    ins = e.matmul(ps[0:M, 0:N], lhsT=w[:, wc, off:off + M], rhs=rb[:, rc, roff:roff + N], start=(i == 0), stop=(i == tot - 1))
                    i += 1
            return ins
        rd = []
        for p in parts:
            rd += [p[0], p[2]]
        P.op("tensor", mm, rd, [ps])

    def evac(ps, dst_ap_fn, dstbuf, k):
        if k % 2 == 0:
            P.op("scalar", lambda e: e.activation(out=dst_ap_fn(), in_=ps[:, 0:ps_n[0]], func=AF.Copy), [ps], [dstbuf])
        else:
            P.op("vector", lambda e: e.tensor_copy(dst_ap_fn(), ps[:, 0:ps_n[0]]), [ps], [dstbuf])
    ps_n = [0]

    def group(n0, N, halo):
        xsrc = xh_d if halo else x_d[:, :, n0:n0 + N]
        csrc = cath_d if halo else cat_d[:, :, n0:n0 + N]
        ho = 0 if halo else 2
        if not halo and n0 > 0:
            pN = prev_n[0]
            P.op("vector", lambda e: e.tensor_copy(rstd[:, 0:32].bitcast(BF16)[:, 0:32].rearrange("p (c n) -> p c n", n=2), hb[:, :, pN:pN + 2]), [hb], [rstd])
            P.op("vector", lambda e: e.tensor_copy(hb[:, :, 0:2], rstd[:, 0:32].bitcast(BF16)[:, 0:32].rearrange("p (c n) -> p c n", n=2)), [rstd], [hb])
        P.dma(cb16[:, :, 0:N], csrc, writes=[cb16])
        P.dma(A[:, :, 0:N], xsrc, writes=[A])
        for mp in range(8):
            w = load_w(wout_d[:, :, mp * 256:(mp + 1) * 256])
            for j in range(2):
                m = mp * 2 + j
                ps = next_ps()
                matmul_group(ps, 128, N, [(w, j * 128, cb16, [(c, c) for c in range(NCH)])])
                if m % 2 == 0:
                    P.op("scalar", lambda e, ps=ps, m=m: e.activation(out=B[:, m, 0:N], in_=ps[:, 0:N], func=AF.Copy), [ps], [B])
                else:
                    P.op("vector", lambda e, ps=ps, m=m: e.tensor_copy(B[:, m, 0:N], ps[:, 0:N]), [ps], [B])
        emit_rstd(P, B, NCH, N, ones_bf, act, ss_ps, rstd, D)
        emit_scale(P, "vector", B, B, gs, 0, rstd, NCH, N)
        P.op("gpsimd", lambda e: e.tensor_tensor(out=A[:, :, 0:N], in0=A[:, :, 0:N], in1=B[:, :, 0:N], op=ALU.add), [A, B], [A])
        if stop_after == "epi":
            if not halo:
                P.dma(y_d[:, :, n0:n0 + N], A[:, :, 0:N], reads=[A])
            return
        emit_rstd(P, A, NCH, N, ones_bf, act, ss_ps, rstd, D)

        def sc(e):
            ins = None
            for c in range(NCH):
                ins = e.scalar_tensor_tensor(out=hb[:, c, ho:ho + N], in0=A[:, c, 0:N], scalar=gs[:, NCH + c:NCH + c + 1],
                                             in1=rstd[:, 0:N], op0=ALU.mult, op1=ALU.mult)
            return ins
        P.op("vector", sc, [A, gs, rstd], [hb])
        if halo:
            return
        prev_n[0] = N
        NP = N + 2
        for pg in range(16):
            wg = load_w(up_d[:, :, pg * 256:(pg + 1) * 256])
            wv = load_w(up_d[:, :, DFF + pg * 256:DFF + (pg + 1) * 256])
            for j in range(2):
                m = pg * 2 + j
                psg = next_ps()
                psv = next_ps()
                matmul_group(psg, 128, NP, [(wg, j * 128, hb, [(c, c) for c in range(NCH)])])
                matmul_group(psv, 128, NP, [(wv, j * 128, hb, [(c, c) for c in range(NCH)])])
                ag, av = acc[st["ti"] % 2]
                st["ti"] += 1

                def conv(ps_, a, mm_):
                    P.op("scalar", lambda e: e.activation(out=a[:, 0:N], in_=ps_[:, 2:2 + N], func=AF.Identity, scale=cw[:, 128 + mm_:128 + mm_ + 1], bias=cb[:, mm_:mm_ + 1]), [ps_, cw, cb], [a])
                    P.op("vector", lambda e: e.scalar_tensor_tensor(out=a[:, 0:N], in0=ps_[:, 1:1 + N], scalar=cw[:, 64 + mm_:64 + mm_ + 1], in1=a[:, 0:N], op0=ALU.mult, op1=ALU.add), [ps_, cw, a], [a])
                    P.op("vector", lambda e: e.scalar_tensor_tensor(out=a[:, 0:N], in0=ps_[:, 0:N], scalar=cw[:, mm_:mm_ + 1], in1=a[:, 0:N], op0=ALU.mult, op1=ALU.add), [ps_, cw, a], [a])
                conv(psg, ag, m)
                conv(psv, av, 32 + m)
                P.op("scalar", lambda e, ag=ag: e.activation(out=ag[:, 0:N], in_=ag[:, 0:N], func=AF.Gelu_apprx_tanh), [ag], [ag])
                P.op("gpsimd", lambda e, ag=ag, av=av, m=m: e.tensor_tensor(out=act[:, m, 0:N], in0=ag[:, 0:N], in1=av[:, 0:N], op=ALU.mult), [ag, av], [act])
        if stop_after == "up":
            P.op("vector", lambda e: e.tensor_copy(A[:, :, 0:N], act[:, 0:16, 0:N]), [act], [A])
            P.dma(y_d[:, :, n0:n0 + N], A[:, :, 0:N], reads=[A])
            return
        for mp in range(8):
            w0 = load_w(down_d[:, 0:16, mp * 256:(mp + 1) * 256])
            w1 = load_w(down_d[:, 16:32, mp * 256:(mp + 1) * 256])
            for j in range(2):
                m = mp * 2 + j
                ps = next_ps()
                matmul_group(ps, 128, N, [(w0, j * 128, act, [(c, c) for c in range(16)]),
                                          (w1, j * 128, act, [(c, 16 + c) for c in range(16)])])
                if m % 2 == 0:
                    P.op("scalar", lambda e, ps=ps, m=m: e.activation(out=B[:, m, 0:N], in_=ps[:, 0:N], func=AF.Copy), [ps], [B])
                else:
                    P.op("vector", lambda e, ps=ps, m=m: e.tensor_copy(B[:, m, 0:N], ps[:, 0:N]), [ps], [B])
        emit_rstd(P, B, NCH, N, ones_bf, act, ss_ps, rstd, D)
        emit_scale(P, "vector", B, B, gs, 2 * NCH, rstd, NCH, N)
        P.op("gpsimd", lambda e: e.tensor_tensor(out=A[:, :, 0:N], in0=A[:, :, 0:N], in1=B[:, :, 0:N], op=ALU.add), [A, B], [A])
        P.dma(y_d[:, :, n0:n0 + N], A[:, :, 0:N], reads=[A])

    prev_n = [0]
    group(0, 2, True)
    n0 = 0
    for N in split_groups(TOK):
        group(n0, N, False)
        n0 += N
    wait_all_dma(P)
    P.build()
    P.close()
    return nc


WINS = (2, 4, 8, 16)


def build_KC(TOK=4096, NG=512):
    nc = bass.Bass("TRN2", target_bir_lowering=False)
    P = Prog(nc)
    u_d = dram_in(nc, "uT", [1024, TOK], F32).rearrange("(c p) n -> p c n", p=128)
    uh_d = dram_in(nc, "uhT", [1024, 16], F32).rearrange("(c p) n -> p c n", p=128)
    rc_d = dram_in(nc, "rc", [128, 4 * 2 * 16], F32)
    pw_d = dram_in(nc, "pw", [4, 256, 256], BF16).rearrange("g (c p) d -> p g c d", p=128)
    psb_d = dram_in(nc, "psb", [128, 16], F32)
    o_d = dram_out(nc, "boutT", [1024, TOK], BF16).rearrange("(c p) n -> p c n", p=128)
    W = 16 + NG
    U = [P.sb("U%d" % i, [128, 8, W], F32) for i in range(2)]
    S1 = P.sb("S1", [128, 2, W], F32)
    S2 = P.sb("S2", [128, 2, W], F32)
    PB = [P.sb("PB%d" % i, [128, 8, NG], BF16) for i in range(2)]
    OB = [P.sb("OB%d" % i, [128, 8, NG], BF16) for i in range(2)]
    rc = P.sb("rc", [128, 4, 2, 16], F32)
    tmp = P.sb("tmp", [128, 2, 16], F32)
    pw = P.sb("pw", [128, 4, 2, 256], BF16)
    psb = P.sb("psb", [128, 16], F32)
    bs = P.sb("bs", [128, 8], F32)
    pss = [P.ps("pss%d" % i, [128, NG], F32) for i in range(4)]
    P.dma(rc[:].rearrange("p a b c -> p (a b c)"), rc_d, writes=[rc])
    P.dma(psb[:], psb_d, writes=[psb])
    for g in range(4):
        P.dma(pw[:, g, :, :], pw_d[:, g, :, :], writes=[pw])
    P.op("vector", lambda e: e.tensor_tensor(out=bs[:], in0=psb[:, 0:8], in1=psb[:, 8:16], op=ALU.mult), [psb], [bs])
    pi = 0
    for gi in range(TOK // NG):
        n0 = gi * NG
        Ub, PBb, OBb = U[gi % 2], PB[gi % 2], OB[gi % 2]
        if gi == 0:
            P.dma(Ub[:, :, 0:16], uh_d, writes=[Ub])
            P.dma(Ub[:, :, 16:W], u_d[:, :, 0:NG], writes=[Ub])
        else:
            P.dma(Ub[:, :, 0:W], u_d[:, :, n0 - 16:n0 + NG], writes=[Ub])
        for g in range(4):
            ks = slice(2 * g, 2 * g + 2)
            P.op("gpsimd", lambda e, Ub=Ub, ks=ks: e.tensor_tensor(out=S1[:, :, 1:W], in0=Ub[:, ks, 1:W], in1=Ub[:, ks, 0:W - 1], op=ALU.add), [Ub], [S1])
            fin = S1
            if g >= 1:
                P.op("gpsimd", lambda e: e.tensor_tensor(out=S2[:, :, 3:W], in0=S1[:, :, 3:W], in1=S1[:, :, 1:W - 2], op=ALU.add), [S1], [S2])
                fin = S2
            if g >= 2:
                P.op("gpsimd", lambda e: e.tensor_tensor(out=S1[:, :, 7:W], in0=S2[:, :, 7:W], in1=S2[:, :, 3:W - 4], op=ALU.add), [S2], [S1])
                fin = S1
            if g >= 3:
                P.op("gpsimd", lambda e: e.tensor_tensor(out=S2[:, :, 15:W], in0=S1[:, :, 15:W], in1=S1[:, :, 7:W - 8], op=ALU.add), [S1], [S2])
                fin = S2
            P.op("vector", lambda e, fin=fin, Ub=Ub, PBb=PBb, ks=ks, g=g: e.scalar_tensor_tensor(
                out=PBb[:, ks, 0:NG], in0=fin[:, :, 16:W], scalar=1.0 / WINS[g], in1=Ub[:, ks, 16:W], op0=ALU.mult, op1=ALU.subtract), [fin, Ub], [PBb])
            if gi == 0:
                P.op("vector", lambda e, fin=fin, g=g: e.tensor_tensor(out=tmp[:], in0=fin[:, :, 16:32], in1=rc[:, g, :, :], op=ALU.mult), [fin, rc], [tmp])
                P.op("vector", lambda e, Ub=Ub, PBb=PBb, ks=ks: e.tensor_tensor(out=PBb[:, ks, 0:16], in0=tmp[:], in1=Ub[:, ks, 16:32], op=ALU.subtract), [tmp, Ub, PBb], [PBb])
            for dt in range(2):
                ps = pss[pi % 4]
                pi += 1

                def mm(e, ps=ps, g=g, dt=dt, PBb=PBb):
                    e.matmul(ps[:, 0:NG], lhsT=pw[:, g, 0, dt * 128:(dt + 1) * 128], rhs=PBb[:, 2 * g, 0:NG], start=True, stop=False)
                    return e.matmul(ps[:, 0:NG], lhsT=pw[:, g, 1, dt * 128:(dt + 1) * 128], rhs=PBb[:, 2 * g + 1, 0:NG], start=False, stop=True)
                P.op("tensor", mm, [pw, PBb], [ps])
                k = 2 * g + dt
                P.op("scalar", lambda e, ps=ps, k=k, OBb=OBb: e.activation(out=OBb[:, k, 0:NG], in_=ps[:, 0:NG], func=AF.Identity, scale=psb[:, k:k + 1], bias=bs[:, k:k + 1]), [ps, psb, bs], [OBb])
        P.dma(o_d[:, :, n0:n0 + NG], OBb[:, :, 0:NG], reads=[OBb])
    wait_all_dma(P)
    P.build()
    P.close()
    return nc


TOPK = 256
NBIS = 26


def build_KB(T=16384, NSLOT=32):
    nc = bass.Bass("TRN2", target_bir_lowering=False)
    P = Prog(nc)
    NQ = NSLOT * 128
    q_d = dram_in(nc, "qT", [1024, NQ], BF16).rearrange("(h d) n -> d h n", d=128)
    qi_d = dram_in(nc, "qiT", [1024, NQ], BF16).rearrange("(h d) n -> d h n", d=64)
    wi_d = dram_in(nc, "wi", [NQ, 16], F32)
    ki_d = dram_in(nc, "kiT", [64, T], BF16)
    k_d = dram_in(nc, "kT", [1024, T], BF16).rearrange("(h d) n -> d h n", d=128)
    v_d = dram_in(nc, "v", [T, 1024], BF16).rearrange("(t s) n -> s t n", s=128)
    nm_d = dram_in(nc, "negmask", [128, 512], F32)
    id_d = dram_in(nc, "ident", [128, 128], F32)
    o_d = dram_out(nc, "aoutT", [1024, NQ], BF16).rearrange("(h d) n -> d h n", d=128)

    LMAX = 512 * NSLOT
    KI = P.sb("KI", [64, T], BF16)
    score = P.sb("score", [128, LMAX], F32)
    Mq = P.sb("Mq", [128, LMAX], BF16)
    MTc = [P.sb("MTc%d" % i, [128, 4, 128], BF16) for i in range(2)]
    Q = P.sb("Q", [128, 8, 128], BF16)
    QI = P.sb("QI", [64, 16, 128], BF16)
    W = P.sb("W", [128, 16], F32)
    DG = P.sb("DG", [128, 16, 128], BF16)
    negm = P.sb("negm", [128, 512], F32)
    posm = P.sb("posm", [128, 512], F32)
    idf = P.sb("idf", [128, 128], F32)
    ident = P.sb("ident", [128, 128], BF16)
    ones_bf = P.sb("ones_bf", [128, 128], BF16)
    R = [P.sb("R%d" % i, [128, 512], BF16) for i in range(3)]
    tmpm = P.sb("tmpm", [128, 512], F32)
    st8 = P.sb("st8", [128, 8], F32)
    KTc = [P.sb("KTc%d" % i, [128, 8, 512], BF16) for i in range(2)]
    Vc = [P.sb("Vc%d" % i, [128, 4, 1024], BF16) for i in range(2)]
    PT = [P.sb("PT%d" % i, [128, 8, 128], BF16) for i in range(2)]
    rs = P.sb("rs", [128, 8, 128], F32)
    ob = [P.sb("ob%d" % i, [128, 8, 128], BF16) for i in range(2)]
    psA = [P.ps("psA%d" % i, [128, 512], F32) for i in range(2)]
    psB = [P.ps("psB%d" % i, [128, 512], F32) for i in range(2)]
    OT = [P.ps("OT%d" % i, [128, 4, 128], F32) for i in range(2)]
    SUM = [P.ps("SUM%d" % i, [128, 4, 128], F32) for i in range(2)]
    att_scale = 128.0 ** -0.5

    P.dma(KI[:], ki_d, writes=[KI])
    P.dma(negm[:], nm_d, writes=[negm])
    P.dma(idf[:], id_d, writes=[idf])
    P.op("vector", lambda e: e.tensor_copy(ident[:], idf[:]), [idf], [ident])
    P.op("vector", lambda e: e.memset(ones_bf[:], 1.0), [], [ones_bf])
    P.op("vector", lambda e: e.tensor_scalar(posm[:], negm[:], -1.0, None, op0=ALU.mult), [negm], [posm])
    c = {"a": 0, "b": 0, "r": 0, "kv": 0, "pt": 0, "ob": 0, "ev": 0}

    for m in range(NSLOT):
        L = 512 * (m + 1)
        ntile = 4 * (m + 1)
        qs = slice(m * 128, (m + 1) * 128)
        P.dma(Q[:], q_d[:, :, qs], writes=[Q])
        P.dma(QI[:], qi_d[:, :, qs], writes=[QI])
        P.dma(W[:], wi_d[qs, :], writes=[W])

        def mkdg(e):
            ins = None
            for h in range(16):
                ins = e.tensor_scalar(DG[:, h, :], ident[:], W[:, h:h + 1], None, op0=ALU.mult)
            return ins
        P.op("vector", mkdg, [ident, W], [DG])
        for ch in range(m + 1):
            sc_ps = psB[c["b"] % 2]
            c["b"] += 1
            for h in range(16):
                lg = psA[c["a"] % 2]
                c["a"] += 1
                r = R[c["r"] % 3]
                c["r"] += 1
                P.op("tensor", lambda e, lg=lg, h=h, ch=ch: e.matmul(lg[:, :], lhsT=QI[:, h, :], rhs=KI[:, ch * 512:(ch + 1) * 512], start=True, stop=True), [QI, KI], [lg])
                if h % 2 == 0:
                    P.op("scalar", lambda e, lg=lg, r=r: e.activation(out=r[:], in_=lg[:, :], func=AF.Relu), [lg], [r])
                else:
                    P.op("vector", lambda e, lg=lg, r=r: e.tensor_scalar(r[:], lg[:, :], 0.0, None, op0=ALU.max), [lg], [r])
                P.op("tensor", lambda e, sc_ps=sc_ps, h=h, r=r: e.matmul(sc_ps[:, :], lhsT=DG[:, h, :], rhs=r[:], start=(h == 0), stop=(h == 15)), [DG, r], [sc_ps])
            if ch < m:
                P.op("scalar", lambda e, sc_ps=sc_ps, ch=ch: e.activation(out=score[:, ch * 512:(ch + 1) * 512], in_=sc_ps[:, :], func=AF.Copy), [sc_ps], [score])
            else:
                P.op("vector", lambda e, sc_ps=sc_ps: e.tensor_tensor(out=tmpm[:], in0=sc_ps[:, :], in1=posm[:], op=ALU.add), [sc_ps, posm], [tmpm])
                P.op("vector", lambda e, sc_ps=sc_ps, ch=ch: e.tensor_tensor(out=score[:, ch * 512:(ch + 1) * 512], in0=sc_ps[:, :], in1=negm[:], op=ALU.add), [sc_ps, negm], [score])
        P.op("vector", lambda e, L=L: e.tensor_reduce(out=st8[:, 0:1], in_=score[:, 0:L], axis=AX.X, op=ALU.max), [score], [st8])
        P.op("vector", lambda e: e.tensor_reduce(out=st8[:, 1:2], in_=tmpm[:], axis=AX.X, op=ALU.min), [tmpm], [st8])
        if m > 0:
            P.op("vector", lambda e, L=L: e.tensor_reduce(out=st8[:, 2:3], in_=score[:, 0:L - 512], axis=AX.X, op=ALU.min), [score], [st8])
            P.op("vector", lambda e: e.tensor_tensor(out=st8[:, 3:4], in0=st8[:, 1:2], in1=st8[:, 2:3], op=ALU.min), [st8], [st8])
        else:
            P.op("vector", lambda e: e.tensor_copy(st8[:, 3:4], st8[:, 1:2]), [st8], [st8])
        P.op("vector", lambda e: e.tensor_tensor(out=st8[:, 4:5], in0=st8[:, 0:1], in1=st8[:, 3:4], op=ALU.subtract), [st8], [st8])
        for it in range(NBIS):
            P.op("vector", lambda e: e.tensor_scalar(st8[:, 4:5], st8[:, 4:5], 0.5, None, op0=ALU.mult), [st8], [st8])
            P.op("vector", lambda e: e.tensor_tensor(out=st8[:, 5:6], in0=st8[:, 3:4], in1=st8[:, 4:5], op=ALU.add), [st8], [st8])
            P.op("vector", lambda e, L=L: e.tensor_scalar(Mq[:, 0:L], score[:, 0:L], st8[:, 5:6], 0.0, op0=ALU.is_ge, op1=ALU.add, accum_out=st8[:, 6:7]), [score, st8], [Mq, st8])
            P.op("vector", lambda e: e.scalar_tensor_tensor(out=st8[:, 7:8], in0=st8[:, 6:7], scalar=float(TOPK), in1=st8[:, 4:5], op0=ALU.is_ge, op1=ALU.mult), [st8], [st8])
            P.op("vector", lambda e: e.tensor_tensor(out=st8[:, 3:4], in0=st8[:, 3:4], in1=st8[:, 7:8], op=ALU.add), [st8], [st8])
        P.op("vector", lambda e, L=L: e.tensor_scalar(Mq[:, 0:L], score[:, 0:L], st8[:, 3:4], None, op0=ALU.is_ge), [score, st8], [Mq])
        for ch in range(m + 1):
            kt, vt = KTc[c["kv"] % 2], Vc[c["kv"] % 2]
            c["kv"] += 1
            P.dma(kt[:], k_d[:, :, ch * 512:(ch + 1) * 512], writes=[kt])
            P.dma(vt[:], v_d[:, ch * 4:ch * 4 + 4, :], writes=[vt])
            MT = MTc[c["kv"] % 2]
            pb = psB[c["b"] % 2]
            c["b"] += 1
            psT = pb[:, :].bitcast(BF16)

            def tr(e, ch=ch, psT=psT):
                ins = None
                for k in range(4):
                    t_ = ch * 4 + k
                    ins = e.transpose(psT[:, k * 128:(k + 1) * 128], Mq[:, t_ * 128:(t_ + 1) * 128], ident[:])
                return ins
            P.op("tensor", tr, [Mq, ident], [pb])
            if ch % 2 == 0:
                P.op("scalar", lambda e, MT=MT, psT=psT: e.activation(out=MT[:], in_=psT[:, 0:512].rearrange("p (a b) -> p a b", b=128), func=AF.Copy), [pb], [MT])
            else:
                P.op("vector", lambda e, MT=MT, psT=psT: e.tensor_copy(MT[:], psT[:, 0:512].rearrange("p (a b) -> p a b", b=128)), [pb], [MT])
            for tt in range(4):
                t = ch * 4 + tt
                pt = PT[c["pt"] % 2]
                c["pt"] += 1
                for hg in range(2):
                    st = psA[c["a"] % 2]
                    c["a"] += 1

                    def mmqk(e, st=st, hg=hg, kt=kt, tt=tt):
                        ins = None
                        for hh in range(4):
                            h = hg * 4 + hh
                            ins = e.matmul(st[:, hh * 128:(hh + 1) * 128], lhsT=kt[:, h, tt * 128:(tt + 1) * 128], rhs=Q[:, h, :], start=True, stop=True)
                        return ins
                    P.op("tensor", mmqk, [kt, Q], [st])
                    P.op("scalar", lambda e, st=st, pt=pt, hg=hg: e.activation(out=pt[:, hg * 4:hg * 4 + 4, :], in_=st[:, :].rearrange("p (a b) -> p a b", b=128), func=AF.Exp, scale=att_scale), [st], [pt])
                P.op("gpsimd", lambda e, pt=pt, tt=tt, MT=MT: e.tensor_tensor(out=pt[:], in0=pt[:], in1=MT[:, tt:tt + 1, :].to_broadcast([128, 8, 128]), op=ALU.mult), [pt, MT], [pt])

                def mmpv(e, pt=pt, vt=vt, tt=tt, t=t, ntile=ntile):
                    ins = None
                    for h in range(8):
                        ins = e.matmul(OT[h // 4][:, h % 4, :], lhsT=vt[:, tt, h * 128:(h + 1) * 128], rhs=pt[:, h, :], start=(t == 0 and h % 4 == 0), stop=(t == ntile - 1 and h % 4 == 3))
                    for hg in range(2):
                        ins = e.matmul(SUM[hg][:].rearrange("p a b -> p (a b)"), lhsT=ones_bf[:], rhs=pt[:, hg * 4:hg * 4 + 4, :].rearrange("p a b -> p (a b)"),
                                       start=(t == 0), stop=(t == ntile - 1))
                    return ins
                P.op("tensor", mmpv, [vt, pt, ones_bf], [OT[0], OT[1], SUM[0], SUM[1]])
        o = ob[c["ob"] % 2]
        c["ob"] += 1
        for hg in range(2):
            P.op("vector", lambda e, hg=hg: e.reciprocal(rs[:, hg * 4:hg * 4 + 4, :], SUM[hg][:]), [SUM[hg]], [rs])
            P.op("vector", lambda e, hg=hg, o=o: e.tensor_tensor(out=o[:, hg * 4:hg * 4 + 4, :], in0=OT[hg][:], in1=rs[:, hg * 4:hg * 4 + 4, :], op=ALU.mult), [OT[hg], rs], [o])
        P.dma(o_d[:, :, qs], o[:], reads=[o])
    wait_all_dma(P)
    P.build()
    P.close()
    return nc


import math

PI = math.pi


def emit_mla(P, nc, T, NB, ones_bf, pj):
    TT = T * NB
    NG = 512
    r3 = lambda ap: ap.rearrange("(c p) n -> p c n", p=128)
    cq_d = r3(dram_in(nc, "cqT", [512, TT], F32))
    ckv_d = r3(dram_in(nc, "ckvT", [256, TT], F32))
    kr_d = dram_in(nc, "krT", [64, TT], F32)
    krs_d = dram_in(nc, "krsT", [64, TT], F32)
    pos_d = dram_in(nc, "posrep", [64, TT], I32)
    fq_d = dram_in(nc, "fq", [64, 2], F32)
    wq_d = r3(dram_in(nc, "wq", [512, 256], BF16))
    wkv_d = r3(dram_in(nc, "wkv", [256, 256], BF16))
    g_d = dram_in(nc, "mlag", [128, 6], F32)
    mask_d = dram_in(nc, "mask", [128, 4 * NG], BF16)
    co_d = dram_out(nc, "coutT", [128, TT], BF16)

    KT = P.sb("KT", [128, T], BF16)
    KRT = P.sb("KRT", [64, T], BF16)
    V = P.sb("V", [128, T // 128, 128], BF16)
    mask = P.sb("mask", [128, 4, NG], BF16)
    wq = P.sb("wq", [128, 4, 256], BF16)
    wkv = P.sb("wkv", [128, 2, 256], BF16)
    gg = P.sb("mlag", [128, 6], F32)
    fq = P.sb("fq", [64, 2], F32)
    ckv = P.sb("ckv", [128, 2, NG], F32)
    cq = P.sb("cq", [128, 4, NG], F32)
    sq = P.sb("sq", [128, 4, NG], BF16)
    cqn = P.sb("cqn", [128, 4, NG], BF16)
    ckvn = P.sb("ckvn", [128, 2, NG], BF16)
    rstd = P.sb("rstd", [128, NG], F32)
    QN = P.sb("QN", [128, NG], BF16)
    QR = P.sb("QR", [64, NG], BF16)
    posi = P.sb("posi", [64, NG], I32)
    ang = P.sb("ang", [64, NG], F32)
    cos = P.sb("cos", [64, NG], F32)
    sin = P.sb("sin", [64, NG], F32)
    kr = P.sb("kr", [64, NG], F32)
    krs = P.sb("krs", [64, NG], F32)
    t1 = P.sb("t1", [64, NG], F32)
    t2 = P.sb("t2", [64, NG], F32)
    PT = [P.sb("PT%d" % i, [128, NG], BF16) for i in range(3)]
    rs = P.sb("rs", [128, NG], F32)
    ob = [P.sb("ob%d" % i, [128, NG], BF16) for i in range(2)]
    ss_ps = P.ps("ss_ps", [128, NG], F32)
    STp = [P.ps("ST%d" % i, [128, NG], F32) for i in range(2)]
    OT = P.ps("OT", [128, NG], F32)
    SUM = P.ps("SUM", [128, NG], F32)
    scale = 192.0 ** -0.5

    P.dma(mask[:].rearrange("p a b -> p (a b)"), mask_d, writes=[mask])
    P.dma(wq[:], wq_d, writes=[wq])
    P.dma(wkv[:], wkv_d, writes=[wkv])
    P.dma(gg[:], g_d, writes=[gg])
    P.dma(fq[:], fq_d, writes=[fq])
    cnt = {"pj": 0, "st": 0, "pt": 0, "ob": 0}

    def nextpj():
        b = pj[cnt["pj"] % 2]
        cnt["pj"] += 1
        return b

    def proj(ps, M, w, woff, src, nchunk, N=NG):
        def mm(e):
            ins = None
            for c in range(nchunk):
                ins = e.matmul(ps[0:M, 0:N], lhsT=w[:, c, woff:woff + M], rhs=src[:, c, 0:N], start=(c == 0), stop=(c == nchunk - 1))
            return ins
        P.op("tensor", mm, [w, src], [ps])

    for b in range(NB):
        for g in range(T // NG):
            c0 = b * T + g * NG
            P.dma(ckv[:], ckv_d[:, :, c0:c0 + NG], writes=[ckv])
            P.dma(kr[:], kr_d[:, c0:c0 + NG], writes=[kr])
            P.dma(krs[:], krs_d[:, c0:c0 + NG], writes=[krs])
            P.dma(posi[:], pos_d[:, c0:c0 + NG], writes=[posi])
            P.dma(cq[:], cq_d[:, :, c0:c0 + NG], writes=[cq])
            emit_rstd(P, ckv, 2, NG, ones_bf, sq, ss_ps, rstd, 256)

            def sc_kv(e):
                ins = None
                for c in range(2):
                    ins = e.scalar_tensor_tensor(out=ckvn[:, c, :], in0=ckv[:, c, :], scalar=gg[:, 4 + c:5 + c], in1=rstd[:], op0=ALU.mult, op1=ALU.mult)
                return ins
            P.op("vector", sc_kv, [ckv, gg, rstd], [ckvn])
            ps = nextpj()
            proj(ps, 128, wkv, 0, ckvn, 2)
            P.op("scalar", lambda e, ps=ps, g=g: e.activation(out=KT[:, g * NG:(g + 1) * NG], in_=ps[:, 0:NG], func=AF.Copy), [ps], [KT])
            ps = nextpj()

            def mmv(e, ps=ps):
                ins = None
                for st_ in range(4):
                    for c in range(2):
                        ins = e.matmul(ps[:, st_ * 128:(st_ + 1) * 128], lhsT=ckvn[:, c, st_ * 128:(st_ + 1) * 128], rhs=wkv[:, c, 128:256], start=(c == 0), stop=(c == 1))
                return ins
            P.op("tensor", mmv, [ckvn, wkv], [ps])
            P.op("vector", lambda e, ps=ps, g=g: e.tensor_copy(V[:, 4 * g:4 * g + 4, :], ps[:, 0:NG].rearrange("p (a b) -> p a b", b=128)), [ps], [V])
            P.op("vector", lambda e: e.tensor_copy(ang[:], posi[:]), [posi], [ang])
            P.op("vector", lambda e: e.tensor_scalar(ang[:], ang[:], fq[:, 0:1], None, op0=ALU.mult), [ang, fq], [ang])
            P.op("vector", lambda e: e.tensor_scalar(t1[:], ang[:], 1.0 / (2 * PI), None, op0=ALU.mult), [ang], [t1])
            P.op("vector", lambda e: e.tensor_copy(posi[:], t1[:]), [t1], [posi])
            P.op("vector", lambda e: e.tensor_copy(t1[:], posi[:]), [posi], [t1])
            P.op("vector", lambda e: e.scalar_tensor_tensor(out=sin[:], in0=t1[:], scalar=-2 * PI, in1=ang[:], op0=ALU.mult, op1=ALU.add), [t1, ang], [sin])
            P.op("vector", lambda e: e.tensor_single_scalar(t1[:], sin[:], PI, op=ALU.is_gt), [sin], [t1])
            P.op("vector", lambda e: e.scalar_tensor_tensor(out=sin[:], in0=t1[:], scalar=-2 * PI, in1=sin[:], op0=ALU.mult, op1=ALU.add), [t1, sin], [sin])
            P.op("vector", lambda e: e.tensor_single_scalar(t1[:], sin[:], -PI, op=ALU.is_lt), [sin], [t1])
            P.op("vector", lambda e: e.scalar_tensor_tensor(out=sin[:], in0=t1[:], scalar=2 * PI, in1=sin[:], op0=ALU.mult, op1=ALU.add), [t1, sin], [sin])
            P.op("vector", lambda e: e.tensor_scalar(cos[:], sin[:], 0.5 * PI, None, op0=ALU.add), [sin], [cos])
            P.op("vector", lambda e: e.tensor_single_scalar(t1[:], cos[:], PI, op=ALU.is_gt), [cos], [t1])
            P.op("vector", lambda e: e.scalar_tensor_tensor(out=cos[:], in0=t1[:], scalar=-2 * PI, in1=cos[:], op0=ALU.mult, op1=ALU.add), [t1, cos], [cos])
            P.op("scalar", lambda e: e.activation(out=sin[:], in_=sin[:], func=AF.Sin), [sin], [sin])
            P.op("scalar", lambda e: e.activation(out=cos[:], in_=cos[:], func=AF.Sin), [cos], [cos])
            P.op("vector", lambda e: e.tensor_tensor(out=t1[:], in0=kr[:], in1=cos[:], op=ALU.mult), [kr, cos], [t1])
            P.op("vector", lambda e: e.scalar_tensor_tensor(out=t2[:], in0=krs[:], scalar=fq[:, 1:2], in1=sin[:], op0=ALU.mult, op1=ALU.mult), [krs, fq, sin], [t2])
            P.op("vector", lambda e, g=g: e.tensor_tensor(out=KRT[:, g * NG:(g + 1) * NG], in0=t1[:], in1=t2[:], op=ALU.add), [t1, t2], [KRT])
            emit_rstd(P, cq, 4, NG, ones_bf, sq, ss_ps, rstd, 512)

            def sc_q(e):
                ins = None
                for c in range(4):
                    ins = e.scalar_tensor_tensor(out=cqn[:, c, :], in0=cq[:, c, :], scalar=gg[:, c:c + 1], in1=rstd[:], op0=ALU.mult, op1=ALU.mult)
                return ins
            P.op("vector", sc_q, [cq, gg, rstd], [cqn])
            ps = nextpj()
            proj(ps, 128, wq, 0, cqn, 4)
            P.op("scalar", lambda e, ps=ps: e.activation(out=QN[:], in_=ps[:, 0:NG], func=AF.Copy, scale=scale), [ps], [QN])
            psa = nextpj()
            proj(psa, 64, wq, 128, cqn, 4)
            P.op("vector", lambda e, psa=psa: e.tensor_tensor(out=t1[:], in0=psa[0:64, 0:NG], in1=cos[:], op=ALU.mult), [psa, cos], [t1])
            psb = nextpj()
            proj(psb, 64, wq, 192, cqn, 4)
            P.op("vector", lambda e, psb=psb: e.scalar_tensor_tensor(out=t2[:], in0=psb[0:64, 0:NG], scalar=fq[:, 1:2], in1=sin[:], op0=ALU.mult, op1=ALU.mult), [psb, fq, sin], [t2])
            P.op("vector", lambda e: e.tensor_tensor(out=t1[:], in0=t1[:], in1=t2[:], op=ALU.add), [t1, t2], [t1])
            P.op("scalar", lambda e: e.activation(out=QR[:], in_=t1[:], func=AF.Copy, scale=scale), [t1], [QR])
            nj = 4 * (g + 1)
            for j in range(nj):
                st = STp[cnt["st"] % 2]
                cnt["st"] += 1
                pt = PT[cnt["pt"] % 3]
                cnt["pt"] += 1

                def mms(e, st=st, j=j):
                    e.matmul(st[:, 0:NG], lhsT=KT[:, j * 128:(j + 1) * 128], rhs=QN[:], start=True, stop=False)
                    return e.matmul(st[:, 0:NG], lhsT=KRT[:, j * 128:(j + 1) * 128], rhs=QR[:], start=False, stop=True)
                P.op("tensor", mms, [KT, KRT, QN, QR], [st])
                P.op("scalar", lambda e, st=st, pt=pt: e.activation(out=pt[:], in_=st[:, 0:NG], func=AF.Exp), [st], [pt])
                if j >= 4 * g:
                    P.op("gpsimd", lambda e, pt=pt, jj=j - 4 * g: e.tensor_tensor(out=pt[:], in0=pt[:], in1=mask[:, jj, :], op=ALU.mult), [pt, mask], [pt])

                def mmo(e, pt=pt, j=j, nj=nj):
                    e.matmul(OT[:, 0:NG], lhsT=V[:, j, :], rhs=pt[:], start=(j == 0), stop=(j == nj - 1))
                    return e.matmul(SUM[:, 0:NG], lhsT=ones_bf[:], rhs=pt[:], start=(j == 0), stop=(j == nj - 1))
                P.op("tensor", mmo, [V, pt, ones_bf], [OT, SUM])
            o = ob[cnt["ob"] % 2]
            cnt["ob"] += 1
            P.op("vector", lambda e: e.reciprocal(rs[:], SUM[:, 0:NG]), [SUM], [rs])
            P.op("vector", lambda e, o=o: e.tensor_tensor(out=o[:], in0=OT[:, 0:NG], in1=rs[:], op=ALU.mult), [OT, rs], [o])
            P.dma(co_d[:, c0:c0 + NG], o[:], reads=[o])


def emit_lru(P, nc, T, NB, pj, chunk=512):
    TT = T * NB
    NG = chunk
    xl_d = dram_in(nc, "xlT", [128, TT], F32)
    yg_d = dram_in(nc, "ygT", [128, TT], F32)
    wa_d = dram_in(nc, "lwa", [128, 128], BF16)
    wx_d = dram_in(nc, "lwx", [128, 128], BF16)
    lp_d = dram_in(nc, "lrup", [128, 8], F32)
    do_d = dram_out(nc, "doutT", [128, TT], BF16)
    wa = P.sb("lwa", [128, 128], BF16)
    wx = P.sb("lwx", [128, 128], BF16)
    lp = P.sb("lrup", [128, 8], F32)
    sp = P.sb("lsp", [128, 2], F32)
    X = [P.sb("lX%d" % i, [128, 3 + NG], F32) for i in range(2)]
    Y = [P.sb("lY%d" % i, [128, NG], F32) for i in range(2)]
    xc = P.sb("lxc", [128, NG], F32)
    xcb = P.sb("lxcb", [128, NG], BF16)
    gr = P.sb("lgr", [128, NG], F32)
    gi_ = P.sb("lgi", [128, NG], F32)
    a = P.sb("la", [128, NG], F32)
    mu = P.sb("lmu", [128, NG], F32)
    H = [P.sb("lH%d" % i, [128, NG], F32) for i in range(2)]
    do = [P.sb("ldo%d" % i, [128, NG], BF16) for i in range(2)]
    pa, px = pj
    P.dma(wa[:], wa_d, writes=[wa])
    P.dma(wx[:], wx_d, writes=[wx])
    P.dma(lp[:], lp_d, writes=[lp])
    P.op("scalar", lambda e: e.activation(out=sp[:, 0:1], in_=lp[:, 7:8], func=AF.Exp, scale=-1.0), [lp], [sp])
    P.op("scalar", lambda e: e.activation(out=sp[:, 0:1], in_=sp[:, 0:1], func=AF.Ln, bias=1.0), [sp], [sp])
    P.op("vector", lambda e: e.tensor_scalar(sp[:, 1:2], sp[:, 0:1], -8.0, None, op0=ALU.mult), [sp], [sp])
    it = 0
    for b in range(NB):
        for ci in range(T // NG):
            c0 = b * T + ci * NG
            Xb, Yb, Hb, dob = X[it % 2], Y[it % 2], H[it % 2], do[it % 2]
            Hprev = H[(it + 1) % 2]
            it += 1
            if ci == 0:
                P.op("gpsimd", lambda e, Xb=Xb: e.memset(Xb[:, 0:3], 0.0), [], [Xb])
                P.dma(Xb[:, 3:3 + NG], xl_d[:, c0:c0 + NG], writes=[Xb])
            else:
                P.dma(Xb[:, 0:3 + NG], xl_d[:, c0 - 3:c0 + NG], writes=[Xb])
            P.dma(Yb[:], yg_d[:, c0:c0 + NG], writes=[Yb])
            P.op("vector", lambda e, Xb=Xb: e.tensor_scalar(xc[:], Xb[:, 3:3 + NG], lp[:, 3:4], lp[:, 4:5], op0=ALU.mult, op1=ALU.add), [Xb, lp], [xc])
            for k in range(3):
                P.op("vector", lambda e, Xb=Xb, k=k: e.scalar_tensor_tensor(out=xc[:], in0=Xb[:, k:k + NG], scalar=lp[:, k:k + 1], in1=xc[:], op0=ALU.mult, op1=ALU.add), [Xb, lp, xc], [xc])
            P.op("gpsimd", lambda e: e.tensor_copy(xcb[:], xc[:]), [xc], [xcb])
            P.op("tensor", lambda e: e.matmul(pa[:], lhsT=wa[:], rhs=xcb[:], start=True, stop=True), [wa, xcb], [pa])
            P.op("tensor", lambda e: e.matmul(px[:], lhsT=wx[:], rhs=xcb[:], start=True, stop=True), [wx, xcb], [px])
            P.op("scalar", lambda e: e.activation(out=gr[:], in_=pa[:], func=AF.Sigmoid, bias=lp[:, 5:6]), [pa, lp], [gr])
            P.op("scalar", lambda e: e.activation(out=gi_[:], in_=px[:], func=AF.Sigmoid, bias=lp[:, 6:7]), [px, lp], [gi_])
            P.op("scalar", lambda e: e.activation(out=a[:], in_=gr[:], func=AF.Exp, scale=sp[:, 1:2]), [gr, sp], [a])
            P.op("vector", lambda e: e.tensor_tensor(out=mu[:], in0=a[:], in1=a[:], op=ALU.mult), [a], [mu])
            P.op("vector", lambda e: e.tensor_scalar(mu[:], mu[:], -1.0, 1.0, op0=ALU.mult, op1=ALU.add), [mu], [mu])
            P.op("scalar", lambda e: e.activation(out=mu[:], in_=mu[:], func=AF.Sqrt), [mu], [mu])
            P.op("gpsimd", lambda e: e.tensor_tensor(out=gi_[:], in0=gi_[:], in1=xc[:], op=ALU.mult), [gi_, xc], [gi_])
            P.op("gpsimd", lambda e: e.tensor_tensor(out=mu[:], in0=mu[:], in1=gi_[:], op=ALU.mult), [mu, gi_], [mu])
            if ci == 0:
                P.op("vector", lambda e, Hb=Hb: e.tensor_tensor_scan(Hb[:], a[:], mu[:], 0.0, op0=ALU.mult, op1=ALU.add), [a, mu], [Hb])
            else:
                P.op("vector", lambda e, Hb=Hb, Hprev=Hprev: e.tensor_tensor_scan(Hb[:], a[:], mu[:], Hprev[:, NG - 1:NG], op0=ALU.mult, op1=ALU.add), [a, mu, Hprev], [Hb])
            P.op("scalar", lambda e, Yb=Yb: e.activation(out=Yb[:], in_=Yb[:], func=AF.Gelu_apprx_tanh), [Yb], [Yb])
            P.op("gpsimd", lambda e, Hb=Hb, Yb=Yb, dob=dob: e.tensor_tensor(out=dob[:], in0=Hb[:], in1=Yb[:], op=ALU.mult), [Hb, Yb], [dob])
            P.dma(do_d[:, c0:c0 + NG], dob[:], reads=[dob])


def build_KD(T=16384, NB=2, do_mla=True, do_lru=True):
    nc = bass.Bass("TRN2", target_bir_lowering=False)
    P = Prog(nc)
    ones_d = dram_in(nc, "ones", [128, 128], F32)
    ones_f = P.sb("ones_f", [128, 128], F32)
    ones_bf = P.sb("ones_bf", [128, 128], BF16)
    P.dma(ones_f[:], ones_d, writes=[ones_f])
    P.op("vector", lambda e: e.tensor_copy(ones_bf[:], ones_f[:]), [ones_f], [ones_bf])
    pj = [P.ps("pj%d" % i, [128, 512], F32) for i in range(2)]
    if do_lru:
        emit_lru(P, nc, T, NB, pj)
    if do_mla:
        emit_mla(P, nc, T, NB, ones_bf, pj)
    wait_all_dma(P)
    P.build()
    P.close()
    return nc


def build_KW(L, CH=4096):
    nc = bass.Bass("TRN2", target_bir_lowering=False)
    P = Prog(nc)
    x_d = dram_in(nc, "wf", [128, L], F32)
    o_d = dram_out(nc, "wb", [128, L], BF16)
    xin = [P.sb("xin%d" % i, [128, CH], F32) for i in range(3)]
    xo = [P.sb("xo%d" % i, [128, CH], BF16) for i in range(3)]
    engs = ["vector", "scalar", "gpsimd"]
    n0 = 0
    i = 0
    while n0 < L:
        n = min(CH, L - n0)
        a, b = xin[i % 3], xo[i % 3]
        P.dma(a[:, 0:n], x_d[:, n0:n0 + n], writes=[a])
        eng = engs[i % 3]
        if eng == "scalar":
            P.op(eng, lambda e, a=a, b=b, n=n: e.activation(out=b[:, 0:n], in_=a[:, 0:n], func=AF.Copy), [a], [b])
        else:
            P.op(eng, lambda e, a=a, b=b, n=n: e.tensor_copy(b[:, 0:n], a[:, 0:n]), [a], [b])
        P.dma(o_d[:, n0:n0 + n], b[:, 0:n], reads=[b])
        n0 += n
        i += 1
    wait_all_dma(P)
    P.build()
    P.close()
    return nc


import ml_dtypes
from concourse.bass_utils import run_bass_kernel_spmd

NCORES = 8
BATCH, SEQ, DM = 2, 16384, 2048
TPC = SEQ // 4
_NP_BF16 = ml_dtypes.bfloat16
_cache = {}


def _run(name, builder, in_maps):
    if name not in _cache:
        _cache[name] = builder()
    nc = _cache[name]
    res = run_bass_kernel_spmd(nc, in_maps, core_ids=list(range(NCORES)))
    return res.results


def _pc(a):
    return np.ascontiguousarray(a)


def _g16(g):
    return _pc(g.reshape(-1, 128).T)


def _cast_weights(ws):
    names = list(ws.keys())
    flat = np.concatenate([ws[k].reshape(-1) for k in names])
    n = flat.size
    per = NCORES * 128
    L = -(-n // per)
    L = -(-L // 8) * 8
    pad = np.zeros(per * L, np.float32)
    pad[:n] = flat
    pad = pad.reshape(NCORES, 128, L)
    res = _run("KW%d" % L, lambda: build_KW(L), [{"wf": pad[c]} for c in range(NCORES)])
    out = np.concatenate([np.asarray(r["wb"]).reshape(-1) for r in res])
    outd = {}
    o = 0
    for k in names:
        sz = ws[k].size
        outd[k] = out[o:o + sz].reshape(ws[k].shape)
        o += sz
    return outd


def _mla_mask():
    m = np.zeros((128, 4, 512), np.float32)
    sl = np.arange(128)[:, None]
    ql = np.arange(512)[None, :]
    for j in range(4):
        m[:, j, :] = (128 * j + sl < (ql // 64 + 1) * 64)
    return m.reshape(128, 2048).astype(_NP_BF16)


def _ke_layer(xT_full, catT_full, L, gpost, gpre, gfpost, w_out, up, down, conv_w, conv_b):
    gs = _pc(np.concatenate([_g16(gpost), _g16(gpre), _g16(gfpost)], axis=1))
    cwm = _pc(conv_w.reshape(3, 64, 128).transpose(2, 0, 1).reshape(128, 192))
    cbm = _pc(conv_b.reshape(64, 128).T)
    ones = np.ones((128, 128), np.float32)
    ims = []
    for c in range(NCORES):
        b, j = divmod(c, 4)
        t0 = j * TPC
        xs = xT_full[b]
        cs = catT_full[b]
        if j == 0:
            xh = np.zeros((DM, 2), np.float32)
            ch = np.zeros((DM, 2), _NP_BF16)
        else:
            xh = _pc(xs[:, t0 - 2:t0])
            ch = _pc(cs[:, t0 - 2:t0])
        ims.append({"xT": _pc(xs[:, t0:t0 + TPC]), "catT": _pc(cs[:, t0:t0 + TPC]), "xhT": xh, "cathT": ch,
                    "gs": gs, "w_out": w_out, "up": up, "down": down, "cw": cwm, "cb": cbm, "ones": ones})
    res = _run("KE", lambda: build_KE(TOK=TPC), ims)
    out = []
    for b in range(BATCH):
        out.append(np.concatenate([np.asarray(res[b * 4 + j]["yT"]) for j in range(4)], axis=1))
    return out


def _ka_layer(xT_full, g, W, tiles, nout, outs, key):
    ones = np.ones((128, 128), np.float32)
    ims = []
    for c in range(NCORES):
        b, j = divmod(c, 4)
        ims.append({"xT": _pc(xT_full[b][:, j * TPC:(j + 1) * TPC]), "g": _g16(g), "W": W, "ones": ones})
    res = _run(key, lambda: build_KA(tiles, nout, outs, TOK=TPC, TOKB=1024), ims)
    z = {}
    for k in outs:
        z[k] = [np.concatenate([np.asarray(res[b * 4 + j][k]) for j in range(4)], axis=1) for b in range(BATCH)]
    return z


def kernel(x, positions, mix_pre_g, mix_post_g, ffn_pre_g, ffn_post_g,
           ffn_up, ffn_conv_w, ffn_conv_b, ffn_down,
           ab_w_in, pool_w, pool_b, pool_scale, ab_w_out,
           cd_w_in, q_norm_g, w_q_up, kv_norm_g, w_kv_up,
           lru_conv_w, lru_conv_b, lru_wa, lru_ba, lru_wx, lru_bx, lru_lambda, cd_w_out):
    f32 = lambda a: np.asarray(a, dtype=np.float32)
    x = f32(x)
    positions = np.asarray(positions).astype(np.int32)
    wb = _cast_weights({"ffn_up": f32(ffn_up), "ffn_down": f32(ffn_down), "ab_w_in": f32(ab_w_in), "ab_w_out": f32(ab_w_out),
                        "cd_w_in": f32(cd_w_in), "cd_w_out": f32(cd_w_out), "w_q_up": f32(w_q_up), "w_kv_up": f32(w_kv_up),
                        "pool_w": f32(pool_w), "lru_wa": f32(lru_wa), "lru_wx": f32(lru_wx)})
    xT = [_pc(x[b].T) for b in range(BATCH)]
    z = _ka_layer(xT, f32(mix_pre_g)[0], _pc(wb["ab_w_in"][0]), AB_TILES, 5200, AB_OUTS, "KA0")
    ident = np.eye(128, dtype=np.float32)
    ims = []
    qidx_all = []
    for c in range(NCORES):
        b, j = divmod(c, 4)
        qidx = np.concatenate([np.arange((4 * m + j) * 128, (4 * m + j + 1) * 128) for m in range(32)])
        qidx_all.append(qidx)
        ql = np.arange(128)[:, None]
        sl = np.arange(512)[None, :]
        negmask = np.where(sl < 128 * j + 64 * (ql // 64 + 1), 0.0, -1e30).astype(np.float32)
        ims.append({"qT": _pc(z["qT"][b][:, qidx]), "qiT": _pc(z["qiT"][b][:, qidx]), "wi": _pc(z["wiT"][b][:, qidx].T),
                    "kiT": z["kiT"][b], "kT": z["kT"][b], "v": _pc(z["vT"][b].T), "negmask": negmask, "ident": ident})
    res = _run("KB", lambda: build_KB(T=SEQ, NSLOT=32), ims)
    aoutT = [np.zeros((1024, SEQ), _NP_BF16) for _ in range(BATCH)]
    for c in range(NCORES):
        b, j = divmod(c, 4)
        aoutT[b][:, qidx_all[c]] = np.asarray(res[c]["aoutT"])
    del ims
    psb = _pc(np.concatenate([f32(pool_scale)[0].reshape(8, 128).T, f32(pool_b)[0].reshape(8, 128).T], axis=1))
    ims = []
    for c in range(NCORES):
        b, j = divmod(c, 4)
        t0 = j * TPC
        u = z["uT"][b]
        rcv = np.zeros((4, 16), np.float32)
        for g_, w_ in enumerate((2, 4, 8, 16)):
            rcv[g_] = 1.0 / (np.minimum(np.arange(16) + 1, w_) if j == 0 else w_)
        rc = _pc(np.broadcast_to(rcv[None, :, None, :], (128, 4, 2, 16)).reshape(128, -1))
        uh = np.zeros((1024, 16), np.float32) if j == 0 else _pc(u[:, t0 - 16:t0])
        ims.append({"uT": _pc(u[:, t0:t0 + TPC]), "uhT": uh, "rc": rc, "pw": _pc(wb["pool_w"][0]), "psb": psb})
    res = _run("KC", lambda: build_KC(TOK=TPC), ims)
    catT = []
    for b in range(BATCH):
        bout = np.concatenate([np.asarray(res[b * 4 + j]["boutT"]) for j in range(4)], axis=1)
        catT.append(np.concatenate([aoutT[b], bout], axis=0))
    del z
    x2T = _ke_layer(xT, catT, 0, f32(mix_post_g)[0], f32(ffn_pre_g)[0], f32(ffn_post_g)[0], _pc(wb["ab_w_out"][0]),
                    _pc(wb["ffn_up"][0]), _pc(wb["ffn_down"][0]), f32(ffn_conv_w)[0], f32(ffn_conv_b)[0])
    del catT, xT
    z = _ka_layer(x2T, f32(mix_pre_g)[1], _pc(wb["cd_w_in"][0]), CD_TILES, 2880, CD_OUTS, "KA1")
    allc = lambda k: np.concatenate([z[k][b] for b in range(BATCH)], axis=1)
    cq_all, ckv_all, kr_all, xl_all, yg_all = allc("cqT"), allc("ckvT"), allc("krT"), allc("xlT"), allc("ygT")
    krs_all = _pc(np.concatenate([kr_all[32:], kr_all[:32]], axis=0))
    posrep = _pc(np.broadcast_to(positions.reshape(1, -1), (64, BATCH * SEQ)))
    freq = (np.float32(10000.0) ** (-np.arange(32, dtype=np.float32) / np.float32(32))).astype(np.float32)
    fq = _pc(np.stack([np.concatenate([freq, freq]), np.concatenate([-np.ones(32), np.ones(32)])], axis=1).astype(np.float32))
    mlag = _pc(np.concatenate([f32(q_norm_g)[0].reshape(4, 128).T, f32(kv_norm_g)[0].reshape(2, 128).T], axis=1))
    mask = _mla_mask()
    ones = np.ones((128, 128), np.float32)
    ims = []
    for c in range(NCORES):
        wq_full = wb["w_q_up"][0][:, c * 192:(c + 1) * 192]
        wq = _pc(np.concatenate([wq_full, wq_full[:, 160:192], wq_full[:, 128:160]], axis=1))
        wkv = _pc(wb["w_kv_up"][0][:, c * 256:(c + 1) * 256])
        sl = slice(c * 128, (c + 1) * 128)
        lrup = _pc(np.stack([f32(lru_conv_w)[0][0, sl], f32(lru_conv_w)[0][1, sl], f32(lru_conv_w)[0][2, sl], f32(lru_conv_w)[0][3, sl],
                             f32(lru_conv_b)[0][sl], f32(lru_ba)[0][sl], f32(lru_bx)[0][sl], f32(lru_lambda)[0][sl]], axis=1).astype(np.float32))
        ims.append({"ones": ones, "cqT": cq_all, "ckvT": ckv_all, "krT": kr_all, "krsT": krs_all, "posrep": posrep, "fq": fq,
                    "wq": wq, "wkv": wkv, "mlag": mlag, "mask": mask,
                    "xlT": _pc(xl_all[sl]), "ygT": _pc(yg_all[sl]), "lwa": _pc(wb["lru_wa"][0][c]), "lwx": _pc(wb["lru_wx"][0][c]), "lrup": lrup})
    res = _run("KD", lambda: build_KD(T=SEQ, NB=BATCH), ims)
    cat_all = np.concatenate([np.asarray(res[c]["coutT"]) for c in range(NCORES)] + [np.asarray(res[c]["doutT"]) for c in range(NCORES)], axis=0)
    catT = [_pc(cat_all[:, b * SEQ:(b + 1) * SEQ]) for b in range(BATCH)]
    del ims, z, cq_all, ckv_all, kr_all, xl_all, yg_all
    yT = _ke_layer(x2T, catT, 1, f32(mix_post_g)[1], f32(ffn_pre_g)[1], f32(ffn_post_g)[1], _pc(wb["cd_w_out"][0]),
                   _pc(wb["ffn_up"][1]), _pc(wb["ffn_down"][1]), f32(ffn_conv_w)[1], f32(ffn_conv_b)[1])
    out = np.stack([_pc(yT[b].T) for b in range(BATCH)], axis=0).astype(np.float32)
    return out
```

```python
import numpy as np
import concourse.bass as bass
import concourse.mybir as mybir

F32 = mybir.dt.float32
BF16 = mybir.dt.bfloat16
I32 = mybir.dt.int32
ALU = mybir.AluOpType
AF = mybir.ActivationFunctionType
AX = mybir.AxisListType


class Buf:
    __slots__ = ("name", "t", "last_w", "readers")

    def __init__(self, name, t=None):
        self.name = name
        self.t = t
        self.last_w = None
        self.readers = []

    def __getitem__(self, idx):
        return self.t[idx]


class Prog:
    COMPUTE = ("tensor", "vector", "scalar", "gpsimd")
    DMAQ = ("sync", "gpsimd")

    def __init__(self, nc, n_chan=12):
        self.nc = nc
        self.stack = []
        self.ops = {e: [] for e in ("sync", "tensor", "vector", "scalar", "gpsimd")}
        self.cnt = {}
        self.sems = {}
        for e in self.COMPUTE:
            self.sems[e] = self._enter(nc.semaphore("s_" + e))
            self.cnt[e] = 0
        self.chans = []
        for i in range(n_chan):
            k = "ch%d" % i
            self.sems[k] = self._enter(nc.semaphore("s_" + k))
            self.cnt[k] = 0
            self.chans.append(k)
        self.chan_rr = 0
        self.waited = {e: {} for e in self.ops}
        self.nbuf = 0

    def _enter(self, cm):
        v = cm.__enter__()
        self.stack.append(cm)
        return v

    def sb(self, name, shape, dt):
        t = self._enter(self.nc.sbuf_tensor("sb_" + name, list(shape), dt))
        return Buf(name, t)

    def ps(self, name, shape, dt=F32):
        t = self._enter(self.nc.psum_tensor("ps_" + name, list(shape), dt))
        return Buf(name, t)

    def view(self, name):
        return Buf(name, None)

    def _deps(self, reads, writes):
        deps = {}
        def add(tok):
            if tok is None:
                return
            k, v = tok
            if deps.get(k, 0) < v:
                deps[k] = v
        for b in reads:
            add(b.last_w)
        for b in writes:
            add(b.last_w)
            for r in b.readers:
                add(r)
        return deps

    def _commit(self, tok, reads, writes):
        for b in reads:
            b.readers.append(tok)
            if len(b.readers) > 64:
                m = {}
                for k, v in b.readers:
                    if m.get(k, 0) < v:
                        m[k] = v
                b.readers = list(m.items())
        for b in writes:
            b.last_w = tok
            b.readers = []

    def _waits(self, eng, deps, same_engine_sync=True):
        w = []
        wd = self.waited[eng]
        for k, v in deps.items():
            if k == eng and (eng == "tensor" or not same_engine_sync):
                continue
            if wd.get(k, 0) >= v:
                continue
            wd[k] = v
            w.append((k, v))
        return w

    def op(self, eng, fn, reads=(), writes=(), sync_self=True):
        deps = self._deps(reads, writes)
        waits = self._waits(eng, deps, sync_self)
        self.cnt[eng] += 1
        tok = (eng, self.cnt[eng])
        self.ops[eng].append((waits, fn, (eng, 1)))
        self._commit(tok, reads, writes)
        return tok

    def dma(self, out_ap, in_ap, reads=(), writes=(), q="sync", **kw):
        deps = self._deps(reads, writes)
        ch = self.chans[self.chan_rr]
        self.chan_rr = (self.chan_rr + 1) % len(self.chans)
        if self.cnt[ch] > 0:
            v = 16 * self.cnt[ch]
            if deps.get(ch, 0) < v:
                deps[ch] = v
        waits = self._waits(q, deps)
        self.cnt[ch] += 1
        tok = (ch, 16 * self.cnt[ch])

        def fn(e, out_ap=out_ap, in_ap=in_ap, kw=kw):
            return e.dma_start(out=out_ap, in_=in_ap, **kw)
        self.ops[q].append((waits, fn, (ch, 16)))
        self._commit(tok, reads, writes)
        return tok

    def finish_wait(self, eng, bufs):
        deps = self._deps(bufs, ())
        waits = self._waits(eng, deps)
        self.ops[eng].append((waits, None, None))

    def build(self):
        nc = self.nc
        blk = self._enter(nc.Block())
        P = self

        def emit(ename):
            def body(e):
                for waits, fn, inc in P.ops[ename]:
                    for k, v in waits:
                        e.wait_ge(P.sems[k], v)
                    if fn is not None:
                        ins = fn(e)
                        ins.then_inc(P.sems[inc[0]], inc[1])
            return body

        blk.sync(emit("sync"))
        blk.tensor(emit("tensor"))
        blk.vector(emit("vector"))
        blk.scalar(emit("scalar"))
        blk.gpsimd(emit("gpsimd"))

    def close(self):
        while self.stack:
            cm = self.stack.pop()
            cm.__exit__(None, None, None)


EPS = 1e-6


def dram_in(nc, name, shape, dt):
    return nc.dram_tensor(name, list(shape), dt, kind="ExternalInput").ap()


def dram_out(nc, name, shape, dt):
    return nc.dram_tensor(name, list(shape), dt, kind="ExternalOutput").ap()


def wait_all_dma(P, eng="sync"):
    for ch in P.chans:
        if P.cnt[ch]:
            P.ops[eng].append(([(ch, 16 * P.cnt[ch])], None, None))


def emit_rmsnorm_T(P, xs, g_sb, ones_bf, sq, ss_ps, rstd, hT, hoff, N, D, nch, x_reads, tag=""):
    P.op("scalar", lambda e: e.activation(out=sq[:, 0:nch, 0:N], in_=xs[:, 0:nch, 0:N], func=AF.Square), [xs], [sq])

    def mm(e):
        ins = None
        for c in range(nch):
            ins = e.matmul(ss_ps[:, 0:N], lhsT=ones_bf[:], rhs=sq[:, c, 0:N], start=(c == 0), stop=(c == nch - 1))
        return ins
    P.op("tensor", mm, [sq, ones_bf], [ss_ps])
    P.op("vector", lambda e: e.tensor_scalar(rstd[:, 0:N], ss_ps[:, 0:N], 1.0 / D, EPS, op0=ALU.mult, op1=ALU.add), [ss_ps], [rstd])
    P.op("scalar", lambda e: e.activation(out=rstd[:, 0:N], in_=rstd[:, 0:N], func=AF.Sqrt), [rstd], [rstd])
    P.op("vector", lambda e: e.reciprocal(rstd[:, 0:N], rstd[:, 0:N]), [rstd], [rstd])

    def sc(e):
        ins = None
        for c in range(nch):
            ins = e.scalar_tensor_tensor(out=hT[:, c, hoff:hoff + N], in0=xs[:, c, 0:N], scalar=g_sb[:, c:c + 1],
                                         in1=rstd[:, 0:N], op0=ALU.mult, op1=ALU.mult)
        return ins
    P.op("vector", sc, [xs, g_sb, rstd], [hT])


def build_KA(tiles, NOUT, outs, TOK=4096, TOKB=2048, D=2048, WG=256):
    nc = bass.Bass("TRN2", target_bir_lowering=False)
    P = Prog(nc)
    nch = D // 128
    x_d = dram_in(nc, "xT", [D, TOK], F32).rearrange("(c p) n -> p c n", p=128)
    g_d = dram_in(nc, "g", [128, nch], F32)
    w_d = dram_in(nc, "W", [D, NOUT], BF16).rearrange("(c p) n -> p c n", p=128)
    ones_d = dram_in(nc, "ones", [128, 128], F32)
    o_d = {k: dram_out(nc, k, [r, TOK], dt) for k, (r, dt) in outs.items()}
    odt = {k: dt for k, (r, dt) in outs.items()}

    NG = 512
    xs = P.sb("xs", [128, nch, NG], F32)
    sq = P.sb("sq", [128, nch, NG], BF16)
    hT = P.sb("hT", [128, nch, TOKB], BF16)
    g_sb = P.sb("g_sb", [128, nch], F32)
    ones_f = P.sb("ones_f", [128, 128], F32)
    ones_bf = P.sb("ones_bf", [128, 128], BF16)
    rstd = P.sb("rstd", [128, NG], F32)
    ss_ps = P.ps("ss_ps", [128, NG], F32)
    wbf = [P.sb("wbf%d" % i, [128, nch, WG], BF16) for i in range(3)]
    mm_ps = [P.ps("mm_ps%d" % i, [128, NG], F32) for i in range(3)]
    ost = {}
    for k, (r, dt) in outs.items():
        if dt not in ost:
            ost[dt] = [P.sb("ost_%s_%d" % (str(dt)[-4:], i), [128, TOKB], dt) for i in range(2)]
    ocnt = {dt: 0 for dt in ost}

    P.dma(g_sb[:], g_d, writes=[g_sb])
    P.dma(ones_f[:], ones_d, writes=[ones_f])
    P.op("vector", lambda e: e.tensor_copy(ones_bf[:], ones_f[:]), [ones_f], [ones_bf])

    groups = []
    cur = []
    for t in tiles:
        if cur and (t[0] + t[1] - cur[0][0] > WG or t[0] != cur[-1][0] + cur[-1][1]):
            groups.append(cur)
            cur = []
        cur.append(t)
    if cur:
        groups.append(cur)

    outv = []
    wi = 0
    pi = 0
    for tb in range(TOK // TOKB):
        t0 = tb * TOKB
        for gi in range(TOKB // NG):
            P.dma(xs[:], x_d[:, :, t0 + gi * NG: t0 + (gi + 1) * NG], writes=[xs])
            emit_rmsnorm_T(P, xs, g_sb, ones_bf, sq, ss_ps, rstd, hT, gi * NG, NG, D, nch, None)
        for grp in groups:
            c0 = grp[0][0]
            c1 = grp[-1][0] + grp[-1][1]
            wb_ = wbf[wi % 3]
            wi += 1
            P.dma(wb_[:, :, 0:c1 - c0], w_d[:, :, c0:c1], writes=[wb_])
            for (col0, ncols, oname, row0) in grp:
                dt = odt[oname]
                ob = ost[dt][ocnt[dt] % 2]
                ocnt[dt] += 1
                for gi in range(TOKB // NG):
                    ps = mm_ps[pi % 3]
                    pi += 1

                    def mm(e, ps=ps, wb_=wb_, off=col0 - c0, ncols=ncols, gi=gi):
                        ins = None
                        for c in range(nch):
                            ins = e.matmul(ps[0:ncols, :], lhsT=wb_[:, c, off:off + ncols], rhs=hT[:, c, gi * NG:(gi + 1) * NG],
                                           start=(c == 0), stop=(c == nch - 1))
                        return ins
                    P.op("tensor", mm, [wb_, hT], [ps])
                    if gi % 2 == 0:
                        P.op("scalar", lambda e, ps=ps, ob=ob, ncols=ncols, gi=gi: e.activation(out=ob[0:ncols, gi * NG:(gi + 1) * NG], in_=ps[0:ncols, :], func=AF.Copy), [ps], [ob])
                    else:
                        P.op("vector", lambda e, ps=ps, ob=ob, ncols=ncols, gi=gi: e.tensor_copy(ob[0:ncols, gi * NG:(gi + 1) * NG], ps[0:ncols, :]), [ps], [ob])
                dst = P.view("o")
                outv.append(dst)
                P.dma(o_d[oname][row0:row0 + ncols, t0:t0 + TOKB], ob[0:ncols, :], reads=[ob], writes=[dst])
    wait_all_dma(P)
    P.build()
    P.close()
    return nc


AB_TILES = ([(i * 128, 128, "qT", i * 128) for i in range(8)]
            + [(1024 + i * 128, 128, "kT", i * 128) for i in range(8)]
            + [(2048 + i * 128, 128, "vT", i * 128) for i in range(8)]
            + [(3072 + i * 128, 128, "qiT", i * 128) for i in range(8)]
            + [(4096, 64, "kiT", 0), (4160, 16, "wiT", 0)]
            + [(4176 + i * 128, 128, "uT", i * 128) for i in range(8)])
AB_OUTS = {"qT": (1024, BF16), "kT": (1024, BF16), "vT": (1024, BF16), "qiT": (1024, BF16),
           "kiT": (64, BF16), "wiT": (16, F32), "uT": (1024, F32)}

CD_TILES = ([(i * 128, 128, "cqT", i * 128) for i in range(4)]
            + [(512 + i * 128, 128, "ckvT", i * 128) for i in range(2)]
            + [(768, 64, "krT", 0)]
            + [(832 + i * 128, 128, "xlT", i * 128) for i in range(8)]
            + [(1856 + i * 128, 128, "ygT", i * 128) for i in range(8)])
CD_OUTS = {"cqT": (512, F32), "ckvT": (256, F32), "krT": (64, F32), "xlT": (1024, F32), "ygT": (1024, F32)}


D = 2048
NCH = 16
DFF = 4096


def emit_rstd(P, src, nch, N, ones_bf, sq, ss_ps, rstd, Dn):
    P.op("scalar", lambda e: e.activation(out=sq[:, 0:nch, 0:N], in_=src[:, 0:nch, 0:N], func=AF.Square), [src], [sq])

    def mm(e):
        ins = None
        for c in range(nch):
            ins = e.matmul(ss_ps[:, 0:N], lhsT=ones_bf[:], rhs=sq[:, c, 0:N], start=(c == 0), stop=(c == nch - 1))
        return ins
    P.op("tensor", mm, [sq, ones_bf], [ss_ps])
    P.op("vector", lambda e: e.tensor_scalar(rstd[:, 0:N], ss_ps[:, 0:N], 1.0 / Dn, EPS, op0=ALU.mult, op1=ALU.add), [ss_ps], [rstd])
    P.op("scalar", lambda e: e.activation(out=rstd[:, 0:N], in_=rstd[:, 0:N], func=AF.Sqrt), [rstd], [rstd])
    P.op("vector", lambda e: e.reciprocal(rstd[:, 0:N], rstd[:, 0:N]), [rstd], [rstd])


def emit_scale(P, eng, dst, src, g_sb, gcol0, rstd, nch, N):
    def sc(e):
        ins = None
        for c in range(nch):
            ins = e.scalar_tensor_tensor(out=dst[:, c, 0:N], in0=src[:, c, 0:N], scalar=g_sb[:, gcol0 + c:gcol0 + c + 1],
                                         in1=rstd[:, 0:N], op0=ALU.mult, op1=ALU.mult)
        return ins
    rd = [src, g_sb, rstd]
    P.op(eng, sc, rd, [dst])


def split_groups(TOK, maxn=510):
    ng = -(-TOK // maxn)
    base = -(-TOK // ng)
    base = -(-base // 8) * 8
    sizes = []
    rem = TOK
    while rem > 0:
        n = min(base, rem)
        sizes.append(n)
        rem -= n
    return sizes


def build_KE(TOK=4096, stop_after=None):
    nc = bass.Bass("TRN2", target_bir_lowering=False)
    P = Prog(nc)
    r3 = lambda ap: ap.rearrange("(c p) n -> p c n", p=128)
    x_d = r3(dram_in(nc, "xT", [D, TOK], F32))
    cat_d = r3(dram_in(nc, "catT", [D, TOK], BF16))
    xh_d = r3(dram_in(nc, "xhT", [D, 2], F32))
    cath_d = r3(dram_in(nc, "cathT", [D, 2], BF16))
    gs_d = dram_in(nc, "gs", [128, 3 * NCH], F32)
    wout_d = r3(dram_in(nc, "w_out", [D, D], BF16))
    up_d = r3(dram_in(nc, "up", [D, 2 * DFF], BF16))
    down_d = r3(dram_in(nc, "down", [DFF, D], BF16))
    cw_d = dram_in(nc, "cw", [128, 3 * 64], F32)
    cb_d = dram_in(nc, "cb", [128, 64], F32)
    ones_d = dram_in(nc, "ones", [128, 128], F32)
    y_d = r3(dram_out(nc, "yT", [D, TOK], F32))

    NG = 512
    A = P.sb("A", [128, NCH, NG], F32)
    B = P.sb("B", [128, NCH, NG], F32)
    cb16 = P.sb("cb16", [128, NCH, NG], BF16)
    hb = P.sb("hb", [128, NCH, NG], BF16)
    act = P.sb("act", [128, 32, NG], BF16)
    NWB = 4
    wb = [P.sb("wb%d" % i, [128, NCH, 256], BF16) for i in range(NWB)]
    acc = [[P.sb("acc%d%d" % (i, j), [128, NG], F32) for j in range(2)] for i in range(2)]
    rstd = P.sb("rstd", [128, NG], F32)
    gs = P.sb("gs", [128, 3 * NCH], F32)
    cw = P.sb("cw", [128, 3 * 64], F32)
    cb = P.sb("cb", [128, 64], F32)
    ones_f = P.sb("ones_f", [128, 128], F32)
    ones_bf = P.sb("ones_bf", [128, 128], BF16)
    ss_ps = P.ps("ss_ps", [128, NG], F32)
    NPS = 5
    mm_ps = [P.ps("mm_ps%d" % i, [128, NG], F32) for i in range(NPS)]
    st = {"wi": 0, "pi": 0, "ti": 0}

    P.dma(gs[:], gs_d, writes=[gs])
    P.dma(cw[:], cw_d, writes=[cw])
    P.dma(cb[:], cb_d, writes=[cb])
    P.dma(ones_f[:], ones_d, writes=[ones_f])
    P.op("vector", lambda e: e.tensor_copy(ones_bf[:], ones_f[:]), [ones_f], [ones_bf])

    def next_wb():
        b = wb[st["wi"] % NWB]
        st["wi"] += 1
        return b

    def next_ps():
        b = mm_ps[st["pi"] % NPS]
        st["pi"] += 1
        return b

    def load_w(src_ap, ncols=256):
        b = next_wb()
        P.dma(b[:, :, 0:ncols], src_ap, writes=[b])
        return b

    def matmul_group(ps, M, N, parts, roff=0):
        def mm(e):
            ins = None
            tot = sum(len(p[3]) for p in parts)
            i = 0
            for (w, off, rb, chunks) in parts:
                for (wc, rc) in chunks:
                    ins = e.matmul(ps[0:M, 0:N], lhsT=w[:, wc, off:off + M], rhs=rb[:, rc, roff:roff + N], start=(i == 0), stop=(i == tot - 1))
                    i += 1
            return ins
        rd = []
        for p in parts:
            rd += [p[0], p[2]]
        P.op("tensor", mm, rd, [ps])

    def evac(ps, dst_ap_fn, dstbuf, k):
        if k % 2 == 0:
            P.op("scalar", lambda e: e.activation(out=dst_ap_fn(), in_=ps[:, 0:ps_n[0]], func=AF.Copy), [ps], [dstbuf])
        else:
            P.op("vector", lambda e: e.tensor_copy(dst_ap_fn(), ps[:, 0:ps_n[0]]), [ps], [dstbuf])
    ps_n = [0]

    def group(n0, N, halo):
        xsrc = xh_d if halo else x_d[:, :, n0:n0 + N]
        csrc = cath_d if halo else cat_d[:, :, n0:n0 + N]
        ho = 0 if halo else 2
        if not halo and n0 > 0:
            pN = prev_n[0]
            P.op("vector", lambda e: e.tensor_copy(rstd[:, 0:32].bitcast(BF16)[:, 0:32].rearrange("p (c n) -> p c n", n=2), hb[:, :, pN:pN + 2]), [hb], [rstd])
            P.op("vector", lambda e: e.tensor_copy(hb[:, :, 0:2], rstd[:, 0:32].bitcast(BF16)[:, 0:32].rearrange("p (c n) -> p c n", n=2)), [rstd], [hb])
        P.dma(cb16[:, :, 0:N], csrc, writes=[cb16])
        P.dma(A[:, :, 0:N], xsrc, writes=[A])
        for mp in range(8):
            w = load_w(wout_d[:, :, mp * 256:(mp + 1) * 256])
            for j in range(2):
                m = mp * 2 + j
                ps = next_ps()
                matmul_group(ps, 128, N, [(w, j * 128, cb16, [(c, c) for c in range(NCH)])])
                if m % 2 == 0:
                    P.op("scalar", lambda e, ps=ps, m=m: e.activation(out=B[:, m, 0:N], in_=ps[:, 0:N], func=AF.Copy), [ps], [B])
                else:
                    P.op("vector", lambda e, ps=ps, m=m: e.tensor_copy(B[:, m, 0:N], ps[:, 0:N]), [ps], [B])
        emit_rstd(P, B, NCH, N, ones_bf, act, ss_ps, rstd, D)
        emit_scale(P, "vector", B, B, gs, 0, rstd, NCH, N)
        P.op("gpsimd", lambda e: e.tensor_tensor(out=A[:, :, 0:N], in0=A[:, :, 0:N], in1=B[:, :, 0:N], op=ALU.add), [A, B], [A])
        if stop_after == "epi":
            if not halo:
                P.dma(y_d[:, :, n0:n0 + N], A[:, :, 0:N], reads=[A])
            return
        emit_rstd(P, A, NCH, N, ones_bf, act, ss_ps, rstd, D)

        def sc(e):
            ins = None
            for c in range(NCH):
                ins = e.scalar_tensor_tensor(out=hb[:, c, ho:ho + N], in0=A[:, c, 0:N], scalar=gs[:, NCH + c:NCH + c + 1],
                                             in1=rstd[:, 0:N], op0=ALU.mult, op1=ALU.mult)
            return ins
        P.op("vector", sc, [A, gs, rstd], [hb])
        if halo:
            return
        prev_n[0] = N
        NP = N + 2
        for pg in range(16):
            wg = load_w(up_d[:, :, pg * 256:(pg + 1) * 256])
            wv = load_w(up_d[:, :, DFF + pg * 256:DFF + (pg + 1) * 256])
            for j in range(2):
                m = pg * 2 + j
                psg = next_ps()
                psv = next_ps()
                matmul_group(psg, 128, NP, [(wg, j * 128, hb, [(c, c) for c in range(NCH)])])
                matmul_group(psv, 128, NP, [(wv, j * 128, hb, [(c, c) for c in range(NCH)])])
                ag, av = acc[st["ti"] % 2]
                st["ti"] += 1

                def conv(ps_, a, mm_):
                    P.op("scalar", lambda e: e.activation(out=a[:, 0:N], in_=ps_[:, 2:2 + N], func=AF.Identity, scale=cw[:, 128 + mm_:128 + mm_ + 1], bias=cb[:, mm_:mm_ + 1]), [ps_, cw, cb], [a])
                    P.op("vector", lambda e: e.scalar_tensor_tensor(out=a[:, 0:N], in0=ps_[:, 1:1 + N], scalar=cw[:, 64 + mm_:64 + mm_ + 1], in1=a[:, 0:N], op0=ALU.mult, op1=ALU.add), [ps_, cw, a], [a])
                    P.op("vector", lambda e: e.scalar_tensor_tensor(out=a[:, 0:N], in0=ps_[:, 0:N], scalar=cw[:, mm_:mm_ + 1], in1=a[:, 0:N], op0=ALU.mult, op1=ALU.add), [ps_, cw, a], [a])
                conv(psg, ag, m)
                conv(psv, av, 32 + m)
                P.op("scalar", lambda e, ag=ag: e.activation(out=ag[:, 0:N], in_=ag[:, 0:N], func=AF.Gelu_apprx_tanh), [ag], [ag])
                P.op("gpsimd", lambda e, ag=ag, av=av, m=m: e.tensor_tensor(out=act[:, m, 0:N], in0=ag[:, 0:N], in1=av[:, 0:N], op=ALU.mult), [ag, av], [act])
        if stop_after == "up":
            P.op("vector", lambda e: e.tensor_copy(A[:, :, 0:N], act[:, 0:16, 0:N]), [act], [A])
            P.dma(y_d[:, :, n0:n0 + N], A[:, :, 0:N], reads=[A])
            return
        for mp in range(8):
            w0 = load_w(down_d[:, 0:16, mp * 256:(mp + 1) * 256])
            w1 = load_w(down_d[:, 16:32, mp * 256:(mp + 1) * 256])
            for j in range(2):
                m = mp * 2 + j
                ps = next_ps()
                matmul_group(ps, 128, N, [(w0, j * 128, act, [(c, c) for c in range(16)]),
                                          (w1, j * 128, act, [(c, 16 + c) for c in range(16)])])
                if m % 2 == 0:
                    P.op("scalar", lambda e, ps=ps, m=m: e.activation(out=B[:, m, 0:N], in_=ps[:, 0:N], func=AF.Copy), [ps], [B])
                else:
                    P.op("vector", lambda e, ps=ps, m=m: e.tensor_copy(B[:, m, 0:N], ps[:, 0:N]), [ps], [B])
        emit_rstd(P, B, NCH, N, ones_bf, act, ss_ps, rstd, D)
        emit_scale(P, "vector", B, B, gs, 2 * NCH, rstd, NCH, N)
        P.op("gpsimd", lambda e: e.tensor_tensor(out=A[:, :, 0:N], in0=A[:, :, 0:N], in1=B[:, :, 0:N], op=ALU.add), [A, B], [A])
        P.dma(y_d[:, :, n0:n0 + N], A[:, :, 0:N], reads=[A])

    prev_n = [0]
    group(0, 2, True)
    n0 = 0
    for N in split_groups(TOK):
        group(n0, N, False)
        n0 += N
    wait_all_dma(P)
    P.build()
    P.close()
    return nc


WINS = (2, 4, 8, 16)


def build_KC(TOK=4096, NG=512):
    nc = bass.Bass("TRN2", target_bir_lowering=False)
    P = Prog(nc)
    u_d = dram_in(nc, "uT", [1024, TOK], F32).rearrange("(c p) n -> p c n", p=128)
    uh_d = dram_in(nc, "uhT", [1024, 16], F32).rearrange("(c p) n -> p c n", p=128)
    rc_d = dram_in(nc, "rc", [128, 4 * 2 * 16], F32)
    pw_d = dram_in(nc, "pw", [4, 256, 256], BF16).rearrange("g (c p) d -> p g c d", p=128)
    psb_d = dram_in(nc, "psb", [128, 16], F32)
    o_d = dram_out(nc, "boutT", [1024, TOK], BF16).rearrange("(c p) n -> p c n", p=128)
    W = 16 + NG
    U = [P.sb("U%d" % i, [128, 8, W], F32) for i in range(2)]
    S1 = P.sb("S1", [128, 2, W], F32)
    S2 = P.sb("S2", [128, 2, W], F32)
    PB = [P.sb("PB%d" % i, [128, 8, NG], BF16) for i in range(2)]
    OB = [P.sb("OB%d" % i, [128, 8, NG], BF16) for i in range(2)]
    rc = P.sb("rc", [128, 4, 2, 16], F32)
    tmp = P.sb("tmp", [128, 2, 16], F32)
    pw = P.sb("pw", [128, 4, 2, 256], BF16)
    psb = P.sb("psb", [128, 16], F32)
    bs = P.sb("bs", [128, 8], F32)
    pss = [P.ps("pss%d" % i, [128, NG], F32) for i in range(4)]
    P.dma(rc[:].rearrange("p a b c -> p (a b c)"), rc_d, writes=[rc])
    P.dma(psb[:], psb_d, writes=[psb])
    for g in range(4):
        P.dma(pw[:, g, :, :], pw_d[:, g, :, :], writes=[pw])
    P.op("vector", lambda e: e.tensor_tensor(out=bs[:], in0=psb[:, 0:8], in1=psb[:, 8:16], op=ALU.mult), [psb], [bs])
    pi = 0
    for gi in range(TOK // NG):
        n0 = gi * NG
        Ub, PBb, OBb = U[gi % 2], PB[gi % 2], OB[gi % 2]
        if gi == 0:
            P.dma(Ub[:, :, 0:16], uh_d, writes=[Ub])
            P.dma(Ub[:, :, 16:W], u_d[:, :, 0:NG], writes=[Ub])
        else:
            P.dma(Ub[:, :, 0:W], u_d[:, :, n0 - 16:n0 + NG], writes=[Ub])
        for g in range(4):
            ks = slice(2 * g, 2 * g + 2)
            P.op("gpsimd", lambda e, Ub=Ub, ks=ks: e.tensor_tensor(out=S1[:, :, 1:W], in0=Ub[:, ks, 1:W], in1=Ub[:, ks, 0:W - 1], op=ALU.add), [Ub], [S1])
            fin = S1
            if g >= 1:
                P.op("gpsimd", lambda e: e.tensor_tensor(out=S2[:, :, 3:W], in0=S1[:, :, 3:W], in1=S1[:, :, 1:W - 2], op=ALU.add), [S1], [S2])
                fin = S2
            if g >= 2:
                P.op("gpsimd", lambda e: e.tensor_tensor(out=S1[:, :, 7:W], in0=S2[:, :, 7:W], in1=S2[:, :, 3:W - 4], op=ALU.add), [S2], [S1])
                fin = S1
            if g >= 3:
                P.op("gpsimd", lambda e: e.tensor_tensor(out=S2[:, :, 15:W], in0=S1[:, :, 15:W], in1=S1[:, :, 7:W - 8], op=ALU.add), [S1], [S2])
                fin = S2
            P.op("vector", lambda e, fin=fin, Ub=Ub, PBb=PBb, ks=ks, g=g: e.scalar_tensor_tensor(
                out=PBb[:, ks, 0:NG], in0=fin[:, :, 16:W], scalar=1.0 / WINS[g], in1=Ub[:, ks, 16:W], op0=ALU.mult, op1=ALU.subtract), [fin, Ub], [PBb])
            if gi == 0:
                P.op("vector", lambda e, fin=fin, g=g: e.tensor_tensor(out=tmp[:], in0=fin[:, :, 16:32], in1=rc[:, g, :, :], op=ALU.mult), [fin, rc], [tmp])
                P.op("vector", lambda e, Ub=Ub, PBb=PBb, ks=ks: e.tensor_tensor(out=PBb[:, ks, 0:16], in0=tmp[:], in1=Ub[:, ks, 16:32], op=ALU.subtract), [tmp, Ub, PBb], [PBb])
            for dt in range(2):
                ps = pss[pi % 4]
                pi += 1

                def mm(e, ps=ps, g=g, dt=dt, PBb=PBb):
                    e.matmul(ps[:, 0:NG], lhsT=pw[:, g, 0, dt * 128:(dt + 1) * 128], rhs=PBb[:, 2 * g, 0:NG], start=True, stop=False)
                    return e.matmul(ps[:, 0:NG], lhsT=pw[:, g, 1, dt * 128:(dt + 1) * 128], rhs=PBb[:, 2 * g + 1, 0:NG], start=False, stop=True)
                P.op("tensor", mm, [pw, PBb], [ps])
                k = 2 * g + dt
                P.op("scalar", lambda e, ps=ps, k=k, OBb=OBb: e.activation(out=OBb[:, k, 0:NG], in_=ps[:, 0:NG], func=AF.Identity, scale=psb[:, k:k + 1], bias=bs[:, k:k + 1]), [ps, psb, bs], [OBb])
        P.dma(o_d[:, :, n0:n0 + NG], OBb[:, :, 0:NG], reads=[OBb])
    wait_all_dma(P)
    P.build()
    P.close()
    return nc


TOPK = 256
NBIS = 26


def build_KB(T=16384, NSLOT=32):
    nc = bass.Bass("TRN2", target_bir_lowering=False)
    P = Prog(nc)
    NQ = NSLOT * 128
    q_d = dram_in(nc, "qT", [1024, NQ], BF16).rearrange("(h d) n -> d h n", d=128)
    qi_d = dram_in(nc, "qiT", [1024, NQ], BF16).rearrange("(h d) n -> d h n", d=64)
    wi_d = dram_in(nc, "wi", [NQ, 16], F32)
    ki_d = dram_in(nc, "kiT", [64, T], BF16)
    k_d = dram_in(nc, "kT", [1024, T], BF16).rearrange("(h d) n -> d h n", d=128)
    v_d = dram_in(nc, "v", [T, 1024], BF16).rearrange("(t s) n -> s t n", s=128)
    nm_d = dram_in(nc, "negmask", [128, 512], F32)
    id_d = dram_in(nc, "ident", [128, 128], F32)
    o_d = dram_out(nc, "aoutT", [1024, NQ], BF16).rearrange("(h d) n -> d h n", d=128)

    LMAX = 512 * NSLOT
    KI = P.sb("KI", [64, T], BF16)
    score = P.sb("score", [128, LMAX], F32)
    Mq = P.sb("Mq", [128, LMAX], BF16)
    MTc = [P.sb("MTc%d" % i, [128, 4, 128], BF16) for i in range(2)]
    Q = P.sb("Q", [128, 8, 128], BF16)
    QI = P.sb("QI", [64, 16, 128], BF16)
    W = P.sb("W", [128, 16], F32)
    DG = P.sb("DG", [128, 16, 128], BF16)
    negm = P.sb("negm", [128, 512], F32)
    posm = P.sb("posm", [128, 512], F32)
    idf = P.sb("idf", [128, 128], F32)
    ident = P.sb("ident", [128, 128], BF16)
    ones_bf = P.sb("ones_bf", [128, 128], BF16)
    R = [P.sb("R%d" % i, [128, 512], BF16) for i in range(3)]
    tmpm = P.sb("tmpm", [128, 512], F32)
    st8 = P.sb("st8", [128, 8], F32)
    KTc = [P.sb("KTc%d" % i, [128, 8, 512], BF16) for i in range(2)]
    Vc = [P.sb("Vc%d" % i, [128, 4, 1024], BF16) for i in range(2)]
    PT = [P.sb("PT%d" % i, [128, 8, 128], BF16) for i in range(2)]
    rs = P.sb("rs", [128, 8, 128], F32)
    ob = [P.sb("ob%d" % i, [128, 8, 128], BF16) for i in range(2)]
    psA = [P.ps("psA%d" % i, [128, 512], F32) for i in range(2)]
    psB = [P.ps("psB%d" % i, [128, 512], F32) for i in range(2)]
    OT = [P.ps("OT%d" % i, [128, 4, 128], F32) for i in range(2)]
    SUM = [P.ps("SUM%d" % i, [128, 4, 128], F32) for i in range(2)]
    att_scale = 128.0 ** -0.5

    P.dma(KI[:], ki_d, writes=[KI])
    P.dma(negm[:], nm_d, writes=[negm])
    P.dma(idf[:], id_d, writes=[idf])
    P.op("vector", lambda e: e.tensor_copy(ident[:], idf[:]), [idf], [ident])
    P.op("vector", lambda e: e.memset(ones_bf[:], 1.0), [], [ones_bf])
    P.op("vector", lambda e: e.tensor_scalar(posm[:], negm[:], -1.0, None, op0=ALU.mult), [negm], [posm])
    c = {"a": 0, "b": 0, "r": 0, "kv": 0, "pt": 0, "ob": 0, "ev": 0}

    for m in range(NSLOT):
        L = 512 * (m + 1)
        ntile = 4 * (m + 1)
        qs = slice(m * 128, (m + 1) * 128)
        P.dma(Q[:], q_d[:, :, qs], writes=[Q])
        P.dma(QI[:], qi_d[:, :, qs], writes=[QI])
        P.dma(W[:], wi_d[qs, :], writes=[W])

        def mkdg(e):
            ins = None
            for h in range(16):
                ins = e.tensor_scalar(DG[:, h, :], ident[:], W[:, h:h + 1], None, op0=ALU.mult)
            return ins
        P.op("vector", mkdg, [ident, W], [DG])
        kvbuf = {}

        def loads(ch):
            kt, vt = KTc[ch % 2], Vc[ch % 2]
            P.dma(kt[:], k_d[:, :, ch * 512:(ch + 1) * 512], writes=[kt])
            P.dma(vt[:], v_d[:, ch * 4:ch * 4 + 4, :], writes=[vt])
            kvbuf[ch] = (kt, vt)
        loads(0)
        if m >= 1:
            loads(1)
        items = [(ch, h) for ch in range(m + 1) for h in range(16)]
        scb = {}
        rbuf = {}

        def emit_lg(i):
            ch, h = items[i]
            if h == 0:
                scb[ch] = psB[c["b"] % 2]
                c["b"] += 1
            lg = psA[c["a"] % 2]
            c["a"] += 1
            r = R[c["r"] % 3]
            c["r"] += 1
            rbuf[i] = r
            P.op("tensor", lambda e, lg=lg, h=h, ch=ch: e.matmul(lg[:, :], lhsT=QI[:, h, :], rhs=KI[:, ch * 512:(ch + 1) * 512], start=True, stop=True), [QI, KI], [lg])
            if h % 2 == 0:
                P.op("scalar", lambda e, lg=lg, r=r: e.activation(out=r[:], in_=lg[:, :], func=AF.Relu), [lg], [r])
            else:
                P.op("vector", lambda e, lg=lg, r=r: e.tensor_scalar(r[:], lg[:, :], 0.0, None, op0=ALU.max), [lg], [r])

        def emit_sc(i):
            ch, h = items[i]
            sc_ps = scb[ch]
            r = rbuf.pop(i)
            P.op("tensor", lambda e, sc_ps=sc_ps, h=h, r=r: e.matmul(sc_ps[:, :], lhsT=DG[:, h, :], rhs=r[:], start=(h == 0), stop=(h == 15)), [DG, r], [sc_ps])
            if h == 15:
                if ch < m:
                    P.op("scalar", lambda e, sc_ps=sc_ps, ch=ch: e.activation(out=score[:, ch * 512:(ch + 1) * 512], in_=sc_ps[:, :], func=AF.Copy), [sc_ps], [score])
                else:
                    P.op("vector", lambda e, sc_ps=sc_ps: e.tensor_tensor(out=tmpm[:], in0=sc_ps[:, :], in1=posm[:], op=ALU.add), [sc_ps, posm], [tmpm])
                    P.op("vector", lambda e, sc_ps=sc_ps, ch=ch: e.tensor_tensor(out=score[:, ch * 512:(ch + 1) * 512], in0=sc_ps[:, :], in1=negm[:], op=ALU.add), [sc_ps, negm], [score])
        for i in range(len(items)):
            emit_lg(i)
            if i >= 1:
                emit_sc(i - 1)
        emit_sc(len(items) - 1)
        P.op("vector", lambda e, L=L: e.tensor_reduce(out=st8[:, 0:1], in_=score[:, 0:L], axis=AX.X, op=ALU.max), [score], [st8])
        P.op("vector", lambda e: e.tensor_reduce(out=st8[:, 1:2], in_=tmpm[:], axis=AX.X, op=ALU.min), [tmpm], [st8])
        if m > 0:
            P.op("vector", lambda e, L=L: e.tensor_reduce(out=st8[:, 2:3], in_=score[:, 0:L - 512], axis=AX.X, op=ALU.min), [score], [st8])
            P.op("vector", lambda e: e.tensor_tensor(out=st8[:, 3:4], in0=st8[:, 1:2], in1=st8[:, 2:3], op=ALU.min), [st8], [st8])
        else:
            P.op("vector", lambda e: e.tensor_copy(st8[:, 3:4], st8[:, 1:2]), [st8], [st8])
        P.op("vector", lambda e: e.tensor_tensor(out=st8[:, 4:5], in0=st8[:, 0:1], in1=st8[:, 3:4], op=ALU.subtract), [st8], [st8])
        for it in range(NBIS):
            P.op("vector", lambda e: e.tensor_scalar(st8[:, 4:5], st8[:, 4:5], 0.5, None, op0=ALU.mult), [st8], [st8])
            P.op("vector", lambda e: e.tensor_tensor(out=st8[:, 5:6], in0=st8[:, 3:4], in1=st8[:, 4:5], op=ALU.add), [st8], [st8])
            P.op("vector", lambda e, L=L: e.tensor_scalar(Mq[:, 0:L], score[:, 0:L], st8[:, 5:6], 0.0, op0=ALU.is_ge, op1=ALU.add, accum_out=st8[:, 6:7]), [score, st8], [Mq, st8])
            P.op("vector", lambda e: e.scalar_tensor_tensor(out=st8[:, 7:8], in0=st8[:, 6:7], scalar=float(TOPK), in1=st8[:, 4:5], op0=ALU.is_ge, op1=ALU.mult), [st8], [st8])
            P.op("vector", lambda e: e.tensor_tensor(out=st8[:, 3:4], in0=st8[:, 3:4], in1=st8[:, 7:8], op=ALU.add), [st8], [st8])
        P.op("vector", lambda e, L=L: e.tensor_scalar(Mq[:, 0:L], score[:, 0:L], st8[:, 3:4], None, op0=ALU.is_ge), [score, st8], [Mq])
        tiles = [(ch, tt) for ch in range(m + 1) for tt in range(4)]
        mtb = {}
        ptb = {}

        def transposes(ch):
            MT = MTc[ch % 2]
            pb = psB[c["b"] % 2]
            c["b"] += 1
            psT = pb[:, :].bitcast(BF16)

            def tr(e, ch=ch, psT=psT):
                ins = None
                for k in range(4):
                    t_ = ch * 4 + k
                    ins = e.transpose(psT[:, k * 128:(k + 1) * 128], Mq[:, t_ * 128:(t_ + 1) * 128], ident[:])
                return ins
            P.op("tensor", tr, [Mq, ident], [pb])
            if ch % 2 == 0:
                P.op("scalar", lambda e, MT=MT, psT=psT: e.activation(out=MT[:], in_=psT[:, 0:512].rearrange("p (a b) -> p a b", b=128), func=AF.Copy), [pb], [MT])
            else:
                P.op("vector", lambda e, MT=MT, psT=psT: e.tensor_copy(MT[:], psT[:, 0:512].rearrange("p (a b) -> p a b", b=128)), [pb], [MT])
            mtb[ch] = MT

        def qk_exp(i, hg):
            ch, tt = tiles[i]
            kt, vt = kvbuf[ch]
            if hg == 0:
                ptb[i] = PT[c["pt"] % 2]
                c["pt"] += 1
            pt = ptb[i]
            st = psA[c["a"] % 2]
            c["a"] += 1

            def mmqk(e, st=st, hg=hg, kt=kt, tt=tt):
                ins = None
                for hh in range(4):
                    h = hg * 4 + hh
                    ins = e.matmul(st[:, hh * 128:(hh + 1) * 128], lhsT=kt[:, h, tt * 128:(tt + 1) * 128], rhs=Q[:, h, :], start=True, stop=True)
                return ins
            P.op("tensor", mmqk, [kt, Q], [st])
            P.op("scalar", lambda e, st=st, pt=pt, hg=hg: e.activation(out=pt[:, hg * 4:hg * 4 + 4, :], in_=st[:, :].rearrange("p (a b) -> p a b", b=128), func=AF.Exp, scale=att_scale), [st], [pt])

        def maskmul(i):
            ch, tt = tiles[i]
            pt = ptb[i]
            MT = mtb[ch]
            P.op("gpsimd", lambda e, pt=pt, tt=tt, MT=MT: e.tensor_tensor(out=pt[:], in0=pt[:], in1=MT[:, tt:tt + 1, :].to_broadcast([128, 8, 128]), op=ALU.mult), [pt, MT], [pt])

        def pv(i):
            ch, tt = tiles[i]
            kt, vt = kvbuf[ch]
            pt = ptb.pop(i)
            t = ch * 4 + tt

            def mmpv(e, pt=pt, vt=vt, tt=tt, t=t, ntile=ntile):
                ins = None
                for h in range(8):
                    ins = e.matmul(OT[h // 4][:, h % 4, :], lhsT=vt[:, tt, h * 128:(h + 1) * 128], rhs=pt[:, h, :], start=(t == 0 and h % 4 == 0), stop=(t == ntile - 1 and h % 4 == 3))
                for hg in range(2):
                    ins = e.matmul(SUM[hg][:].rearrange("p a b -> p (a b)"), lhsT=ones_bf[:], rhs=pt[:, hg * 4:hg * 4 + 4, :].rearrange("p a b -> p (a b)"),
                                   start=(t == 0), stop=(t == ntile - 1))
                return ins
            P.op("tensor", mmpv, [vt, pt, ones_bf], [OT[0], OT[1], SUM[0], SUM[1]])

        nt = len(tiles)
        transposes(0)
        qk_exp(0, 0)
        qk_exp(0, 1)
        maskmul(0)
        for i in range(nt):
            if i + 1 < nt:
                if tiles[i + 1][1] == 0:
                    transposes(tiles[i + 1][0])
                qk_exp(i + 1, 0)
            pv(i)
            if tiles[i][1] == 3 and tiles[i][0] + 2 <= m:
                loads(tiles[i][0] + 2)
            if i + 1 < nt:
                qk_exp(i + 1, 1)
                maskmul(i + 1)
        o = ob[c["ob"] % 2]
        c["ob"] += 1
        for hg in range(2):
            P.op("vector", lambda e, hg=hg: e.reciprocal(rs[:, hg * 4:hg * 4 + 4, :], SUM[hg][:]), [SUM[hg]], [rs])
            P.op("vector", lambda e, hg=hg, o=o: e.tensor_tensor(out=o[:, hg * 4:hg * 4 + 4, :], in0=OT[hg][:], in1=rs[:, hg * 4:hg * 4 + 4, :], op=ALU.mult), [OT[hg], rs], [o])
        P.dma(o_d[:, :, qs], o[:], reads=[o])
    wait_all_dma(P)
    P.build()
    P.close()
    return nc


import math

PI = math.pi


def emit_mla(P, nc, T, NB, ones_bf, pj):
    TT = T * NB
    NG = 512
    r3 = lambda ap: ap.rearrange("(c p) n -> p c n", p=128)
    cq_d = r3(dram_in(nc, "cqT", [512, TT], F32))
    ckv_d = r3(dram_in(nc, "ckvT", [256, TT], F32))
    kr_d = dram_in(nc, "krT", [64, TT], F32)
    krs_d = dram_in(nc, "krsT", [64, TT], F32)
    pos_d = dram_in(nc, "posrep", [64, TT], I32)
    fq_d = dram_in(nc, "fq", [64, 2], F32)
    wq_d = r3(dram_in(nc, "wq", [512, 256], BF16))
    wkv_d = r3(dram_in(nc, "wkv", [256, 256], BF16))
    g_d = dram_in(nc, "mlag", [128, 6], F32)
    mask_d = dram_in(nc, "mask", [128, 4 * NG], BF16)
    co_d = dram_out(nc, "coutT", [128, TT], BF16)

    KT = P.sb("KT", [128, T], BF16)
    KRT = P.sb("KRT", [64, T], BF16)
    V = P.sb("V", [128, T // 128, 128], BF16)
    mask = P.sb("mask", [128, 4, NG], BF16)
    wq = P.sb("wq", [128, 4, 256], BF16)
    wkv = P.sb("wkv", [128, 2, 256], BF16)
    gg = P.sb("mlag", [128, 6], F32)
    fq = P.sb("fq", [64, 2], F32)
    ckv = P.sb("ckv", [128, 2, NG], F32)
    cq = P.sb("cq", [128, 4, NG], F32)
    sq = P.sb("sq", [128, 4, NG], BF16)
    cqn = P.sb("cqn", [128, 4, NG], BF16)
    ckvn = P.sb("ckvn", [128, 2, NG], BF16)
    rstd = P.sb("rstd", [128, NG], F32)
    QN = P.sb("QN", [128, NG], BF16)
    QR = P.sb("QR", [64, NG], BF16)
    posi = P.sb("posi", [64, NG], I32)
    ang = P.sb("ang", [64, NG], F32)
    cos = P.sb("cos", [64, NG], F32)
    sin = P.sb("sin", [64, NG], F32)
    kr = P.sb("kr", [64, NG], F32)
    krs = P.sb("krs", [64, NG], F32)
    t1 = P.sb("t1", [64, NG], F32)
    t2 = P.sb("t2", [64, NG], F32)
    PT = [P.sb("PT%d" % i, [128, NG], BF16) for i in range(3)]
    rs = P.sb("rs", [128, NG], F32)
    ob = [P.sb("ob%d" % i, [128, NG], BF16) for i in range(2)]
    ss_ps = P.ps("ss_ps", [128, NG], F32)
    STp = [P.ps("ST%d" % i, [128, NG], F32) for i in range(2)]
    OT = P.ps("OT", [128, NG], F32)
    SUM = P.ps("SUM", [128, NG], F32)
    scale = 192.0 ** -0.5

    P.dma(mask[:].rearrange("p a b -> p (a b)"), mask_d, writes=[mask])
    P.dma(wq[:], wq_d, writes=[wq])
    P.dma(wkv[:], wkv_d, writes=[wkv])
    P.dma(gg[:], g_d, writes=[gg])
    P.dma(fq[:], fq_d, writes=[fq])
    cnt = {"pj": 0, "st": 0, "pt": 0, "ob": 0}

    def nextpj():
        b = pj[cnt["pj"] % 2]
        cnt["pj"] += 1
        return b

    def proj(ps, M, w, woff, src, nchunk, N=NG):
        def mm(e):
            ins = None
            for c in range(nchunk):
                ins = e.matmul(ps[0:M, 0:N], lhsT=w[:, c, woff:woff + M], rhs=src[:, c, 0:N], start=(c == 0), stop=(c == nchunk - 1))
            return ins
        P.op("tensor", mm, [w, src], [ps])

    for b in range(NB):
        for g in range(T // NG):
            c0 = b * T + g * NG
            P.dma(ckv[:], ckv_d[:, :, c0:c0 + NG], writes=[ckv])
            P.dma(kr[:], kr_d[:, c0:c0 + NG], writes=[kr])
            P.dma(krs[:], krs_d[:, c0:c0 + NG], writes=[krs])
            P.dma(posi[:], pos_d[:, c0:c0 + NG], writes=[posi])
            P.dma(cq[:], cq_d[:, :, c0:c0 + NG], writes=[cq])
            emit_rstd(P, ckv, 2, NG, ones_bf, sq, ss_ps, rstd, 256)

            def sc_kv(e):
                ins = None
                for c in range(2):
                    ins = e.scalar_tensor_tensor(out=ckvn[:, c, :], in0=ckv[:, c, :], scalar=gg[:, 4 + c:5 + c], in1=rstd[:], op0=ALU.mult, op1=ALU.mult)
                return ins
            P.op("vector", sc_kv, [ckv, gg, rstd], [ckvn])
            ps = nextpj()
            proj(ps, 128, wkv, 0, ckvn, 2)
            P.op("scalar", lambda e, ps=ps, g=g: e.activation(out=KT[:, g * NG:(g + 1) * NG], in_=ps[:, 0:NG], func=AF.Copy), [ps], [KT])
            ps = nextpj()

            def mmv(e, ps=ps):
                ins = None
                for st_ in range(4):
                    for c in range(2):
                        ins = e.matmul(ps[:, st_ * 128:(st_ + 1) * 128], lhsT=ckvn[:, c, st_ * 128:(st_ + 1) * 128], rhs=wkv[:, c, 128:256], start=(c == 0), stop=(c == 1))
                return ins
            P.op("tensor", mmv, [ckvn, wkv], [ps])
            P.op("vector", lambda e, ps=ps, g=g: e.tensor_copy(V[:, 4 * g:4 * g + 4, :], ps[:, 0:NG].rearrange("p (a b) -> p a b", b=128)), [ps], [V])
            P.op("vector", lambda e: e.tensor_copy(ang[:], posi[:]), [posi], [ang])
            P.op("vector", lambda e: e.tensor_scalar(ang[:], ang[:], fq[:, 0:1], None, op0=ALU.mult), [ang, fq], [ang])
            P.op("vector", lambda e: e.tensor_scalar(t1[:], ang[:], 1.0 / (2 * PI), None, op0=ALU.mult), [ang], [t1])
            P.op("vector", lambda e: e.tensor_copy(posi[:], t1[:]), [t1], [posi])
            P.op("vector", lambda e: e.tensor_copy(t1[:], posi[:]), [posi], [t1])
            P.op("vector", lambda e: e.scalar_tensor_tensor(out=sin[:], in0=t1[:], scalar=-2 * PI, in1=ang[:], op0=ALU.mult, op1=ALU.add), [t1, ang], [sin])
            P.op("vector", lambda e: e.tensor_single_scalar(t1[:], sin[:], PI, op=ALU.is_gt), [sin], [t1])
            P.op("vector", lambda e: e.scalar_tensor_tensor(out=sin[:], in0=t1[:], scalar=-2 * PI, in1=sin[:], op0=ALU.mult, op1=ALU.add), [t1, sin], [sin])
            P.op("vector", lambda e: e.tensor_single_scalar(t1[:], sin[:], -PI, op=ALU.is_lt), [sin], [t1])
            P.op("vector", lambda e: e.scalar_tensor_tensor(out=sin[:], in0=t1[:], scalar=2 * PI, in1=sin[:], op0=ALU.mult, op1=ALU.add), [t1, sin], [sin])
            P.op("vector", lambda e: e.tensor_scalar(cos[:], sin[:], 0.5 * PI, None, op0=ALU.add), [sin], [cos])
            P.op("vector", lambda e: e.tensor_single_scalar(t1[:], cos[:], PI, op=ALU.is_gt), [cos], [t1])
            P.op("vector", lambda e: e.scalar_tensor_tensor(out=cos[:], in0=t1[:], scalar=-2 * PI, in1=cos[:], op0=ALU.mult, op1=ALU.add), [t1, cos], [cos])
            P.op("scalar", lambda e: e.activation(out=sin[:], in_=sin[:], func=AF.Sin), [sin], [sin])
            P.op("scalar", lambda e: e.activation(out=cos[:], in_=cos[:], func=AF.Sin), [cos], [cos])
            P.op("vector", lambda e: e.tensor_tensor(out=t1[:], in0=kr[:], in1=cos[:], op=ALU.mult), [kr, cos], [t1])
            P.op("vector", lambda e: e.scalar_tensor_tensor(out=t2[:], in0=krs[:], scalar=fq[:, 1:2], in1=sin[:], op0=ALU.mult, op1=ALU.mult), [krs, fq, sin], [t2])
            P.op("vector", lambda e, g=g: e.tensor_tensor(out=KRT[:, g * NG:(g + 1) * NG], in0=t1[:], in1=t2[:], op=ALU.add), [t1, t2], [KRT])
            emit_rstd(P, cq, 4, NG, ones_bf, sq, ss_ps, rstd, 512)

            def sc_q(e):
                ins = None
                for c in range(4):
                    ins = e.scalar_tensor_tensor(out=cqn[:, c, :], in0=cq[:, c, :], scalar=gg[:, c:c + 1], in1=rstd[:], op0=ALU.mult, op1=ALU.mult)
                return ins
            P.op("vector", sc_q, [cq, gg, rstd], [cqn])
            ps = nextpj()
            proj(ps, 128, wq, 0, cqn, 4)
            P.op("scalar", lambda e, ps=ps: e.activation(out=QN[:], in_=ps[:, 0:NG], func=AF.Copy, scale=scale), [ps], [QN])
            psa = nextpj()
            proj(psa, 64, wq, 128, cqn, 4)
            P.op("vector", lambda e, psa=psa: e.tensor_tensor(out=t1[:], in0=psa[0:64, 0:NG], in1=cos[:], op=ALU.mult), [psa, cos], [t1])
            psb = nextpj()
            proj(psb, 64, wq, 192, cqn, 4)
            P.op("vector", lambda e, psb=psb: e.scalar_tensor_tensor(out=t2[:], in0=psb[0:64, 0:NG], scalar=fq[:, 1:2], in1=sin[:], op0=ALU.mult, op1=ALU.mult), [psb, fq, sin], [t2])
            P.op("vector", lambda e: e.tensor_tensor(out=t1[:], in0=t1[:], in1=t2[:], op=ALU.add), [t1, t2], [t1])
            P.op("scalar", lambda e: e.activation(out=QR[:], in_=t1[:], func=AF.Copy, scale=scale), [t1], [QR])
            nj = 4 * (g + 1)
            for j in range(nj):
                st = STp[cnt["st"] % 2]
                cnt["st"] += 1
                pt = PT[cnt["pt"] % 3]
                cnt["pt"] += 1

                def mms(e, st=st, j=j):
                    e.matmul(st[:, 0:NG], lhsT=KT[:, j * 128:(j + 1) * 128], rhs=QN[:], start=True, stop=False)
                    return e.matmul(st[:, 0:NG], lhsT=KRT[:, j * 128:(j + 1) * 128], rhs=QR[:], start=False, stop=True)
                P.op("tensor", mms, [KT, KRT, QN, QR], [st])
                P.op("scalar", lambda e, st=st, pt=pt: e.activation(out=pt[:], in_=st[:, 0:NG], func=AF.Exp), [st], [pt])
                if j >= 4 * g:
                    P.op("gpsimd", lambda e, pt=pt, jj=j - 4 * g: e.tensor_tensor(out=pt[:], in0=pt[:], in1=mask[:, jj, :], op=ALU.mult), [pt, mask], [pt])

                def mmo(e, pt=pt, j=j, nj=nj):
                    e.matmul(OT[:, 0:NG], lhsT=V[:, j, :], rhs=pt[:], start=(j == 0), stop=(j == nj - 1))
                    return e.matmul(SUM[:, 0:NG], lhsT=ones_bf[:], rhs=pt[:], start=(j == 0), stop=(j == nj - 1))
                P.op("tensor", mmo, [V, pt, ones_bf], [OT, SUM])
            o = ob[cnt["ob"] % 2]
            cnt["ob"] += 1
            P.op("vector", lambda e: e.reciprocal(rs[:], SUM[:, 0:NG]), [SUM], [rs])
            P.op("vector", lambda e, o=o: e.tensor_tensor(out=o[:], in0=OT[:, 0:NG], in1=rs[:], op=ALU.mult), [OT, rs], [o])
            P.dma(co_d[:, c0:c0 + NG], o[:], reads=[o])


def emit_lru(P, nc, T, NB, pj, chunk=512):
    TT = T * NB
    NG = chunk
    xl_d = dram_in(nc, "xlT", [128, TT], F32)
    yg_d = dram_in(nc, "ygT", [128, TT], F32)
    wa_d = dram_in(nc, "lwa", [128, 128], BF16)
    wx_d = dram_in(nc, "lwx", [128, 128], BF16)
    lp_d = dram_in(nc, "lrup", [128, 8], F32)
    do_d = dram_out(nc, "doutT", [128, TT], BF16)
    wa = P.sb("lwa", [128, 128], BF16)
    wx = P.sb("lwx", [128, 128], BF16)
    lp = P.sb("lrup", [128, 8], F32)
    sp = P.sb("lsp", [128, 2], F32)
    X = [P.sb("lX%d" % i, [128, 3 + NG], F32) for i in range(2)]
    Y = [P.sb("lY%d" % i, [128, NG], F32) for i in range(2)]
    xc = P.sb("lxc", [128, NG], F32)
    xcb = P.sb("lxcb", [128, NG], BF16)
    gr = P.sb("lgr", [128, NG], F32)
    gi_ = P.sb("lgi", [128, NG], F32)
    a = P.sb("la", [128, NG], F32)
    mu = P.sb("lmu", [128, NG], F32)
    H = [P.sb("lH%d" % i, [128, NG], F32) for i in range(2)]
    do = [P.sb("ldo%d" % i, [128, NG], BF16) for i in range(2)]
    pa, px = pj
    P.dma(wa[:], wa_d, writes=[wa])
    P.dma(wx[:], wx_d, writes=[wx])
    P.dma(lp[:], lp_d, writes=[lp])
    P.op("scalar", lambda e: e.activation(out=sp[:, 0:1], in_=lp[:, 7:8], func=AF.Exp, scale=-1.0), [lp], [sp])
    P.op("scalar", lambda e: e.activation(out=sp[:, 0:1], in_=sp[:, 0:1], func=AF.Ln, bias=1.0), [sp], [sp])
    P.op("vector", lambda e: e.tensor_scalar(sp[:, 1:2], sp[:, 0:1], -8.0, None, op0=ALU.mult), [sp], [sp])
    it = 0
    for b in range(NB):
        for ci in range(T // NG):
            c0 = b * T + ci * NG
            Xb, Yb, Hb, dob = X[it % 2], Y[it % 2], H[it % 2], do[it % 2]
            Hprev = H[(it + 1) % 2]
            it += 1
            if ci == 0:
                P.op("gpsimd", lambda e, Xb=Xb: e.memset(Xb[:, 0:3], 0.0), [], [Xb])
                P.dma(Xb[:, 3:3 + NG], xl_d[:, c0:c0 + NG], writes=[Xb])
            else:
                P.dma(Xb[:, 0:3 + NG], xl_d[:, c0 - 3:c0 + NG], writes=[Xb])
            P.dma(Yb[:], yg_d[:, c0:c0 + NG], writes=[Yb])
            P.op("vector", lambda e, Xb=Xb: e.tensor_scalar(xc[:], Xb[:, 3:3 + NG], lp[:, 3:4], lp[:, 4:5], op0=ALU.mult, op1=ALU.add), [Xb, lp], [xc])
            for k in range(3):
                P.op("vector", lambda e, Xb=Xb, k=k: e.scalar_tensor_tensor(out=xc[:], in0=Xb[:, k:k + NG], scalar=lp[:, k:k + 1], in1=xc[:], op0=ALU.mult, op1=ALU.add), [Xb, lp, xc], [xc])
            P.op("gpsimd", lambda e: e.tensor_copy(xcb[:], xc[:]), [xc], [xcb])
            P.op("tensor", lambda e: e.matmul(pa[:], lhsT=wa[:], rhs=xcb[:], start=True, stop=True), [wa, xcb], [pa])
            P.op("tensor", lambda e: e.matmul(px[:], lhsT=wx[:], rhs=xcb[:], start=True, stop=True), [wx, xcb], [px])
            P.op("scalar", lambda e: e.activation(out=gr[:], in_=pa[:], func=AF.Sigmoid, bias=lp[:, 5:6]), [pa, lp], [gr])
            P.op("scalar", lambda e: e.activation(out=gi_[:], in_=px[:], func=AF.Sigmoid, bias=lp[:, 6:7]), [px, lp], [gi_])
            P.op("scalar", lambda e: e.activation(out=a[:], in_=gr[:], func=AF.Exp, scale=sp[:, 1:2]), [gr, sp], [a])
            P.op("vector", lambda e: e.tensor_tensor(out=mu[:], in0=a[:], in1=a[:], op=ALU.mult), [a], [mu])
            P.op("vector", lambda e: e.tensor_scalar(mu[:], mu[:], -1.0, 1.0, op0=ALU.mult, op1=ALU.add), [mu], [mu])
            P.op("scalar", lambda e: e.activation(out=mu[:], in_=mu[:], func=AF.Sqrt), [mu], [mu])
            P.op("gpsimd", lambda e: e.tensor_tensor(out=gi_[:], in0=gi_[:], in1=xc[:], op=ALU.mult), [gi_, xc], [gi_])
            P.op("gpsimd", lambda e: e.tensor_tensor(out=mu[:], in0=mu[:], in1=gi_[:], op=ALU.mult), [mu, gi_], [mu])
            if ci == 0:
                P.op("vector", lambda e, Hb=Hb: e.tensor_tensor_scan(Hb[:], a[:], mu[:], 0.0, op0=ALU.mult, op1=ALU.add), [a, mu], [Hb])
            else:
                P.op("vector", lambda e, Hb=Hb, Hprev=Hprev: e.tensor_tensor_scan(Hb[:], a[:], mu[:], Hprev[:, NG - 1:NG], op0=ALU.mult, op1=ALU.add), [a, mu, Hprev], [Hb])
            P.op("scalar", lambda e, Yb=Yb: e.activation(out=Yb[:], in_=Yb[:], func=AF.Gelu_apprx_tanh), [Yb], [Yb])
            P.op("gpsimd", lambda e, Hb=Hb, Yb=Yb, dob=dob: e.tensor_tensor(out=dob[:], in0=Hb[:], in1=Yb[:], op=ALU.mult), [Hb, Yb], [dob])
            P.dma(do_d[:, c0:c0 + NG], dob[:], reads=[dob])


def build_KD(T=16384, NB=2, do_mla=True, do_lru=True):
    nc = bass.Bass("TRN2", target_bir_lowering=False)
    P = Prog(nc)
    ones_d = dram_in(nc, "ones", [128, 128], F32)
    ones_f = P.sb("ones_f", [128, 128], F32)
    ones_bf = P.sb("ones_bf", [128, 128], BF16)
    P.dma(ones_f[:], ones_d, writes=[ones_f])
    P.op("vector", lambda e: e.tensor_copy(ones_bf[:], ones_f[:]), [ones_f], [ones_bf])
    pj = [P.ps("pj%d" % i, [128, 512], F32) for i in range(2)]
    if do_lru:
        emit_lru(P, nc, T, NB, pj)
    if do_mla:
        emit_mla(P, nc, T, NB, ones_bf, pj)
    wait_all_dma(P)
    P.build()
    P.close()
    return nc


def build_KW(L, CH=4096):
    nc = bass.Bass("TRN2", target_bir_lowering=False)
    P = Prog(nc)
    x_d = dram_in(nc, "wf", [128, L], F32)
    o_d = dram_out(nc, "wb", [128, L], BF16)
    xin = [P.sb("xin%d" % i, [128, CH], F32) for i in range(3)]
    xo = [P.sb("xo%d" % i, [128, CH], BF16) for i in range(3)]
    import os
    engs = os.environ.get("KW_ENGS", "vector,scalar,gpsimd").split(",")
    n0 = 0
    i = 0
    while n0 < L:
        n = min(CH, L - n0)
        a, b = xin[i % 3], xo[i % 3]
        P.dma(a[:, 0:n], x_d[:, n0:n0 + n], writes=[a])
        eng = engs[i % len(engs)]
        if eng == "scalar":
            P.op(eng, lambda e, a=a, b=b, n=n: e.activation(out=b[:, 0:n], in_=a[:, 0:n], func=AF.Copy), [a], [b])
        else:
            P.op(eng, lambda e, a=a, b=b, n=n: e.tensor_copy(b[:, 0:n], a[:, 0:n]), [a], [b])
        P.dma(o_d[:, n0:n0 + n], b[:, 0:n], reads=[b])
        n0 += n
        i += 1
    wait_all_dma(P)
    P.build()
    P.close()
    return nc


import ml_dtypes
from concourse.bass_utils import run_bass_kernel_spmd

NCORES = 8
BATCH, SEQ, DM = 2, 16384, 2048
TPC = SEQ // 4
_NP_BF16 = ml_dtypes.bfloat16
_cache = {}


def _run(name, builder, in_maps):
    if name not in _cache:
        _cache[name] = builder()
    nc = _cache[name]
    res = run_bass_kernel_spmd(nc, in_maps, core_ids=list(range(NCORES)))
    return res.results


def _pc(a):
    return np.ascontiguousarray(a)


def _g16(g):
    return _pc(g.reshape(-1, 128).T)


def _cast_weights(ws):
    names = list(ws.keys())
    flat = np.concatenate([ws[k].reshape(-1) for k in names])
    n = flat.size
    per = NCORES * 128
    L = -(-n // per)
    L = -(-L // 8) * 8
    pad = np.zeros(per * L, np.float32)
    pad[:n] = flat
    pad = pad.reshape(NCORES, 128, L)
    res = _run("KW%d" % L, lambda: build_KW(L), [{"wf": pad[c]} for c in range(NCORES)])
    out = np.concatenate([np.asarray(r["wb"]).reshape(-1) for r in res])
    outd = {}
    o = 0
    for k in names:
        sz = ws[k].size
        outd[k] = out[o:o + sz].reshape(ws[k].shape)
        o += sz
    return outd


def _mla_mask():
    m = np.zeros((128, 4, 512), np.float32)
    sl = np.arange(128)[:, None]
    ql = np.arange(512)[None, :]
    for j in range(4):
        m[:, j, :] = (128 * j + sl < (ql // 64 + 1) * 64)
    return m.reshape(128, 2048).astype(_NP_BF16)


def _ke_layer(xT_full, catT_full, L, gpost, gpre, gfpost, w_out, up, down, conv_w, conv_b):
    gs = _pc(np.concatenate([_g16(gpost), _g16(gpre), _g16(gfpost)], axis=1))
    cwm = _pc(conv_w.reshape(3, 64, 128).transpose(2, 0, 1).reshape(128, 192))
    cbm = _pc(conv_b.reshape(64, 128).T)
    ones = np.ones((128, 128), np.float32)
    ims = []
    for c in range(NCORES):
        b, j = divmod(c, 4)
        t0 = j * TPC
        xs = xT_full[b]
        cs = catT_full[b]
        if j == 0:
            xh = np.zeros((DM, 2), np.float32)
            ch = np.zeros((DM, 2), _NP_BF16)
        else:
            xh = _pc(xs[:, t0 - 2:t0])
            ch = _pc(cs[:, t0 - 2:t0])
        ims.append({"xT": _pc(xs[:, t0:t0 + TPC]), "catT": _pc(cs[:, t0:t0 + TPC]), "xhT": xh, "cathT": ch,
                    "gs": gs, "w_out": w_out, "up": up, "down": down, "cw": cwm, "cb": cbm, "ones": ones})
    res = _run("KE", lambda: build_KE(TOK=TPC), ims)
    out = []
    for b in range(BATCH):
        out.append(np.concatenate([np.asarray(res[b * 4 + j]["yT"]) for j in range(4)], axis=1))
    return out


def _ka_layer(xT_full, g, W, tiles, nout, outs, key):
    ones = np.ones((128, 128), np.float32)
    ims = []
    for c in range(NCORES):
        b, j = divmod(c, 4)
        ims.append({"xT": _pc(xT_full[b][:, j * TPC:(j + 1) * TPC]), "g": _g16(g), "W": W, "ones": ones})
    res = _run(key, lambda: build_KA(tiles, nout, outs, TOK=TPC, TOKB=1024), ims)
    z = {}
    for k in outs:
        z[k] = [np.concatenate([np.asarray(res[b * 4 + j][k]) for j in range(4)], axis=1) for b in range(BATCH)]
    return z


def kernel(x, positions, mix_pre_g, mix_post_g, ffn_pre_g, ffn_post_g,
           ffn_up, ffn_conv_w, ffn_conv_b, ffn_down,
           ab_w_in, pool_w, pool_b, pool_scale, ab_w_out,
           cd_w_in, q_norm_g, w_q_up, kv_norm_g, w_kv_up,
           lru_conv_w, lru_conv_b, lru_wa, lru_ba, lru_wx, lru_bx, lru_lambda, cd_w_out):
    f32 = lambda a: np.asarray(a, dtype=np.float32)
    x = f32(x)
    positions = np.asarray(positions).astype(np.int32)
    wb = _cast_weights({"ffn_up": f32(ffn_up), "ffn_down": f32(ffn_down), "ab_w_in": f32(ab_w_in), "ab_w_out": f32(ab_w_out),
                        "cd_w_in": f32(cd_w_in), "cd_w_out": f32(cd_w_out), "w_q_up": f32(w_q_up), "w_kv_up": f32(w_kv_up),
                        "pool_w": f32(pool_w), "lru_wa": f32(lru_wa), "lru_wx": f32(lru_wx)})
    xT = [_pc(x[b].T) for b in range(BATCH)]
    z = _ka_layer(xT, f32(mix_pre_g)[0], _pc(wb["ab_w_in"][0]), AB_TILES, 5200, AB_OUTS, "KA0")
    ident = np.eye(128, dtype=np.float32)
    ims = []
    qidx_all = []
    for c in range(NCORES):
        b, j = divmod(c, 4)
        qidx = np.concatenate([np.arange((4 * m + j) * 128, (4 * m + j + 1) * 128) for m in range(32)])
        qidx_all.append(qidx)
        ql = np.arange(128)[:, None]
        sl = np.arange(512)[None, :]
        negmask = np.where(sl < 128 * j + 64 * (ql // 64 + 1), 0.0, -1e30).astype(np.float32)
        ims.append({"qT": _pc(z["qT"][b][:, qidx]), "qiT": _pc(z["qiT"][b][:, qidx]), "wi": _pc(z["wiT"][b][:, qidx].T),
                    "kiT": z["kiT"][b], "kT": z["kT"][b], "v": _pc(z["vT"][b].T), "negmask": negmask, "ident": ident})
    res = _run("KB", lambda: build_KB(T=SEQ, NSLOT=32), ims)
    aoutT = [np.zeros((1024, SEQ), _NP_BF16) for _ in range(BATCH)]
    for c in range(NCORES):
        b, j = divmod(c, 4)
        aoutT[b][:, qidx_all[c]] = np.asarray(res[c]["aoutT"])
    del ims
    psb = _pc(np.concatenate([f32(pool_scale)[0].reshape(8, 128).T, f32(pool_b)[0].reshape(8, 128).T], axis=1))
    ims = []
    for c in range(NCORES):
        b, j = divmod(c, 4)
        t0 = j * TPC
        u = z["uT"][b]
        rcv = np.zeros((4, 16), np.float32)
        for g_, w_ in enumerate((2, 4, 8, 16)):
            rcv[g_] = 1.0 / (np.minimum(np.arange(16) + 1, w_) if j == 0 else w_)
        rc = _pc(np.broadcast_to(rcv[None, :, None, :], (128, 4, 2, 16)).reshape(128, -1))
        uh = np.zeros((1024, 16), np.float32) if j == 0 else _pc(u[:, t0 - 16:t0])
        ims.append({"uT": _pc(u[:, t0:t0 + TPC]), "uhT": uh, "rc": rc, "pw": _pc(wb["pool_w"][0]), "psb": psb})
    res = _run("KC", lambda: build_KC(TOK=TPC), ims)
    catT = []
    for b in range(BATCH):
        bout = np.concatenate([np.asarray(res[b * 4 + j]["boutT"]) for j in range(4)], axis=1)
        catT.append(np.concatenate([aoutT[b], bout], axis=0))
    del z
    x2T = _ke_layer(xT, catT, 0, f32(mix_post_g)[0], f32(ffn_pre_g)[0], f32(ffn_post_g)[0], _pc(wb["ab_w_out"][0]),
                    _pc(wb["ffn_up"][0]), _pc(wb["ffn_down"][0]), f32(ffn_conv_w)[0], f32(ffn_conv_b)[0])
    del catT, xT
    z = _ka_layer(x2T, f32(mix_pre_g)[1], _pc(wb["cd_w_in"][0]), CD_TILES, 2880, CD_OUTS, "KA1")
    allc = lambda k: np.concatenate([z[k][b] for b in range(BATCH)], axis=1)
    cq_all, ckv_all, kr_all, xl_all, yg_all = allc("cqT"), allc("ckvT"), allc("krT"), allc("xlT"), allc("ygT")
    krs_all = _pc(np.concatenate([kr_all[32:], kr_all[:32]], axis=0))
    posrep = _pc(np.broadcast_to(positions.reshape(1, -1), (64, BATCH * SEQ)))
    freq = (np.float32(10000.0) ** (-np.arange(32, dtype=np.float32) / np.float32(32))).astype(np.float32)
    fq = _pc(np.stack([np.concatenate([freq, freq]), np.concatenate([-np.ones(32), np.ones(32)])], axis=1).astype(np.float32))
    mlag = _pc(np.concatenate([f32(q_norm_g)[0].reshape(4, 128).T, f32(kv_norm_g)[0].reshape(2, 128).T], axis=1))
    mask = _mla_mask()
    ones = np.ones((128, 128), np.float32)
    ims = []
    for c in range(NCORES):
        wq_full = wb["w_q_up"][0][:, c * 192:(c + 1) * 192]
        wq = _pc(np.concatenate([wq_full, wq_full[:, 160:192], wq_full[:, 128:160]], axis=1))
        wkv = _pc(wb["w_kv_up"][0][:, c * 256:(c + 1) * 256])
        sl = slice(c * 128, (c + 1) * 128)
        lrup = _pc(np.stack([f32(lru_conv_w)[0][0, sl], f32(lru_conv_w)[0][1, sl], f32(lru_conv_w)[0][2, sl], f32(lru_conv_w)[0][3, sl],
                             f32(lru_conv_b)[0][sl], f32(lru_ba)[0][sl], f32(lru_bx)[0][sl], f32(lru_lambda)[0][sl]], axis=1).astype(np.float32))
        ims.append({"ones": ones, "cqT": cq_all, "ckvT": ckv_all, "krT": kr_all, "krsT": krs_all, "posrep": posrep, "fq": fq,
                    "wq": wq, "wkv": wkv, "mlag": mlag, "mask": mask,
                    "xlT": _pc(xl_all[sl]), "ygT": _pc(yg_all[sl]), "lwa": _pc(wb["lru_wa"][0][c]), "lwx": _pc(wb["lru_wx"][0][c]), "lrup": lrup})
    res = _run("KD", lambda: build_KD(T=SEQ, NB=BATCH), ims)
    cat_all = np.concatenate([np.asarray(res[c]["coutT"]) for c in range(NCORES)] + [np.asarray(res[c]["doutT"]) for c in range(NCORES)], axis=0)
    catT = [_pc(cat_all[:, b * SEQ:(b + 1) * SEQ]) for b in range(BATCH)]
    del ims, z, cq_all, ckv_all, kr_all, xl_all, yg_all
    yT = _ke_layer(x2T, catT, 1, f32(mix_post_g)[1], f32(ffn_pre_g)[1], f32(ffn_post_g)[1], _pc(wb["cd_w_out"][0]),
                   _pc(wb["ffn_up"][1]), _pc(wb["ffn_down"][1]), f32(ffn_conv_w)[1], f32(ffn_conv_b)[1])
    out = np.stack([_pc(yT[b].T) for b in range(BATCH)], axis=0).astype(np.float32)
    return out
```

```python
import numpy as np
import concourse.bass as bass
import concourse.mybir as mybir

F32 = mybir.dt.float32
BF16 = mybir.dt.bfloat16
I32 = mybir.dt.int32
ALU = mybir.AluOpType
AF = mybir.ActivationFunctionType
AX = mybir.AxisListType


class Buf:
    __slots__ = ("name", "t", "last_w", "readers")

    def __init__(self, name, t=None):
        self.name = name
        self.t = t
        self.last_w = None
        self.readers = []

    def __getitem__(self, idx):
        return self.t[idx]


class Prog:
    COMPUTE = ("tensor", "vector", "scalar", "gpsimd")
    DMAQ = ("sync", "gpsimd")

    def __init__(self, nc, n_chan=12):
        self.nc = nc
        self.stack = []
        self.ops = {e: [] for e in ("sync", "tensor", "vector", "scalar", "gpsimd")}
        self.cnt = {}
        self.sems = {}
        for e in self.COMPUTE:
            self.sems[e] = self._enter(nc.semaphore("s_" + e))
            self.cnt[e] = 0
        self.chans = []
        for i in range(n_chan):
            k = "ch%d" % i
            self.sems[k] = self._enter(nc.semaphore("s_" + k))
            self.cnt[k] = 0
            self.chans.append(k)
        self.chan_rr = 0
        self.waited = {e: {} for e in self.ops}
        self.nbuf = 0

    def _enter(self, cm):
        v = cm.__enter__()
        self.stack.append(cm)
        return v

    def sb(self, name, shape, dt):
        t = self._enter(self.nc.sbuf_tensor("sb_" + name, list(shape), dt))
        return Buf(name, t)

    def ps(self, name, shape, dt=F32):
        t = self._enter(self.nc.psum_tensor("ps_" + name, list(shape), dt))
        return Buf(name, t)

    def view(self, name):
        return Buf(name, None)

    def _deps(self, reads, writes):
        deps = {}
        def add(tok):
            if tok is None:
                return
            k, v = tok
            if deps.get(k, 0) < v:
                deps[k] = v
        for b in reads:
            add(b.last_w)
        for b in writes:
            add(b.last_w)
            for r in b.readers:
                add(r)
        return deps

    def _commit(self, tok, reads, writes):
        for b in reads:
            b.readers.append(tok)
            if len(b.readers) > 64:
                m = {}
                for k, v in b.readers:
                    if m.get(k, 0) < v:
                        m[k] = v
                b.readers = list(m.items())
        for b in writes:
            b.last_w = tok
            b.readers = []

    def _waits(self, eng, deps, same_engine_sync=True):
        w = []
        wd = self.waited[eng]
        for k, v in deps.items():
            if k == eng and (eng == "tensor" or not same_engine_sync):
                continue
            if wd.get(k, 0) >= v:
                continue
            wd[k] = v
            w.append((k, v))
        return w

    def op(self, eng, fn, reads=(), writes=(), sync_self=True):
        deps = self._deps(reads, writes)
        waits = self._waits(eng, deps, sync_self)
        self.cnt[eng] += 1
        tok = (eng, self.cnt[eng])
        self.ops[eng].append((waits, fn, (eng, 1)))
        self._commit(tok, reads, writes)
        return tok

    def dma(self, out_ap, in_ap, reads=(), writes=(), q="sync", **kw):
        deps = self._deps(reads, writes)
        ch = self.chans[self.chan_rr]
        self.chan_rr = (self.chan_rr + 1) % len(self.chans)
        if self.cnt[ch] > 0:
            v = 16 * self.cnt[ch]
            if deps.get(ch, 0) < v:
                deps[ch] = v
        waits = self._waits(q, deps)
        self.cnt[ch] += 1
        tok = (ch, 16 * self.cnt[ch])

        def fn(e, out_ap=out_ap, in_ap=in_ap, kw=kw):
            return e.dma_start(out=out_ap, in_=in_ap, **kw)
        self.ops[q].append((waits, fn, (ch, 16)))
        self._commit(tok, reads, writes)
        return tok

    def finish_wait(self, eng, bufs):
        deps = self._deps(bufs, ())
        waits = self._waits(eng, deps)
        self.ops[eng].append((waits, None, None))

    def build(self):
        nc = self.nc
        blk = self._enter(nc.Block())
        P = self

        def emit(ename):
            def body(e):
                for waits, fn, inc in P.ops[ename]:
                    for k, v in waits:
                        e.wait_ge(P.sems[k], v)
                    if fn is not None:
                        ins = fn(e)
                        ins.then_inc(P.sems[inc[0]], inc[1])
            return body

        blk.sync(emit("sync"))
        blk.tensor(emit("tensor"))
        blk.vector(emit("vector"))
        blk.scalar(emit("scalar"))
        blk.gpsimd(emit("gpsimd"))

    def close(self):
        while self.stack:
            cm = self.stack.pop()
            cm.__exit__(None, None, None)


EPS = 1e-6


def dram_in(nc, name, shape, dt):
    return nc.dram_tensor(name, list(shape), dt, kind="ExternalInput").ap()


def dram_out(nc, name, shape, dt):
    return nc.dram_tensor(name, list(shape), dt, kind="ExternalOutput").ap()


def wait_all_dma(P, eng="sync"):
    for ch in P.chans:
        if P.cnt[ch]:
            P.ops[eng].append(([(ch, 16 * P.cnt[ch])], None, None))


def emit_rmsnorm_T(P, xs, g_sb, ones_bf, sq, ss_ps, rstd, hT, hoff, N, D, nch, x_reads, tag=""):
    P.op("scalar", lambda e: e.activation(out=sq[:, 0:nch, 0:N], in_=xs[:, 0:nch, 0:N], func=AF.Square), [xs], [sq])

    def mm(e):
        ins = None
        for c in range(nch):
            ins = e.matmul(ss_ps[:, 0:N], lhsT=ones_bf[:], rhs=sq[:, c, 0:N], start=(c == 0), stop=(c == nch - 1))
        return ins
    P.op("tensor", mm, [sq, ones_bf], [ss_ps])
    P.op("vector", lambda e: e.tensor_scalar(rstd[:, 0:N], ss_ps[:, 0:N], 1.0 / D, EPS, op0=ALU.mult, op1=ALU.add), [ss_ps], [rstd])
    P.op("scalar", lambda e: e.activation(out=rstd[:, 0:N], in_=rstd[:, 0:N], func=AF.Sqrt), [rstd], [rstd])
    P.op("vector", lambda e: e.reciprocal(rstd[:, 0:N], rstd[:, 0:N]), [rstd], [rstd])

    def sc(e):
        ins = None
        for c in range(nch):
            ins = e.scalar_tensor_tensor(out=hT[:, c, hoff:hoff + N], in0=xs[:, c, 0:N], scalar=g_sb[:, c:c + 1],
                                         in1=rstd[:, 0:N], op0=ALU.mult, op1=ALU.mult)
        return ins
    P.op("vector", sc, [xs, g_sb, rstd], [hT])


def build_KA(tiles, NOUT, outs, TOK=4096, TOKB=2048, D=2048, WG=256):
    nc = bass.Bass("TRN2", target_bir_lowering=False)
    P = Prog(nc)
    nch = D // 128
    x_d = dram_in(nc, "xT", [D, TOK], F32).rearrange("(c p) n -> p c n", p=128)
    g_d = dram_in(nc, "g", [128, nch], F32)
    w_d = dram_in(nc, "W", [D, NOUT], BF16).rearrange("(c p) n -> p c n", p=128)
    ones_d = dram_in(nc, "ones", [128, 128], F32)
    o_d = {k: dram_out(nc, k, [r, TOK], dt) for k, (r, dt) in outs.items()}
    odt = {k: dt for k, (r, dt) in outs.items()}

    NG = 512
    xs = P.sb("xs", [128, nch, NG], F32)
    sq = P.sb("sq", [128, nch, NG], BF16)
    hT = P.sb("hT", [128, nch, TOKB], BF16)
    g_sb = P.sb("g_sb", [128, nch], F32)
    ones_f = P.sb("ones_f", [128, 128], F32)
    ones_bf = P.sb("ones_bf", [128, 128], BF16)
    rstd = P.sb("rstd", [128, NG], F32)
    ss_ps = P.ps("ss_ps", [128, NG], F32)
    wbf = [P.sb("wbf%d" % i, [128, nch, WG], BF16) for i in range(3)]
    mm_ps = [P.ps("mm_ps%d" % i, [128, NG], F32) for i in range(3)]
    ost = {}
    for k, (r, dt) in outs.items():
        if dt not in ost:
            ost[dt] = [P.sb("ost_%s_%d" % (str(dt)[-4:], i), [128, TOKB], dt) for i in range(2)]
    ocnt = {dt: 0 for dt in ost}

    P.dma(g_sb[:], g_d, writes=[g_sb])
    P.dma(ones_f[:], ones_d, writes=[ones_f])
    P.op("vector", lambda e: e.tensor_copy(ones_bf[:], ones_f[:]), [ones_f], [ones_bf])

    groups = []
    cur = []
    for t in tiles:
        if cur and (t[0] + t[1] - cur[0][0] > WG or t[0] != cur[-1][0] + cur[-1][1]):
            groups.append(cur)
            cur = []
        cur.append(t)
    if cur:
        groups.append(cur)

    outv = []
    wi = 0
    pi = 0
    for tb in range(TOK // TOKB):
        t0 = tb * TOKB
        for gi in range(TOKB // NG):
            P.dma(xs[:], x_d[:, :, t0 + gi * NG: t0 + (gi + 1) * NG], writes=[xs])
            emit_rmsnorm_T(P, xs, g_sb, ones_bf, sq, ss_ps, rstd, hT, gi * NG, NG, D, nch, None)
        for grp in groups:
            c0 = grp[0][0]
            c1 = grp[-1][0] + grp[-1][1]
            wb_ = wbf[wi % 3]
            wi += 1
            P.dma(wb_[:, :, 0:c1 - c0], w_d[:, :, c0:c1], writes=[wb_])
            for (col0, ncols, oname, row0) in grp:
                dt = odt[oname]
                ob = ost[dt][ocnt[dt] % 2]
                ocnt[dt] += 1
                for gi in range(TOKB // NG):
                    ps = mm_ps[pi % 3]
                    pi += 1

                    def mm(e, ps=ps, wb_=wb_, off=col0 - c0, ncols=ncols, gi=gi):
                        ins = None
                        for c in range(nch):
                            ins = e.matmul(ps[0:ncols, :], lhsT=wb_[:, c, off:off + ncols], rhs=hT[:, c, gi * NG:(gi + 1) * NG],
                                           start=(c == 0), stop=(c == nch - 1))
                        return ins
                    P.op("tensor", mm, [wb_, hT], [ps])
                    if gi % 2 == 0:
                        P.op("scalar", lambda e, ps=ps, ob=ob, ncols=ncols, gi=gi: e.activation(out=ob[0:ncols, gi * NG:(gi + 1) * NG], in_=ps[0:ncols, :], func=AF.Copy), [ps], [ob])
                    else:
                        P.op("vector", lambda e, ps=ps, ob=ob, ncols=ncols, gi=gi: e.tensor_copy(ob[0:ncols, gi * NG:(gi + 1) * NG], ps[0:ncols, :]), [ps], [ob])
                dst = P.view("o")
                outv.append(dst)
                P.dma(o_d[oname][row0:row0 + ncols, t0:t0 + TOKB], ob[0:ncols, :], reads=[ob], writes=[dst])
    wait_all_dma(P)
    P.build()
    P.close()
    return nc


AB_TILES = ([(i * 128, 128, "qT", i * 128) for i in range(8)]
            + [(1024 + i * 128, 128, "kT", i * 128) for i in range(8)]
            + [(2048 + i * 128, 128, "vT", i * 128) for i in range(8)]
            + [(3072 + i * 128, 128, "qiT", i * 128) for i in range(8)]
            + [(4096, 64, "kiT", 0), (4160, 16, "wiT", 0)]
            + [(4176 + i * 128, 128, "uT", i * 128) for i in range(8)])
AB_OUTS = {"qT": (1024, BF16), "kT": (1024, BF16), "vT": (1024, BF16), "qiT": (1024, BF16),
           "kiT": (64, BF16), "wiT": (16, F32), "uT": (1024, F32)}

CD_TILES = ([(i * 128, 128, "cqT", i * 128) for i in range(4)]
            + [(512 + i * 128, 128, "ckvT", i * 128) for i in range(2)]
            + [(768, 64, "krT", 0)]
            + [(832 + i * 128, 128, "xlT", i * 128) for i in range(8)]
            + [(1856 + i * 128, 128, "ygT", i * 128) for i in range(8)])
CD_OUTS = {"cqT": (512, F32), "ckvT": (256, F32), "krT": (64, F32), "xlT": (1024, F32), "ygT": (1024, F32)}


D = 2048
NCH = 16
DFF = 4096


def emit_rstd(P, src, nch, N, ones_bf, sq, ss_ps, rstd, Dn):
    P.op("scalar", lambda e: e.activation(out=sq[:, 0:nch, 0:N], in_=src[:, 0:nch, 0:N], func=AF.Square), [src], [sq])

    def mm(e):
        ins = None
        for c in range(nch):
            ins = e.matmul(ss_ps[:, 0:N], lhsT=ones_bf[:], rhs=sq[:, c, 0:N], start=(c == 0), stop=(c == nch - 1))
        return ins
    P.op("tensor", mm, [sq, ones_bf], [ss_ps])
    P.op("vector", lambda e: e.tensor_scalar(rstd[:, 0:N], ss_ps[:, 0:N], 1.0 / Dn, EPS, op0=ALU.mult, op1=ALU.add), [ss_ps], [rstd])
    P.op("scalar", lambda e: e.activation(out=rstd[:, 0:N], in_=rstd[:, 0:N], func=AF.Sqrt), [rstd], [rstd])
    P.op("vector", lambda e: e.reciprocal(rstd[:, 0:N], rstd[:, 0:N]), [rstd], [rstd])


def emit_scale(P, eng, dst, src, g_sb, gcol0, rstd, nch, N):
    def sc(e):
        ins = None
        for c in range(nch):
            ins = e.scalar_tensor_tensor(out=dst[:, c, 0:N], in0=src[:, c, 0:N], scalar=g_sb[:, gcol0 + c:gcol0 + c + 1],
                                         in1=rstd[:, 0:N], op0=ALU.mult, op1=ALU.mult)
        return ins
    rd = [src, g_sb, rstd]
    P.op(eng, sc, rd, [dst])


def split_groups(TOK, maxn=510):
    ng = -(-TOK // maxn)
    base = -(-TOK // ng)
    base = -(-base // 8) * 8
    sizes = []
    rem = TOK
    while rem > 0:
        n = min(base, rem)
        sizes.append(n)
        rem -= n
    return sizes


def build_KE(TOK=4096, stop_after=None):
    nc = bass.Bass("TRN2", target_bir_lowering=False)
    P = Prog(nc)
    r3 = lambda ap: ap.rearrange("(c p) n -> p c n", p=128)
    x_d = r3(dram_in(nc, "xT", [D, TOK], F32))
    cat_d = r3(dram_in(nc, "catT", [D, TOK], BF16))
    xh_d = r3(dram_in(nc, "xhT", [D, 2], F32))
    cath_d = r3(dram_in(nc, "cathT", [D, 2], BF16))
    gs_d = dram_in(nc, "gs", [128, 3 * NCH], F32)
    wout_d = r3(dram_in(nc, "w_out", [D, D], BF16))
    up_d = r3(dram_in(nc, "up", [D, 2 * DFF], BF16))
    down_d = r3(dram_in(nc, "down", [DFF, D], BF16))
    cw_d = dram_in(nc, "cw", [128, 3 * 64], F32)
    cb_d = dram_in(nc, "cb", [128, 64], F32)
    ones_d = dram_in(nc, "ones", [128, 128], F32)
    y_d = r3(dram_out(nc, "yT", [D, TOK], F32))

    NG = 512
    A = P.sb("A", [128, NCH, NG], F32)
    B = P.sb("B", [128, NCH, NG], F32)
    cb16 = P.sb("cb16", [128, NCH, NG], BF16)
    hb = P.sb("hb", [128, NCH, NG], BF16)
    act = P.sb("act", [128, 32, NG], BF16)
    NWB = 4
    wb = [P.sb("wb%d" % i, [128, NCH, 256], BF16) for i in range(NWB)]
    acc = [[P.sb("acc%d%d" % (i, j), [128, NG], F32) for j in range(2)] for i in range(2)]
    rstd = P.sb("rstd", [128, NG], F32)
    gs = P.sb("gs", [128, 3 * NCH], F32)
    cw = P.sb("cw", [128, 3 * 64], F32)
    cb = P.sb("cb", [128, 64], F32)
    ones_f = P.sb("ones_f", [128, 128], F32)
    ones_bf = P.sb("ones_bf", [128, 128], BF16)
    ss_ps = P.ps("ss_ps", [128, NG], F32)
    NPS = 5
    mm_ps = [P.ps("mm_ps%d" % i, [128, NG], F32) for i in range(NPS)]
    st = {"wi": 0, "pi": 0, "ti": 0}

    P.dma(gs[:], gs_d, writes=[gs])
    P.dma(cw[:], cw_d, writes=[cw])
    P.dma(cb[:], cb_d, writes=[cb])
    P.dma(ones_f[:], ones_d, writes=[ones_f])
    P.op("vector", lambda e: e.tensor_copy(ones_bf[:], ones_f[:]), [ones_f], [ones_bf])

    def next_wb():
        b = wb[st["wi"] % NWB]
        st["wi"] += 1
        return b

    def next_ps():
        b = mm_ps[st["pi"] % NPS]
        st["pi"] += 1
        return b

    def load_w(src_ap, ncols=256):
        b = next_wb()
        P.dma(b[:, :, 0:ncols], src_ap, writes=[b])
        return b

    def matmul_group(ps, M, N, parts, roff=0):
        def mm(e):
            ins = None
            tot = sum(len(p[3]) for p in parts)
            i = 0
            for (w, off, rb, chunks) in parts:
                for (wc, rc) in chunks:
                    ins = e.matmul(ps[0:M, 0:N], lhsT=w[:, wc, off:off + M], rhs=rb[:, rc, roff:roff + N], start=(i == 0), stop=(i == tot - 1))
                    i += 1
            return ins
        rd = []
        for p in parts:
            rd += [p[0], p[2]]
        P.op("tensor", mm, rd, [ps])

    def evac(ps, dst_ap_fn, dstbuf, k):
        if k % 2 == 0:
            P.op("scalar", lambda e: e.activation(out=dst_ap_fn(), in_=ps[:, 0:ps_n[0]], func=AF.Copy), [ps], [dstbuf])
        else:
            P.op("vector", lambda e: e.tensor_copy(dst_ap_fn(), ps[:, 0:ps_n[0]]), [ps], [dstbuf])
    ps_n = [0]

    def group(n0, N, halo):
        xsrc = xh_d if halo else x_d[:, :, n0:n0 + N]
        csrc = cath_d if halo else cat_d[:, :, n0:n0 + N]
        ho = 0 if halo else 2
        if not halo and n0 > 0:
            pN = prev_n[0]
            P.op("vector", lambda e: e.tensor_copy(rstd[:, 0:32].bitcast(BF16)[:, 0:32].rearrange("p (c n) -> p c n", n=2), hb[:, :, pN:pN + 2]), [hb], [rstd])
            P.op("vector", lambda e: e.tensor_copy(hb[:, :, 0:2], rstd[:, 0:32].bitcast(BF16)[:, 0:32].rearrange("p (c n) -> p c n", n=2)), [rstd], [hb])
        P.dma(cb16[:, :, 0:N], csrc, writes=[cb16])
        P.dma(A[:, :, 0:N], xsrc, writes=[A])
        for mp in range(8):
            w = load_w(wout_d[:, :, mp * 256:(mp + 1) * 256])
            for j in range(2):
                m = mp * 2 + j
                ps = next_ps()
                matmul_group(ps, 128, N, [(w, j * 128, cb16, [(c, c) for c in range(NCH)])])
                if m % 2 == 0:
                    P.op("scalar", lambda e, ps=ps, m=m: e.activation(out=B[:, m, 0:N], in_=ps[:, 0:N], func=AF.Copy), [ps], [B])
                else:
                    P.op("vector", lambda e, ps=ps, m=m: e.tensor_copy(B[:, m, 0:N], ps[:, 0:N]), [ps], [B])
        emit_rstd(P, B, NCH, N, ones_bf, act, ss_ps, rstd, D)
        emit_scale(P, "vector", B, B, gs, 0, rstd, NCH, N)
        P.op("gpsimd", lambda e: e.tensor_tensor(out=A[:, :, 0:N], in0=A[:, :, 0:N], in1=B[:, :, 0:N], op=ALU.add), [A, B], [A])
        if stop_after == "epi":
            if not halo:
                P.dma(y_d[:, :, n0:n0 + N], A[:, :, 0:N], reads=[A])
            return
        emit_rstd(P, A, NCH, N, ones_bf, act, ss_ps, rstd, D)

        def sc(e):
            ins = None
            for c in range(NCH):
                ins = e.scalar_tensor_tensor(out=hb[:, c, ho:ho + N], in0=A[:, c, 0:N], scalar=gs[:, NCH + c:NCH + c + 1],
                                             in1=rstd[:, 0:N], op0=ALU.mult, op1=ALU.mult)
            return ins
        P.op("vector", sc, [A, gs, rstd], [hb])
        if halo:
            return
        prev_n[0] = N
        NP = N + 2
        for pg in range(16):
            wg = load_w(up_d[:, :, pg * 256:(pg + 1) * 256])
            wv = load_w(up_d[:, :, DFF + pg * 256:DFF + (pg + 1) * 256])
            for j in range(2):
                m = pg * 2 + j
                psg = next_ps()
                psv = next_ps()
                matmul_group(psg, 128, NP, [(wg, j * 128, hb, [(c, c) for c in range(NCH)])])
                matmul_group(psv, 128, NP, [(wv, j * 128, hb, [(c, c) for c in range(NCH)])])
                ag, av = acc[st["ti"] % 2]
                st["ti"] += 1

                def conv(ps_, a, mm_):
                    P.op("scalar", lambda e: e.activation(out=a[:, 0:N], in_=ps_[:, 2:2 + N], func=AF.Identity, scale=cw[:, 128 + mm_:128 + mm_ + 1], bias=cb[:, mm_:mm_ + 1]), [ps_, cw, cb], [a])
                    P.op("vector", lambda e: e.scalar_tensor_tensor(out=a[:, 0:N], in0=ps_[:, 1:1 + N], scalar=cw[:, 64 + mm_:64 + mm_ + 1], in1=a[:, 0:N], op0=ALU.mult, op1=ALU.add), [ps_, cw, a], [a])
                    P.op("vector", lambda e: e.scalar_tensor_tensor(out=a[:, 0:N], in0=ps_[:, 0:N], scalar=cw[:, mm_:mm_ + 1], in1=a[:, 0:N], op0=ALU.mult, op1=ALU.add), [ps_, cw, a], [a])
                conv(psg, ag, m)
                conv(psv, av, 32 + m)
                P.op("scalar", lambda e, ag=ag: e.activation(out=ag[:, 0:N], in_=ag[:, 0:N], func=AF.Gelu_apprx_tanh), [ag], [ag])
                P.op("gpsimd", lambda e, ag=ag, av=av, m=m: e.tensor_tensor(out=act[:, m, 0:N], in0=ag[:, 0:N], in1=av[:, 0:N], op=ALU.mult), [ag, av], [act])
        if stop_after == "up":
            P.op("vector", lambda e: e.tensor_copy(A[:, :, 0:N], act[:, 0:16, 0:N]), [act], [A])
            P.dma(y_d[:, :, n0:n0 + N], A[:, :, 0:N], reads=[A])
            return
        for mp in range(8):
            w0 = load_w(down_d[:, 0:16, mp * 256:(mp + 1) * 256])
            w1 = load_w(down_d[:, 16:32, mp * 256:(mp + 1) * 256])
            for j in range(2):
                m = mp * 2 + j
                ps = next_ps()
                matmul_group(ps, 128, N, [(w0, j * 128, act, [(c, c) for c in range(16)]),
                                          (w1, j * 128, act, [(c, 16 + c) for c in range(16)])])
                if m % 2 == 0:
                    P.op("scalar", lambda e, ps=ps, m=m: e.activation(out=B[:, m, 0:N], in_=ps[:, 0:N], func=AF.Copy), [ps], [B])
                else:
                    P.op("vector", lambda e, ps=ps, m=m: e.tensor_copy(B[:, m, 0:N], ps[:, 0:N]), [ps], [B])
        emit_rstd(P, B, NCH, N, ones_bf, act, ss_ps, rstd, D)
        emit_scale(P, "vector", B, B, gs, 2 * NCH, rstd, NCH, N)
        P.op("gpsimd", lambda e: e.tensor_tensor(out=A[:, :, 0:N], in0=A[:, :, 0:N], in1=B[:, :, 0:N], op=ALU.add), [A, B], [A])
        P.dma(y_d[:, :, n0:n0 + N], A[:, :, 0:N], reads=[A])

    prev_n = [0]
    group(0, 2, True)
    n0 = 0
    for N in split_groups(TOK):
        group(n0, N, False)
        n0 += N
    wait_all_dma(P)
    P.build()
    P.close()
    return nc


WINS = (2, 4, 8, 16)


def build_KC(TOK=4096, NG=512):
    nc = bass.Bass("TRN2", target_bir_lowering=False)
    P = Prog(nc)
    u_d = dram_in(nc, "uT", [1024, TOK], F32).rearrange("(c p) n -> p c n", p=128)
    uh_d = dram_in(nc, "uhT", [1024, 16], F32).rearrange("(c p) n -> p c n", p=128)
    rc_d = dram_in(nc, "rc", [128, 4 * 2 * 16], F32)
    pw_d = dram_in(nc, "pw", [4, 256, 256], BF16).rearrange("g (c p) d -> p g c d", p=128)
    psb_d = dram_in(nc, "psb", [128, 16], F32)
    o_d = dram_out(nc, "boutT", [1024, TOK], BF16).rearrange("(c p) n -> p c n", p=128)
    W = 16 + NG
    U = [P.sb("U%d" % i, [128, 8, W], F32) for i in range(2)]
    S1 = P.sb("S1", [128, 2, W], F32)
    S2 = P.sb("S2", [128, 2, W], F32)
    PB = [P.sb("PB%d" % i, [128, 8, NG], BF16) for i in range(2)]
    OB = [P.sb("OB%d" % i, [128, 8, NG], BF16) for i in range(2)]
    rc = P.sb("rc", [128, 4, 2, 16], F32)
    tmp = P.sb("tmp", [128, 2, 16], F32)
    pw = P.sb("pw", [128, 4, 2, 256], BF16)
    psb = P.sb("psb", [128, 16], F32)
    bs = P.sb("bs", [128, 8], F32)
    pss = [P.ps("pss%d" % i, [128, NG], F32) for i in range(4)]
    P.dma(rc[:].rearrange("p a b c -> p (a b c)"), rc_d, writes=[rc])
    P.dma(psb[:], psb_d, writes=[psb])
    for g in range(4):
        P.dma(pw[:, g, :, :], pw_d[:, g, :, :], writes=[pw])
    P.op("vector", lambda e: e.tensor_tensor(out=bs[:], in0=psb[:, 0:8], in1=psb[:, 8:16], op=ALU.mult), [psb], [bs])
    pi = 0
    for gi in range(TOK // NG):
        n0 = gi * NG
        Ub, PBb, OBb = U[gi % 2], PB[gi % 2], OB[gi % 2]
        if gi == 0:
            P.dma(Ub[:, :, 0:16], uh_d, writes=[Ub])
            P.dma(Ub[:, :, 16:W], u_d[:, :, 0:NG], writes=[Ub])
        else:
            P.dma(Ub[:, :, 0:W], u_d[:, :, n0 - 16:n0 + NG], writes=[Ub])
        for g in range(4):
            ks = slice(2 * g, 2 * g + 2)
            P.op("gpsimd", lambda e, Ub=Ub, ks=ks: e.tensor_tensor(out=S1[:, :, 1:W], in0=Ub[:, ks, 1:W], in1=Ub[:, ks, 0:W - 1], op=ALU.add), [Ub], [S1])
            fin = S1
            if g >= 1:
                P.op("gpsimd", lambda e: e.tensor_tensor(out=S2[:, :, 3:W], in0=S1[:, :, 3:W], in1=S1[:, :, 1:W - 2], op=ALU.add), [S1], [S2])
                fin = S2
            if g >= 2:
                P.op("gpsimd", lambda e: e.tensor_tensor(out=S1[:, :, 7:W], in0=S2[:, :, 7:W], in1=S2[:, :, 3:W - 4], op=ALU.add), [S2], [S1])
                fin = S1
            if g >= 3:
                P.op("gpsimd", lambda e: e.tensor_tensor(out=S2[:, :, 15:W], in0=S1[:, :, 15:W], in1=S1[:, :, 7:W - 8], op=ALU.add), [S1], [S2])
                fin = S2
            P.op("vector", lambda e, fin=fin, Ub=Ub, PBb=PBb, ks=ks, g=g: e.scalar_tensor_tensor(
                out=PBb[:, ks, 0:NG], in0=fin[:, :, 16:W], scalar=1.0 / WINS[g], in1=Ub[:, ks, 16:W], op0=ALU.mult, op1=ALU.subtract), [fin, Ub], [PBb])
            if gi == 0:
                P.op("vector", lambda e, fin=fin, g=g: e.tensor_tensor(out=tmp[:], in0=fin[:, :, 16:32], in1=rc[:, g, :, :], op=ALU.mult), [fin, rc], [tmp])
                P.op("vector", lambda e, Ub=Ub, PBb=PBb, ks=ks: e.tensor_tensor(out=PBb[:, ks, 0:16], in0=tmp[:], in1=Ub[:, ks, 16:32], op=ALU.subtract), [tmp, Ub, PBb], [PBb])
            for dt in range(2):
                ps = pss[pi % 4]
                pi += 1

                def mm(e, ps=ps, g=g, dt=dt, PBb=PBb):
                    e.matmul(ps[:, 0:NG], lhsT=pw[:, g, 0, dt * 128:(dt + 1) * 128], rhs=PBb[:, 2 * g, 0:NG], start=True, stop=False)
                    return e.matmul(ps[:, 0:NG], lhsT=pw[:, g, 1, dt * 128:(dt + 1) * 128], rhs=PBb[:, 2 * g + 1, 0:NG], start=False, stop=True)
                P.op("tensor", mm, [pw, PBb], [ps])
                k = 2 * g + dt
                P.op("scalar", lambda e, ps=ps, k=k, OBb=OBb: e.activation(out=OBb[:, k, 0:NG], in_=ps[:, 0:NG], func=AF.Identity, scale=psb[:, k:k + 1], bias=bs[:, k:k + 1]), [ps, psb, bs], [OBb])
        P.dma(o_d[:, :, n0:n0 + NG], OBb[:, :, 0:NG], reads=[OBb])
    wait_all_dma(P)
    P.build()
    P.close()
    return nc


TOPK = 256
NBIS = 26


def build_KB(T=16384, NSLOT=32):
    nc = bass.Bass("TRN2", target_bir_lowering=False)
    P = Prog(nc)
    NQ = NSLOT * 128
    q_d = dram_in(nc, "qT", [1024, NQ], BF16).rearrange("(h d) n -> d h n", d=128)
    qi_d = dram_in(nc, "qiT", [1024, NQ], BF16).rearrange("(h d) n -> d h n", d=64)
    wi_d = dram_in(nc, "wi", [NQ, 16], F32)
    ki_d = dram_in(nc, "kiT", [64, T], BF16)
    k_d = dram_in(nc, "kT", [1024, T], BF16).rearrange("(h d) n -> d h n", d=128)
    v_d = dram_in(nc, "v", [T, 1024], BF16).rearrange("(t s) n -> s t n", s=128)
    nm_d = dram_in(nc, "negmask", [128, 512], F32)
    id_d = dram_in(nc, "ident", [128, 128], F32)
    o_d = dram_out(nc, "aoutT", [1024, NQ], BF16).rearrange("(h d) n -> d h n", d=128)

    LMAX = 512 * NSLOT
    KIc = [P.sb("KIc%d" % i, [64, 512], BF16) for i in range(2)]
    Mk = P.sb("Mk", [128, LMAX], BF16)
    score = P.sb("score", [128, LMAX], F32)
    Mq = P.sb("Mq", [128, LMAX], BF16)
    MTc = [P.sb("MTc%d" % i, [128, 4, 128], BF16) for i in range(2)]
    Qb = [P.sb("Q%d" % i, [128, 8, 128], BF16) for i in range(2)]
    QI = P.sb("QI", [64, 16, 128], BF16)
    W = P.sb("W", [128, 16], F32)
    DG = P.sb("DG", [128, 16, 128], BF16)
    negm = P.sb("negm", [128, 512], F32)
    posm = P.sb("posm", [128, 512], F32)
    idf = P.sb("idf", [128, 128], F32)
    ident = P.sb("ident", [128, 128], BF16)
    ones_bf = P.sb("ones_bf", [128, 128], BF16)
    R = [P.sb("R%d" % i, [128, 512], BF16) for i in range(3)]
    tmpm = P.sb("tmpm", [128, 512], F32)
    st8 = P.sb("st8", [128, 8], F32)
    KTc = [P.sb("KTc%d" % i, [128, 8, 512], BF16) for i in range(2)]
    Vc = [P.sb("Vc%d" % i, [128, 4, 1024], BF16) for i in range(2)]
    PT = [P.sb("PT%d" % i, [128, 8, 128], BF16) for i in range(2)]
    rs = P.sb("rs", [128, 8, 128], F32)
    ob = [P.sb("ob%d" % i, [128, 8, 128], BF16) for i in range(2)]
    psA = [P.ps("psA%d" % i, [128, 512], F32) for i in range(2)]
    psB = [P.ps("psB%d" % i, [128, 512], F32) for i in range(2)]
    OT = [P.ps("OT%d" % i, [128, 4, 128], F32) for i in range(2)]
    SUM = [P.ps("SUM%d" % i, [128, 4, 128], F32) for i in range(2)]
    att_scale = 128.0 ** -0.5

    P.dma(negm[:], nm_d, writes=[negm])
    P.dma(idf[:], id_d, writes=[idf])
    P.op("vector", lambda e: e.tensor_copy(ident[:], idf[:]), [idf], [ident])
    P.op("vector", lambda e: e.memset(ones_bf[:], 1.0), [], [ones_bf])
    P.op("vector", lambda e: e.tensor_scalar(posm[:], negm[:], -1.0, None, op0=ALU.mult), [negm], [posm])
    c = {"a": 0, "b": 0, "r": 0, "kv": 0, "pt": 0, "ob": 0, "ev": 0, "ki": 0}

    def phase_index(m):
        L = 512 * (m + 1)
        ntile = 4 * (m + 1)
        qs = slice(m * 128, (m + 1) * 128)
        Q = Qb[m % 2]
        P.dma(Q[:], q_d[:, :, qs], writes=[Q])
        P.dma(QI[:], qi_d[:, :, qs], writes=[QI])
        P.dma(W[:], wi_d[qs, :], writes=[W])

        def mkdg(e):
            ins = None
            for h in range(16):
                ins = e.tensor_scalar(DG[:, h, :], ident[:], W[:, h:h + 1], None, op0=ALU.mult)
            return ins
        P.op("vector", mkdg, [ident, W], [DG])
        items = [(ch, h) for ch in range(m + 1) for h in range(16)]
        scb = {}
        kib = {}
        rbuf = {}

        def emit_lg(i):
            ch, h = items[i]
            if h == 0:
                scb[ch] = psB[c["b"] % 2]
                c["b"] += 1
                kib[ch] = KIc[c["ki"] % 2]
                c["ki"] += 1
                P.dma(kib[ch][:], ki_d[:, ch * 512:(ch + 1) * 512], writes=[kib[ch]])
            KIt = kib[ch]
            lg = psA[c["a"] % 2]
            c["a"] += 1
            r = R[c["r"] % 3]
            c["r"] += 1
            rbuf[i] = r
            P.op("tensor", lambda e, lg=lg, h=h, KIt=KIt: e.matmul(lg[:, :], lhsT=QI[:, h, :], rhs=KIt[:], start=True, stop=True), [QI, KIt], [lg])
            if h % 2 == 0:
                P.op("scalar", lambda e, lg=lg, r=r: e.activation(out=r[:], in_=lg[:, :], func=AF.Relu), [lg], [r])
            else:
                P.op("vector", lambda e, lg=lg, r=r: e.tensor_scalar(r[:], lg[:, :], 0.0, None, op0=ALU.max), [lg], [r])

        def emit_sc(i):
            ch, h = items[i]
            sc_ps = scb[ch]
            r = rbuf.pop(i)
            P.op("tensor", lambda e, sc_ps=sc_ps, h=h, r=r: e.matmul(sc_ps[:, :], lhsT=DG[:, h, :], rhs=r[:], start=(h == 0), stop=(h == 15)), [DG, r], [sc_ps])
            if h == 15:
                if ch < m:
                    P.op("scalar", lambda e, sc_ps=sc_ps, ch=ch: e.activation(out=score[:, ch * 512:(ch + 1) * 512], in_=sc_ps[:, :], func=AF.Copy), [sc_ps], [score])
                else:
                    P.op("vector", lambda e, sc_ps=sc_ps: e.tensor_tensor(out=tmpm[:], in0=sc_ps[:, :], in1=posm[:], op=ALU.add), [sc_ps, posm], [tmpm])
                    P.op("vector", lambda e, sc_ps=sc_ps, ch=ch: e.tensor_tensor(out=score[:, ch * 512:(ch + 1) * 512], in0=sc_ps[:, :], in1=negm[:], op=ALU.add), [sc_ps, negm], [score])
        for i in range(len(items)):
            emit_lg(i)
            if i >= 1:
                emit_sc(i - 1)
        emit_sc(len(items) - 1)

    def phase_bisect(m):
        L = 512 * (m + 1)
        P.op("vector", lambda e, L=L: e.tensor_reduce(out=st8[:, 0:1], in_=score[:, 0:L], axis=AX.X, op=ALU.max), [score], [st8])
        P.op("vector", lambda e: e.tensor_reduce(out=st8[:, 1:2], in_=tmpm[:], axis=AX.X, op=ALU.min), [tmpm], [st8])
        if m > 0:
            P.op("vector", lambda e, L=L: e.tensor_reduce(out=st8[:, 2:3], in_=score[:, 0:L - 512], axis=AX.X, op=ALU.min), [score], [st8])
            P.op("vector", lambda e: e.tensor_tensor(out=st8[:, 3:4], in0=st8[:, 1:2], in1=st8[:, 2:3], op=ALU.min), [st8], [st8])
        else:
            P.op("vector", lambda e: e.tensor_copy(st8[:, 3:4], st8[:, 1:2]), [st8], [st8])
        P.op("vector", lambda e: e.tensor_tensor(out=st8[:, 4:5], in0=st8[:, 0:1], in1=st8[:, 3:4], op=ALU.subtract), [st8], [st8])
        for it in range(NBIS):
            P.op("vector", lambda e: e.tensor_scalar(st8[:, 4:5], st8[:, 4:5], 0.5, None, op0=ALU.mult), [st8], [st8])
            P.op("vector", lambda e: e.tensor_tensor(out=st8[:, 5:6], in0=st8[:, 3:4], in1=st8[:, 4:5], op=ALU.add), [st8], [st8])
            P.op("vector", lambda e, L=L: e.tensor_scalar(Mq[:, 0:L], score[:, 0:L], st8[:, 5:6], 0.0, op0=ALU.is_ge, op1=ALU.add, accum_out=st8[:, 6:7]), [score, st8], [Mq, st8])
            P.op("vector", lambda e: e.scalar_tensor_tensor(out=st8[:, 7:8], in0=st8[:, 6:7], scalar=float(TOPK), in1=st8[:, 4:5], op0=ALU.is_ge, op1=ALU.mult), [st8], [st8])
            P.op("vector", lambda e: e.tensor_tensor(out=st8[:, 3:4], in0=st8[:, 3:4], in1=st8[:, 7:8], op=ALU.add), [st8], [st8])

    def phase_maskfinal(m):
        L = 512 * (m + 1)
        P.op("vector", lambda e, L=L: e.tensor_scalar(Mk[:, 0:L], score[:, 0:L], st8[:, 3:4], None, op0=ALU.is_ge), [score, st8], [Mk])

    def phase_attend(m):
        L = 512 * (m + 1)
        ntile = 4 * (m + 1)
        qs = slice(m * 128, (m + 1) * 128)
        Q = Qb[m % 2]
        kvbuf = {}

        def loads(ch):
            kt, vt = KTc[ch % 2], Vc[ch % 2]
            P.dma(kt[:], k_d[:, :, ch * 512:(ch + 1) * 512], writes=[kt])
            P.dma(vt[:], v_d[:, ch * 4:ch * 4 + 4, :], writes=[vt])
            kvbuf[ch] = (kt, vt)
        loads(0)
        if m >= 1:
            loads(1)
        tiles = [(ch, tt) for ch in range(m + 1) for tt in range(4)]
        mtb = {}
        ptb = {}

        def transposes(ch):
            MT = MTc[ch % 2]
            pb = psB[c["b"] % 2]
            c["b"] += 1
            psT = pb[:, :].bitcast(BF16)

            def tr(e, ch=ch, psT=psT):
                ins = None
                for k in range(4):
                    t_ = ch * 4 + k
                    ins = e.transpose(psT[:, k * 128:(k + 1) * 128], Mk[:, t_ * 128:(t_ + 1) * 128], ident[:])
                return ins
            P.op("tensor", tr, [Mk, ident], [pb])
            P.op("scalar", lambda e, MT=MT, psT=psT: e.activation(out=MT[:], in_=psT[:, 0:512].rearrange("p (a b) -> p a b", b=128), func=AF.Copy), [pb], [MT])
            mtb[ch] = MT

        def qk_exp(i, hg):
            ch, tt = tiles[i]
            kt, vt = kvbuf[ch]
            if hg == 0:
                ptb[i] = PT[c["pt"] % 2]
                c["pt"] += 1
            pt = ptb[i]
            st = psA[c["a"] % 2]
            c["a"] += 1

            def mmqk(e, st=st, hg=hg, kt=kt, tt=tt):
                ins = None
                for hh in range(4):
                    h = hg * 4 + hh
                    ins = e.matmul(st[:, hh * 128:(hh + 1) * 128], lhsT=kt[:, h, tt * 128:(tt + 1) * 128], rhs=Q[:, h, :], start=True, stop=True)
                return ins
            P.op("tensor", mmqk, [kt, Q], [st])
            P.op("scalar", lambda e, st=st, pt=pt, hg=hg: e.activation(out=pt[:, hg * 4:hg * 4 + 4, :], in_=st[:, :].rearrange("p (a b) -> p a b", b=128), func=AF.Exp, scale=att_scale), [st], [pt])

        def maskmul(i):
            ch, tt = tiles[i]
            pt = ptb[i]
            MT = mtb[ch]
            P.op("gpsimd", lambda e, pt=pt, tt=tt, MT=MT: e.tensor_tensor(out=pt[:], in0=pt[:], in1=MT[:, tt:tt + 1, :].to_broadcast([128, 8, 128]), op=ALU.mult), [pt, MT], [pt])

        def pv(i):
            ch, tt = tiles[i]
            kt, vt = kvbuf[ch]
            pt = ptb.pop(i)
            t = ch * 4 + tt

            def mmpv(e, pt=pt, vt=vt, tt=tt, t=t, ntile=ntile):
                ins = None
                for h in range(8):
                    ins = e.matmul(OT[h // 4][:, h % 4, :], lhsT=vt[:, tt, h * 128:(h + 1) * 128], rhs=pt[:, h, :], start=(t == 0 and h % 4 == 0), stop=(t == ntile - 1 and h % 4 == 3))
                for hg in range(2):
                    ins = e.matmul(SUM[hg][:].rearrange("p a b -> p (a b)"), lhsT=ones_bf[:], rhs=pt[:, hg * 4:hg * 4 + 4, :].rearrange("p a b -> p (a b)"),
                                   start=(t == 0), stop=(t == ntile - 1))
                return ins
            P.op("tensor", mmpv, [vt, pt, ones_bf], [OT[0], OT[1], SUM[0], SUM[1]])

        nt = len(tiles)
        transposes(0)
        qk_exp(0, 0)
        qk_exp(0, 1)
        maskmul(0)
        for i in range(nt):
            if i + 1 < nt:
                if tiles[i + 1][1] == 0:
                    transposes(tiles[i + 1][0])
                qk_exp(i + 1, 0)
            pv(i)
            if tiles[i][1] == 3 and tiles[i][0] + 2 <= m:
                loads(tiles[i][0] + 2)
            if i + 1 < nt:
                qk_exp(i + 1, 1)
                maskmul(i + 1)
        o = ob[c["ob"] % 2]
        c["ob"] += 1
        for hg in range(2):
            P.op("vector", lambda e, hg=hg: e.reciprocal(rs[:, hg * 4:hg * 4 + 4, :], SUM[hg][:]), [SUM[hg]], [rs])
            P.op("vector", lambda e, hg=hg, o=o: e.tensor_tensor(out=o[:, hg * 4:hg * 4 + 4, :], in0=OT[hg][:], in1=rs[:, hg * 4:hg * 4 + 4, :], op=ALU.mult), [OT[hg], rs], [o])
        P.dma(o_d[:, :, qs], o[:], reads=[o])

    for m in range(NSLOT):
        phase_index(m)
        phase_bisect(m)
        if m >= 1:
            phase_attend(m - 1)
        phase_maskfinal(m)
    phase_attend(NSLOT - 1)
    wait_all_dma(P)
    P.build()
    P.close()
    return nc


import math

PI = math.pi


def emit_mla(P, nc, T, NB, ones_bf, pj):
    TT = T * NB
    NG = 512
    r3 = lambda ap: ap.rearrange("(c p) n -> p c n", p=128)
    cq_d = r3(dram_in(nc, "cqT", [512, TT], F32))
    ckv_d = r3(dram_in(nc, "ckvT", [256, TT], F32))
    kr_d = dram_in(nc, "krT", [64, TT], F32)
    krs_d = dram_in(nc, "krsT", [64, TT], F32)
    pos_d = dram_in(nc, "posrep", [64, TT], I32)
    fq_d = dram_in(nc, "fq", [64, 2], F32)
    wq_d = r3(dram_in(nc, "wq", [512, 256], BF16))
    wkv_d = r3(dram_in(nc, "wkv", [256, 256], BF16))
    g_d = dram_in(nc, "mlag", [128, 6], F32)
    mask_d = dram_in(nc, "mask", [128, 4 * NG], BF16)
    co_d = dram_out(nc, "coutT", [128, TT], BF16)

    KT = P.sb("KT", [128, T], BF16)
    KRT = P.sb("KRT", [64, T], BF16)
    V = P.sb("V", [128, T // 128, 128], BF16)
    mask = P.sb("mask", [128, 4, NG], BF16)
    wq = P.sb("wq", [128, 4, 256], BF16)
    wkv = P.sb("wkv", [128, 2, 256], BF16)
    gg = P.sb("mlag", [128, 6], F32)
    fq = P.sb("fq", [64, 2], F32)
    ckv = P.sb("ckv", [128, 2, NG], F32)
    cq = P.sb("cq", [128, 4, NG], F32)
    sq = P.sb("sq", [128, 4, NG], BF16)
    cqn = P.sb("cqn", [128, 4, NG], BF16)
    ckvn = P.sb("ckvn", [128, 2, NG], BF16)
    rstd = P.sb("rstd", [128, NG], F32)
    QN = P.sb("QN", [128, NG], BF16)
    QR = P.sb("QR", [64, NG], BF16)
    posi = P.sb("posi", [64, NG], I32)
    ang = P.sb("ang", [64, NG], F32)
    cos = P.sb("cos", [64, NG], F32)
    sin = P.sb("sin", [64, NG], F32)
    kr = P.sb("kr", [64, NG], F32)
    krs = P.sb("krs", [64, NG], F32)
    t1 = P.sb("t1", [64, NG], F32)
    t2 = P.sb("t2", [64, NG], F32)
    PT = [P.sb("PT%d" % i, [128, NG], BF16) for i in range(3)]
    rs = P.sb("rs", [128, NG], F32)
    ob = [P.sb("ob%d" % i, [128, NG], BF16) for i in range(2)]
    ss_ps = P.ps("ss_ps", [128, NG], F32)
    STp = [P.ps("ST%d" % i, [128, NG], F32) for i in range(2)]
    OT = P.ps("OT", [128, NG], F32)
    SUM = P.ps("SUM", [128, NG], F32)
    scale = 192.0 ** -0.5

    P.dma(mask[:].rearrange("p a b -> p (a b)"), mask_d, writes=[mask])
    P.dma(wq[:], wq_d, writes=[wq])
    P.dma(wkv[:], wkv_d, writes=[wkv])
    P.dma(gg[:], g_d, writes=[gg])
    P.dma(fq[:], fq_d, writes=[fq])
    cnt = {"pj": 0, "st": 0, "pt": 0, "ob": 0}

    def nextpj():
        b = pj[cnt["pj"] % 2]
        cnt["pj"] += 1
        return b

    def proj(ps, M, w, woff, src, nchunk, N=NG):
        def mm(e):
            ins = None
            for c in range(nchunk):
                ins = e.matmul(ps[0:M, 0:N], lhsT=w[:, c, woff:woff + M], rhs=src[:, c, 0:N], start=(c == 0), stop=(c == nchunk - 1))
            return ins
        P.op("tensor", mm, [w, src], [ps])

    for b in range(NB):
        for g in range(T // NG):
            c0 = b * T + g * NG
            P.dma(ckv[:], ckv_d[:, :, c0:c0 + NG], writes=[ckv])
            P.dma(kr[:], kr_d[:, c0:c0 + NG], writes=[kr])
            P.dma(krs[:], krs_d[:, c0:c0 + NG], writes=[krs])
            P.dma(posi[:], pos_d[:, c0:c0 + NG], writes=[posi])
            P.dma(cq[:], cq_d[:, :, c0:c0 + NG], writes=[cq])
            emit_rstd(P, ckv, 2, NG, ones_bf, sq, ss_ps, rstd, 256)

            def sc_kv(e):
                ins = None
                for c in range(2):
                    ins = e.scalar_tensor_tensor(out=ckvn[:, c, :], in0=ckv[:, c, :], scalar=gg[:, 4 + c:5 + c], in1=rstd[:], op0=ALU.mult, op1=ALU.mult)
                return ins
            P.op("vector", sc_kv, [ckv, gg, rstd], [ckvn])
            ps = nextpj()
            proj(ps, 128, wkv, 0, ckvn, 2)
            P.op("scalar", lambda e, ps=ps, g=g: e.activation(out=KT[:, g * NG:(g + 1) * NG], in_=ps[:, 0:NG], func=AF.Copy), [ps], [KT])
            ps = nextpj()

            def mmv(e, ps=ps):
                ins = None
                for st_ in range(4):
                    for c in range(2):
                        ins = e.matmul(ps[:, st_ * 128:(st_ + 1) * 128], lhsT=ckvn[:, c, st_ * 128:(st_ + 1) * 128], rhs=wkv[:, c, 128:256], start=(c == 0), stop=(c == 1))
                return ins
            P.op("tensor", mmv, [ckvn, wkv], [ps])
            P.op("vector", lambda e, ps=ps, g=g: e.tensor_copy(V[:, 4 * g:4 * g + 4, :], ps[:, 0:NG].rearrange("p (a b) -> p a b", b=128)), [ps], [V])
            P.op("vector", lambda e: e.tensor_copy(ang[:], posi[:]), [posi], [ang])
            P.op("vector", lambda e: e.tensor_scalar(ang[:], ang[:], fq[:, 0:1], None, op0=ALU.mult), [ang, fq], [ang])
            P.op("vector", lambda e: e.tensor_scalar(t1[:], ang[:], 1.0 / (2 * PI), None, op0=ALU.mult), [ang], [t1])
            P.op("vector", lambda e: e.tensor_copy(posi[:], t1[:]), [t1], [posi])
            P.op("vector", lambda e: e.tensor_copy(t1[:], posi[:]), [posi], [t1])
            P.op("vector", lambda e: e.scalar_tensor_tensor(out=sin[:], in0=t1[:], scalar=-2 * PI, in1=ang[:], op0=ALU.mult, op1=ALU.add), [t1, ang], [sin])
            P.op("vector", lambda e: e.tensor_single_scalar(t1[:], sin[:], PI, op=ALU.is_gt), [sin], [t1])
            P.op("vector", lambda e: e.scalar_tensor_tensor(out=sin[:], in0=t1[:], scalar=-2 * PI, in1=sin[:], op0=ALU.mult, op1=ALU.add), [t1, sin], [sin])
            P.op("vector", lambda e: e.tensor_single_scalar(t1[:], sin[:], -PI, op=ALU.is_lt), [sin], [t1])
            P.op("vector", lambda e: e.scalar_tensor_tensor(out=sin[:], in0=t1[:], scalar=2 * PI, in1=sin[:], op0=ALU.mult, op1=ALU.add), [t1, sin], [sin])
            P.op("vector", lambda e: e.tensor_scalar(cos[:], sin[:], 0.5 * PI, None, op0=ALU.add), [sin], [cos])
            P.op("vector", lambda e: e.tensor_single_scalar(t1[:], cos[:], PI, op=ALU.is_gt), [cos], [t1])
            P.op("vector", lambda e: e.scalar_tensor_tensor(out=cos[:], in0=t1[:], scalar=-2 * PI, in1=cos[:], op0=ALU.mult, op1=ALU.add), [t1, cos], [cos])
            P.op("scalar", lambda e: e.activation(out=sin[:], in_=sin[:], func=AF.Sin), [sin], [sin])
            P.op("scalar", lambda e: e.activation(out=cos[:], in_=cos[:], func=AF.Sin), [cos], [cos])
            P.op("vector", lambda e: e.tensor_tensor(out=t1[:], in0=kr[:], in1=cos[:], op=ALU.mult), [kr, cos], [t1])
            P.op("vector", lambda e: e.scalar_tensor_tensor(out=t2[:], in0=krs[:], scalar=fq[:, 1:2], in1=sin[:], op0=ALU.mult, op1=ALU.mult), [krs, fq, sin], [t2])
            P.op("vector", lambda e, g=g: e.tensor_tensor(out=KRT[:, g * NG:(g + 1) * NG], in0=t1[:], in1=t2[:], op=ALU.add), [t1, t2], [KRT])
            emit_rstd(P, cq, 4, NG, ones_bf, sq, ss_ps, rstd, 512)

            def sc_q(e):
                ins = None
                for c in range(4):
                    ins = e.scalar_tensor_tensor(out=cqn[:, c, :], in0=cq[:, c, :], scalar=gg[:, c:c + 1], in1=rstd[:], op0=ALU.mult, op1=ALU.mult)
                return ins
            P.op("vector", sc_q, [cq, gg, rstd], [cqn])
            ps = nextpj()
            proj(ps, 128, wq, 0, cqn, 4)
            P.op("scalar", lambda e, ps=ps: e.activation(out=QN[:], in_=ps[:, 0:NG], func=AF.Copy, scale=scale), [ps], [QN])
            psa = nextpj()
            proj(psa, 64, wq, 128, cqn, 4)
            P.op("vector", lambda e, psa=psa: e.tensor_tensor(out=t1[:], in0=psa[0:64, 0:NG], in1=cos[:], op=ALU.mult), [psa, cos], [t1])
            psb = nextpj()
            proj(psb, 64, wq, 192, cqn, 4)
            P.op("vector", lambda e, psb=psb: e.scalar_tensor_tensor(out=t2[:], in0=psb[0:64, 0:NG], scalar=fq[:, 1:2], in1=sin[:], op0=ALU.mult, op1=ALU.mult), [psb, fq, sin], [t2])
            P.op("vector", lambda e: e.tensor_tensor(out=t1[:], in0=t1[:], in1=t2[:], op=ALU.add), [t1, t2], [t1])
            P.op("scalar", lambda e: e.activation(out=QR[:], in_=t1[:], func=AF.Copy, scale=scale), [t1], [QR])
            nj = 4 * (g + 1)
            for j in range(nj):
                st = STp[cnt["st"] % 2]
                cnt["st"] += 1
                pt = PT[cnt["pt"] % 3]
                cnt["pt"] += 1

                def mms(e, st=st, j=j):
                    e.matmul(st[:, 0:NG], lhsT=KT[:, j * 128:(j + 1) * 128], rhs=QN[:], start=True, stop=False)
                    return e.matmul(st[:, 0:NG], lhsT=KRT[:, j * 128:(j + 1) * 128], rhs=QR[:], start=False, stop=True)
                P.op("tensor", mms, [KT, KRT, QN, QR], [st])
                P.op("scalar", lambda e, st=st, pt=pt: e.activation(out=pt[:], in_=st[:, 0:NG], func=AF.Exp), [st], [pt])
                if j >= 4 * g:
                    P.op("gpsimd", lambda e, pt=pt, jj=j - 4 * g: e.tensor_tensor(out=pt[:], in0=pt[:], in1=mask[:, jj, :], op=ALU.mult), [pt, mask], [pt])

                def mmo(e, pt=pt, j=j, nj=nj):
                    e.matmul(OT[:, 0:NG], lhsT=V[:, j, :], rhs=pt[:], start=(j == 0), stop=(j == nj - 1))
                    return e.matmul(SUM[:, 0:NG], lhsT=ones_bf[:], rhs=pt[:], start=(j == 0), stop=(j == nj - 1))
                P.op("tensor", mmo, [V, pt, ones_bf], [OT, SUM])
            o = ob[cnt["ob"] % 2]
            cnt["ob"] += 1
            P.op("vector", lambda e: e.reciprocal(rs[:], SUM[:, 0:NG]), [SUM], [rs])
            P.op("vector", lambda e, o=o: e.tensor_tensor(out=o[:], in0=OT[:, 0:NG], in1=rs[:], op=ALU.mult), [OT, rs], [o])
            P.dma(co_d[:, c0:c0 + NG], o[:], reads=[o])


def emit_lru(P, nc, T, NB, pj, chunk=512):
    TT = T * NB
    NG = chunk
    xl_d = dram_in(nc, "xlT", [128, TT], F32)
    yg_d = dram_in(nc, "ygT", [128, TT], F32)
    wa_d = dram_in(nc, "lwa", [128, 128], BF16)
    wx_d = dram_in(nc, "lwx", [128, 128], BF16)
    lp_d = dram_in(nc, "lrup", [128, 8], F32)
    do_d = dram_out(nc, "doutT", [128, TT], BF16)
    wa = P.sb("lwa", [128, 128], BF16)
    wx = P.sb("lwx", [128, 128], BF16)
    lp = P.sb("lrup", [128, 8], F32)
    sp = P.sb("lsp", [128, 2], F32)
    X = [P.sb("lX%d" % i, [128, 3 + NG], F32) for i in range(2)]
    Y = [P.sb("lY%d" % i, [128, NG], F32) for i in range(2)]
    xc = P.sb("lxc", [128, NG], F32)
    xcb = P.sb("lxcb", [128, NG], BF16)
    gr = P.sb("lgr", [128, NG], F32)
    gi_ = P.sb("lgi", [128, NG], F32)
    a = P.sb("la", [128, NG], F32)
    mu = P.sb("lmu", [128, NG], F32)
    H = [P.sb("lH%d" % i, [128, NG], F32) for i in range(2)]
    do = [P.sb("ldo%d" % i, [128, NG], BF16) for i in range(2)]
    pa, px = pj
    P.dma(wa[:], wa_d, writes=[wa])
    P.dma(wx[:], wx_d, writes=[wx])
    P.dma(lp[:], lp_d, writes=[lp])
    P.op("scalar", lambda e: e.activation(out=sp[:, 0:1], in_=lp[:, 7:8], func=AF.Exp, scale=-1.0), [lp], [sp])
    P.op("scalar", lambda e: e.activation(out=sp[:, 0:1], in_=sp[:, 0:1], func=AF.Ln, bias=1.0), [sp], [sp])
    P.op("vector", lambda e: e.tensor_scalar(sp[:, 1:2], sp[:, 0:1], -8.0, None, op0=ALU.mult), [sp], [sp])
    it = 0
    for b in range(NB):
        for ci in range(T // NG):
            c0 = b * T + ci * NG
            Xb, Yb, Hb, dob = X[it % 2], Y[it % 2], H[it % 2], do[it % 2]
            Hprev = H[(it + 1) % 2]
            it += 1
            if ci == 0:
                P.op("gpsimd", lambda e, Xb=Xb: e.memset(Xb[:, 0:3], 0.0), [], [Xb])
                P.dma(Xb[:, 3:3 + NG], xl_d[:, c0:c0 + NG], writes=[Xb])
            else:
                P.dma(Xb[:, 0:3 + NG], xl_d[:, c0 - 3:c0 + NG], writes=[Xb])
            P.dma(Yb[:], yg_d[:, c0:c0 + NG], writes=[Yb])
            P.op("vector", lambda e, Xb=Xb: e.tensor_scalar(xc[:], Xb[:, 3:3 + NG], lp[:, 3:4], lp[:, 4:5], op0=ALU.mult, op1=ALU.add), [Xb, lp], [xc])
            for k in range(3):
                P.op("vector", lambda e, Xb=Xb, k=k: e.scalar_tensor_tensor(out=xc[:], in0=Xb[:, k:k + NG], scalar=lp[:, k:k + 1], in1=xc[:], op0=ALU.mult, op1=ALU.add), [Xb, lp, xc], [xc])
            P.op("gpsimd", lambda e: e.tensor_copy(xcb[:], xc[:]), [xc], [xcb])
            P.op("tensor", lambda e: e.matmul(pa[:], lhsT=wa[:], rhs=xcb[:], start=True, stop=True), [wa, xcb], [pa])
            P.op("tensor", lambda e: e.matmul(px[:], lhsT=wx[:], rhs=xcb[:], start=True, stop=True), [wx, xcb], [px])
            P.op("scalar", lambda e: e.activation(out=gr[:], in_=pa[:], func=AF.Sigmoid, bias=lp[:, 5:6]), [pa, lp], [gr])
            P.op("scalar", lambda e: e.activation(out=gi_[:], in_=px[:], func=AF.Sigmoid, bias=lp[:, 6:7]), [px, lp], [gi_])
            P.op("scalar", lambda e: e.activation(out=a[:], in_=gr[:], func=AF.Exp, scale=sp[:, 1:2]), [gr, sp], [a])
            P.op("vector", lambda e: e.tensor_tensor(out=mu[:], in0=a[:], in1=a[:], op=ALU.mult), [a], [mu])
            P.op("vector", lambda e: e.tensor_scalar(mu[:], mu[:], -1.0, 1.0, op0=ALU.mult, op1=ALU.add), [mu], [mu])
            P.op("scalar", lambda e: e.activation(out=mu[:], in_=mu[:], func=AF.Sqrt), [mu], [mu])
            P.op("gpsimd", lambda e: e.tensor_tensor(out=gi_[:], in0=gi_[:], in1=xc[:], op=ALU.mult), [gi_, xc], [gi_])
            P.op("gpsimd", lambda e: e.tensor_tensor(out=mu[:], in0=mu[:], in1=gi_[:], op=ALU.mult), [mu, gi_], [mu])
            if ci == 0:
                P.op("vector", lambda e, Hb=Hb: e.tensor_tensor_scan(Hb[:], a[:], mu[:], 0.0, op0=ALU.mult, op1=ALU.add), [a, mu], [Hb])
            else:
                P.op("vector", lambda e, Hb=Hb, Hprev=Hprev: e.tensor_tensor_scan(Hb[:], a[:], mu[:], Hprev[:, NG - 1:NG], op0=ALU.mult, op1=ALU.add), [a, mu, Hprev], [Hb])
            P.op("scalar", lambda e, Yb=Yb: e.activation(out=Yb[:], in_=Yb[:], func=AF.Gelu_apprx_tanh), [Yb], [Yb])
            P.op("gpsimd", lambda e, Hb=Hb, Yb=Yb, dob=dob: e.tensor_tensor(out=dob[:], in0=Hb[:], in1=Yb[:], op=ALU.mult), [Hb, Yb], [dob])
            P.dma(do_d[:, c0:c0 + NG], dob[:], reads=[dob])


def build_KD(T=16384, NB=2, do_mla=True, do_lru=True):
    nc = bass.Bass("TRN2", target_bir_lowering=False)
    P = Prog(nc)
    ones_d = dram_in(nc, "ones", [128, 128], F32)
    ones_f = P.sb("ones_f", [128, 128], F32)
    ones_bf = P.sb("ones_bf", [128, 128], BF16)
    P.dma(ones_f[:], ones_d, writes=[ones_f])
    P.op("vector", lambda e: e.tensor_copy(ones_bf[:], ones_f[:]), [ones_f], [ones_bf])
    pj = [P.ps("pj%d" % i, [128, 512], F32) for i in range(2)]
    if do_lru:
        emit_lru(P, nc, T, NB, pj)
    if do_mla:
        emit_mla(P, nc, T, NB, ones_bf, pj)
    wait_all_dma(P)
    P.build()
    P.close()
    return nc


def build_KW(L, CH=4096):
    nc = bass.Bass("TRN2", target_bir_lowering=False)
    P = Prog(nc)
    x_d = dram_in(nc, "wf", [128, L], F32)
    o_d = dram_out(nc, "wb", [128, L], BF16)
    xin = [P.sb("xin%d" % i, [128, CH], F32) for i in range(3)]
    xo = [P.sb("xo%d" % i, [128, CH], BF16) for i in range(3)]
    import os
    engs = os.environ.get("KW_ENGS", "vector,scalar,gpsimd").split(",")
    n0 = 0
    i = 0
    while n0 < L:
        n = min(CH, L - n0)
        a, b = xin[i % 3], xo[i % 3]
        P.dma(a[:, 0:n], x_d[:, n0:n0 + n], writes=[a])
        eng = engs[i % len(engs)]
        if eng == "scalar":
            P.op(eng, lambda e, a=a, b=b, n=n: e.activation(out=b[:, 0:n], in_=a[:, 0:n], func=AF.Copy), [a], [b])
        else:
            P.op(eng, lambda e, a=a, b=b, n=n: e.tensor_copy(b[:, 0:n], a[:, 0:n]), [a], [b])
        P.dma(o_d[:, n0:n0 + n], b[:, 0:n], reads=[b])
        n0 += n
        i += 1
    wait_all_dma(P)
    P.build()
    P.close()
    return nc


import ml_dtypes
from concourse.bass_utils import run_bass_kernel_spmd

NCORES = 8
BATCH, SEQ, DM = 2, 16384, 2048
TPC = SEQ // 4
_NP_BF16 = ml_dtypes.bfloat16
_cache = {}


def _run(name, builder, in_maps):
    if name not in _cache:
        _cache[name] = builder()
    nc = _cache[name]
    res = run_bass_kernel_spmd(nc, in_maps, core_ids=list(range(NCORES)))
    return res.results


def _pc(a):
    return np.ascontiguousarray(a)


def _g16(g):
    return _pc(g.reshape(-1, 128).T)


def _cast_weights(ws):
    names = list(ws.keys())
    flat = np.concatenate([ws[k].reshape(-1) for k in names])
    n = flat.size
    per = NCORES * 128
    L = -(-n // per)
    L = -(-L // 8) * 8
    pad = np.zeros(per * L, np.float32)
    pad[:n] = flat
    pad = pad.reshape(NCORES, 128, L)
    res = _run("KW%d" % L, lambda: build_KW(L), [{"wf": pad[c]} for c in range(NCORES)])
    out = np.concatenate([np.asarray(r["wb"]).reshape(-1) for r in res])
    outd = {}
    o = 0
    for k in names:
        sz = ws[k].size
        outd[k] = out[o:o + sz].reshape(ws[k].shape)
        o += sz
    return outd


def _mla_mask():
    m = np.zeros((128, 4, 512), np.float32)
    sl = np.arange(128)[:, None]
    ql = np.arange(512)[None, :]
    for j in range(4):
        m[:, j, :] = (128 * j + sl < (ql // 64 + 1) * 64)
    return m.reshape(128, 2048).astype(_NP_BF16)


def _ke_layer(xT_full, catT_full, L, gpost, gpre, gfpost, w_out, up, down, conv_w, conv_b):
    gs = _pc(np.concatenate([_g16(gpost), _g16(gpre), _g16(gfpost)], axis=1))
    cwm = _pc(conv_w.reshape(3, 64, 128).transpose(2, 0, 1).reshape(128, 192))
    cbm = _pc(conv_b.reshape(64, 128).T)
    ones = np.ones((128, 128), np.float32)
    ims = []
    for c in range(NCORES):
        b, j = divmod(c, 4)
        t0 = j * TPC
        xs = xT_full[b]
        cs = catT_full[b]
        if j == 0:
            xh = np.zeros((DM, 2), np.float32)
            ch = np.zeros((DM, 2), _NP_BF16)
        else:
            xh = _pc(xs[:, t0 - 2:t0])
            ch = _pc(cs[:, t0 - 2:t0])
        ims.append({"xT": _pc(xs[:, t0:t0 + TPC]), "catT": _pc(cs[:, t0:t0 + TPC]), "xhT": xh, "cathT": ch,
                    "gs": gs, "w_out": w_out, "up": up, "down": down, "cw": cwm, "cb": cbm, "ones": ones})
    res = _run("KE", lambda: build_KE(TOK=TPC), ims)
    out = []
    for b in range(BATCH):
        out.append(np.concatenate([np.asarray(res[b * 4 + j]["yT"]) for j in range(4)], axis=1))
    return out


def _ka_layer(xT_full, g, W, tiles, nout, outs, key):
    ones = np.ones((128, 128), np.float32)
    ims = []
    for c in range(NCORES):
        b, j = divmod(c, 4)
        ims.append({"xT": _pc(xT_full[b][:, j * TPC:(j + 1) * TPC]), "g": _g16(g), "W": W, "ones": ones})
    res = _run(key, lambda: build_KA(tiles, nout, outs, TOK=TPC, TOKB=1024), ims)
    z = {}
    for k in outs:
        z[k] = [np.concatenate([np.asarray(res[b * 4 + j][k]) for j in range(4)], axis=1) for b in range(BATCH)]
    return z


def kernel(x, positions, mix_pre_g, mix_post_g, ffn_pre_g, ffn_post_g,
           ffn_up, ffn_conv_w, ffn_conv_b, ffn_down,
           ab_w_in, pool_w, pool_b, pool_scale, ab_w_out,
           cd_w_in, q_norm_g, w_q_up, kv_norm_g, w_kv_up,
           lru_conv_w, lru_conv_b, lru_wa, lru_ba, lru_wx, lru_bx, lru_lambda, cd_w_out):
    f32 = lambda a: np.asarray(a, dtype=np.float32)
    x = f32(x)
    positions = np.asarray(positions).astype(np.int32)
    wb = _cast_weights({"ffn_up": f32(ffn_up), "ffn_down": f32(ffn_down), "ab_w_in": f32(ab_w_in), "ab_w_out": f32(ab_w_out),
                        "cd_w_in": f32(cd_w_in), "cd_w_out": f32(cd_w_out), "w_q_up": f32(w_q_up), "w_kv_up": f32(w_kv_up),
                        "pool_w": f32(pool_w), "lru_wa": f32(lru_wa), "lru_wx": f32(lru_wx)})
    xT = [_pc(x[b].T) for b in range(BATCH)]
    z = _ka_layer(xT, f32(mix_pre_g)[0], _pc(wb["ab_w_in"][0]), AB_TILES, 5200, AB_OUTS, "KA0")
    ident = np.eye(128, dtype=np.float32)
    ims = []
    qidx_all = []
    for c in range(NCORES):
        b, j = divmod(c, 4)
        qidx = np.concatenate([np.arange((4 * m + j) * 128, (4 * m + j + 1) * 128) for m in range(32)])
        qidx_all.append(qidx)
        ql = np.arange(128)[:, None]
        sl = np.arange(512)[None, :]
        negmask = np.where(sl < 128 * j + 64 * (ql // 64 + 1), 0.0, -1e30).astype(np.float32)
        ims.append({"qT": _pc(z["qT"][b][:, qidx]), "qiT": _pc(z["qiT"][b][:, qidx]), "wi": _pc(z["wiT"][b][:, qidx].T),
                    "kiT": z["kiT"][b], "kT": z["kT"][b], "v": _pc(z["vT"][b].T), "negmask": negmask, "ident": ident})
    res = _run("KB", lambda: build_KB(T=SEQ, NSLOT=32), ims)
    aoutT = [np.zeros((1024, SEQ), _NP_BF16) for _ in range(BATCH)]
    for c in range(NCORES):
        b, j = divmod(c, 4)
        aoutT[b][:, qidx_all[c]] = np.asarray(res[c]["aoutT"])
    del ims
    psb = _pc(np.concatenate([f32(pool_scale)[0].reshape(8, 128).T, f32(pool_b)[0].reshape(8, 128).T], axis=1))
    ims = []
    for c in range(NCORES):
        b, j = divmod(c, 4)
        t0 = j * TPC
        u = z["uT"][b]
        rcv = np.zeros((4, 16), np.float32)
        for g_, w_ in enumerate((2, 4, 8, 16)):
            rcv[g_] = 1.0 / (np.minimum(np.arange(16) + 1, w_) if j == 0 else w_)
        rc = _pc(np.broadcast_to(rcv[None, :, None, :], (128, 4, 2, 16)).reshape(128, -1))
        uh = np.zeros((1024, 16), np.float32) if j == 0 else _pc(u[:, t0 - 16:t0])
        ims.append({"uT": _pc(u[:, t0:t0 + TPC]), "uhT": uh, "rc": rc, "pw": _pc(wb["pool_w"][0]), "psb": psb})
    res = _run("KC", lambda: build_KC(TOK=TPC), ims)
    catT = []
    for b in range(BATCH):
        bout = np.concatenate([np.asarray(res[b * 4 + j]["boutT"]) for j in range(4)], axis=1)
        catT.append(np.concatenate([aoutT[b], bout], axis=0))
    del z
    x2T = _ke_layer(xT, catT, 0, f32(mix_post_g)[0], f32(ffn_pre_g)[0], f32(ffn_post_g)[0], _pc(wb["ab_w_out"][0]),
                    _pc(wb["ffn_up"][0]), _pc(wb["ffn_down"][0]), f32(ffn_conv_w)[0], f32(ffn_conv_b)[0])
    del catT, xT
    z = _ka_layer(x2T, f32(mix_pre_g)[1], _pc(wb["cd_w_in"][0]), CD_TILES, 2880, CD_OUTS, "KA1")
    allc = lambda k: np.concatenate([z[k][b] for b in range(BATCH)], axis=1)
    cq_all, ckv_all, kr_all, xl_all, yg_all = allc("cqT"), allc("ckvT"), allc("krT"), allc("xlT"), allc("ygT")
    krs_all = _pc(np.concatenate([kr_all[32:], kr_all[:32]], axis=0))
    posrep = _pc(np.broadcast_to(positions.reshape(1, -1), (64, BATCH * SEQ)))
    freq = (np.float32(10000.0) ** (-np.arange(32, dtype=np.float32) / np.float32(32))).astype(np.float32)
    fq = _pc(np.stack([np.concatenate([freq, freq]), np.concatenate([-np.ones(32), np.ones(32)])], axis=1).astype(np.float32))
    mlag = _pc(np.concatenate([f32(q_norm_g)[0].reshape(4, 128).T, f32(kv_norm_g)[0].reshape(2, 128).T], axis=1))
    mask = _mla_mask()
    ones = np.ones((128, 128), np.float32)
    ims = []
    for c in range(NCORES):
        wq_full = wb["w_q_up"][0][:, c * 192:(c + 1) * 192]
        wq = _pc(np.concatenate([wq_full, wq_full[:, 160:192], wq_full[:, 128:160]], axis=1))
        wkv = _pc(wb["w_kv_up"][0][:, c * 256:(c + 1) * 256])
        sl = slice(c * 128, (c + 1) * 128)
        lrup = _pc(np.stack([f32(lru_conv_w)[0][0, sl], f32(lru_conv_w)[0][1, sl], f32(lru_conv_w)[0][2, sl], f32(lru_conv_w)[0][3, sl],
                             f32(lru_conv_b)[0][sl], f32(lru_ba)[0][sl], f32(lru_bx)[0][sl], f32(lru_lambda)[0][sl]], axis=1).astype(np.float32))
        ims.append({"ones": ones, "cqT": cq_all, "ckvT": ckv_all, "krT": kr_all, "krsT": krs_all, "posrep": posrep, "fq": fq,
                    "wq": wq, "wkv": wkv, "mlag": mlag, "mask": mask,
                    "xlT": _pc(xl_all[sl]), "ygT": _pc(yg_all[sl]), "lwa": _pc(wb["lru_wa"][0][c]), "lwx": _pc(wb["lru_wx"][0][c]), "lrup": lrup})
    res = _run("KD", lambda: build_KD(T=SEQ, NB=BATCH), ims)
    cat_all = np.concatenate([np.asarray(res[c]["coutT"]) for c in range(NCORES)] + [np.asarray(res[c]["doutT"]) for c in range(NCORES)], axis=0)
    catT = [_pc(cat_all[:, b * SEQ:(b + 1) * SEQ]) for b in range(BATCH)]
    del ims, z, cq_all, ckv_all, kr_all, xl_all, yg_all
    yT = _ke_layer(x2T, catT, 1, f32(mix_post_g)[1], f32(ffn_pre_g)[1], f32(ffn_post_g)[1], _pc(wb["cd_w_out"][0]),
                   _pc(wb["ffn_up"][1]), _pc(wb["ffn_down"][1]), f32(ffn_conv_w)[1], f32(ffn_conv_b)[1])
    out = np.stack([_pc(yT[b].T) for b in range(BATCH)], axis=0).astype(np.float32)
    return out
```
